# Optimizing a Trainium2 kernel written in Bass

```python
import jax, jax.numpy as jnp
from jax import lax
import numpy as np


D_MODEL = 2048
BATCH = 4
SEQ = 2048
DEPTH = 1

RWKV_WIDTH = D_MODEL // 2
HEAD_SIZE = 64
N_RWKV_HEADS = RWKV_WIDTH // HEAD_SIZE
DECAY_LORA = 64
ICLR_LORA = 64
GATE_LORA = 160
GN_EPS = 64e-5
POOL_WIDTH = D_MODEL // 2
POOL_WINDOWS = (2, 4, 8, 16)
N_POOL_GROUPS = len(POOL_WINDOWS)
POOL_GROUP = POOL_WIDTH // N_POOL_GROUPS
POOL_OUT_GROUP = D_MODEL // N_POOL_GROUPS
N_SHIFT = 3 * RWKV_WIDTH + DECAY_LORA + ICLR_LORA + GATE_LORA
N_IN = N_SHIFT + POOL_WIDTH + 2 * D_MODEL
N_EXPERTS = 32
TOP_K = 4
D_FF = D_MODEL
SWIGLU_LIMIT = 7.0
SWIGLU_ALPHA = 1.702
MOE_BLOCK = 256
LN_EPS = 1e-5
DEEPNORM_ALPHA = (2 * DEPTH) ** 0.25
DEEPNORM_BETA = (8 * DEPTH) ** -0.25

kernel_name = 'hybrid_rwkv7_pool_moe_deepnorm_adaln'

F32 = jnp.float32


def layer_norm(x, g, b):
    xf = x.astype(F32)
    mu = jnp.mean(xf, axis=-1, keepdims=True)
    var = jnp.mean(jnp.square(xf - mu), axis=-1, keepdims=True)
    return ((xf - mu) * lax.rsqrt(var + LN_EPS) * g + b).astype(x.dtype)


def token_shift(u):
    return jnp.pad(u, ((0, 0), (1, 0), (0, 0)))[:, :-1]


def wkv7_scan(r, w, k, v, a, b):
    bsz, _, h, n = r.shape

    def step(S, inp):
        r_t, w_t, k_t, v_t, a_t, b_t = inp
        sa = jnp.einsum('bhvk,bhk->bhv', S, a_t)
        S = S * w_t[:, :, None, :] + sa[..., None] * b_t[:, :, None, :] + v_t[..., None] * k_t[:, :, None, :]
        return S, jnp.einsum('bhvk,bhk->bhv', S, r_t)

    xs = tuple(jnp.moveaxis(t, 1, 0) for t in (r, w, k, v, a, b))
    _, y = lax.scan(step, jnp.zeros((bsz, h, n, n), F32), xs)
    return jnp.moveaxis(y, 0, 1)


def rwkv7_branch(p, w0, w_decay_up, a0, w_iclr_up, w_glora_up, k_k, k_a, r_k, lnx_g, lnx_b):
    bsz, t, _ = p.shape
    C = RWKV_WIDTH
    r, k, v = p[..., :C], p[..., C:2 * C], p[..., 2 * C:3 * C]
    o = 3 * C
    wd = p[..., o:o + DECAY_LORA]; o += DECAY_LORA
    ad = p[..., o:o + ICLR_LORA]; o += ICLR_LORA
    gd = p[..., o:o + GATE_LORA]
    heads = lambda z: z.reshape(bsz, t, N_RWKV_HEADS, HEAD_SIZE)
    w_log = -jax.nn.softplus(-(w0 + jnp.tanh(wd) @ w_decay_up).astype(F32)) - 0.5
    decay = jnp.exp(-jnp.exp(w_log))
    iclr = jax.nn.sigmoid((a0 + ad @ w_iclr_up).astype(F32))
    g = jax.nn.sigmoid(gd) @ w_glora_up
    kk = heads((k * k_k).astype(F32))
    kk = kk / jnp.maximum(jnp.sqrt(jnp.sum(kk * kk, axis=-1, keepdims=True)), 1e-12)
    k_mod = k.astype(F32) * (1.0 + (iclr - 1.0) * k_a.astype(F32))
    rh, kh, vh = heads(r.astype(F32)), heads(k_mod), heads(v.astype(F32))
    y = wkv7_scan(rh, heads(decay), kh, vh, -kk, kk * heads(iclr))
    mu = jnp.mean(y, axis=-1, keepdims=True)
    var = jnp.mean(jnp.square(y - mu), axis=-1, keepdims=True)
    yn = ((y - mu) * lax.rsqrt(var + GN_EPS)).reshape(bsz, t, C) * lnx_g + lnx_b
    bonus = jnp.sum(rh * kh * r_k.astype(F32), axis=-1, keepdims=True) * vh
    out = (yn + bonus.reshape(bsz, t, C)) * g
    return out.astype(p.dtype)


def pool_branch(u, w_pool, pool_scale):
    bsz, t, _ = u.shape
    uf = u.astype(F32).reshape(bsz, t, N_POOL_GROUPS, POOL_GROUP)
    cs = jnp.cumsum(uf, axis=1)
    pos = jnp.arange(t)
    outs = []
    for gi, win in enumerate(POOL_WINDOWS):
        c_g = cs[:, :, gi]
        lagged = jnp.pad(c_g, ((0, 0), (win, 0), (0, 0)))[:, :t]
        cnt = jnp.minimum(pos + 1, win).astype(F32)
        outs.append((c_g - lagged) / cnt[None, :, None] - uf[:, :, gi])
    d = jnp.stack(outs, axis=2).astype(u.dtype)
    y = jnp.einsum('btgc,gcd->btgd', d, w_pool).reshape(bsz, t, D_MODEL)
    return y * pool_scale


def moe_ffn(h, w_router, b_router, w_gu, b_gu, w_down, b_down):
    bsz, t, d = h.shape
    m = bsz * t
    mk = m * TOP_K
    n_blocks = -(-mk // MOE_BLOCK) + N_EXPERTS
    hf = h.reshape(m, d)
    logits = (hf @ w_router).astype(F32) + b_router.astype(F32)
    top_val, top_idx = lax.top_k(logits, TOP_K)
    probs = jax.nn.softmax(top_val, axis=-1)
    flat_e = top_idx.reshape(mk)
    order = jnp.argsort(flat_e)
    sorted_e = flat_e[order]
    counts = jnp.bincount(flat_e, length=N_EXPERTS)
    padded = (counts + MOE_BLOCK - 1) // MOE_BLOCK * MOE_BLOCK
    pad_end = jnp.cumsum(padded)
    pad_start = pad_end - padded
    grp_start = jnp.cumsum(counts) - counts
    rank = jnp.arange(mk) - grp_start[sorted_e]
    dest_sorted = (pad_start[sorted_e] + rank).astype(jnp.int32)
    dest = jnp.zeros((mk,), jnp.int32).at[order].set(dest_sorted)
    tok = jnp.arange(mk) // TOP_K
    x_buf = jnp.zeros((n_blocks * MOE_BLOCK, d), h.dtype).at[dest].set(hf[tok])
    block_expert = jnp.minimum(
        jnp.searchsorted(pad_end, jnp.arange(n_blocks) * MOE_BLOCK, side='right'), N_EXPERTS - 1)

    def expert_block(args):
        xb, e = args
        gu = xb @ w_gu[e] + b_gu[e]
        gate = jnp.minimum(gu[:, ::2], SWIGLU_LIMIT)
        up = jnp.clip(gu[:, 1::2], -SWIGLU_LIMIT, SWIGLU_LIMIT)
        glu = gate * jax.nn.sigmoid(gate * SWIGLU_ALPHA)
        return ((up + 1.0) * glu) @ w_down[e] + b_down[e]

    y_buf = lax.map(expert_block, (x_buf.reshape(n_blocks, MOE_BLOCK, d), block_expert))
    y = y_buf.reshape(n_blocks * MOE_BLOCK, d)[dest].reshape(m, TOP_K, d)
    out = jnp.einsum('mkd,mk->md', y, probs.astype(y.dtype))
    return out.reshape(bsz, t, d)


def setup_inputs(seed: int = 0) -> dict:
    key = jax.random.key(seed)
    ks = jax.random.split(key, 32)
    L, D, C = DEPTH, D_MODEL, RWKV_WIDTH
    nrm = lambda k, shape, s: jax.random.normal(k, shape, F32) * s
    return {
        'x': nrm(ks[0], (BATCH, SEQ, D), 1.0),
        'c': nrm(ks[1], (BATCH, D), 1.0),
        'w_ada': nrm(ks[2], (L, D, 6 * D), 0.5 * D ** -0.5),
        'b_ada': nrm(ks[3], (L, 6 * D), 0.02),
        'w_in': nrm(ks[4], (L, D, N_IN), D ** -0.5),
        'mu_shift': jax.random.uniform(ks[5], (L, N_SHIFT), F32),
        'w0': jax.random.uniform(ks[6], (L, C), F32, -3.0, 1.0),
        'w_decay_up': nrm(ks[7], (L, DECAY_LORA, C), 0.5 * DECAY_LORA ** -0.5),
        'a0': nrm(ks[8], (L, C), 0.5),
        'w_iclr_up': nrm(ks[9], (L, ICLR_LORA, C), ICLR_LORA ** -0.5),
        'w_glora_up': nrm(ks[10], (L, GATE_LORA, C), GATE_LORA ** -0.5),
        'k_k': 0.85 + nrm(ks[11], (L, C), 0.02),
        'k_a': 1.0 + nrm(ks[12], (L, C), 0.02),
        'r_k': nrm(ks[13], (L, N_RWKV_HEADS, HEAD_SIZE), 0.1),
        'lnx_g': 1.0 + nrm(ks[14], (L, C), 0.02),
        'lnx_b': nrm(ks[15], (L, C), 0.02),
        'w_rwkv_out': nrm(ks[16], (L, C, D), C ** -0.5),
        'w_pool': nrm(ks[17], (L, N_POOL_GROUPS, POOL_GROUP, POOL_OUT_GROUP), POOL_GROUP ** -0.5),
        'pool_scale': 1.0 + nrm(ks[18], (L, D), 0.02),
        'w_out': nrm(ks[19], (L, D, D), DEEPNORM_BETA * D ** -0.5),
        'ln1_g': 1.0 + nrm(ks[20], (L, D), 0.02),
        'ln1_b': nrm(ks[21], (L, D), 0.02),
        'w_router': nrm(ks[22], (L, D, N_EXPERTS), D ** -0.5),
        'b_router': nrm(ks[23], (L, N_EXPERTS), 0.01),
        'w_gu': nrm(ks[24], (L, N_EXPERTS, D, 2 * D_FF), D ** -0.5),
        'b_gu': nrm(ks[25], (L, N_EXPERTS, 2 * D_FF), 0.02),
        'w_down': nrm(ks[26], (L, N_EXPERTS, D_FF, D), DEEPNORM_BETA * D_FF ** -0.5),
        'b_down': nrm(ks[27], (L, N_EXPERTS, D), 0.02),
        'ln2_g': 1.0 + nrm(ks[28], (L, D), 0.02),
        'ln2_b': nrm(ks[29], (L, D), 0.02),
    }


def reference(x, c, w_ada, b_ada, w_in, mu_shift, w0, w_decay_up, a0, w_iclr_up, w_glora_up,
              k_k, k_a, r_k, lnx_g, lnx_b, w_rwkv_out, w_pool, pool_scale, w_out, ln1_g, ln1_b,
              w_router, b_router, w_gu, b_gu, w_down, b_down, ln2_g, ln2_b):
    cond = jax.nn.silu(c)
    for l in range(DEPTH):
        mod = cond @ w_ada[l] + b_ada[l]
        sh1, sc1, g1, sh2, sc2, g2 = jnp.split(mod, 6, axis=-1)
        h = x * (1.0 + sc1[:, None]) + sh1[:, None]
        p = h @ w_in[l]
        ps = p[..., :N_SHIFT]
        ps = ps + (token_shift(ps) - ps) * mu_shift[l]
        y_rwkv = rwkv7_branch(ps, w0[l], w_decay_up[l], a0[l], w_iclr_up[l], w_glora_up[l],
                              k_k[l], k_a[l], r_k[l], lnx_g[l], lnx_b[l])
        o = N_SHIFT
        u_pool = p[..., o:o + POOL_WIDTH]
        o += POOL_WIDTH
        gate_a = jax.nn.sigmoid(p[..., o:o + D_MODEL])
        gate_b = jax.nn.sigmoid(p[..., o + D_MODEL:])
        branch_a = y_rwkv @ w_rwkv_out[l]
        branch_b = pool_branch(u_pool, w_pool[l], pool_scale[l])
        mix = (gate_a * branch_a + gate_b * branch_b) @ w_out[l]
        x = layer_norm(DEEPNORM_ALPHA * x + g1[:, None] * mix, ln1_g[l], ln1_b[l])
        h2 = x * (1.0 + sc2[:, None]) + sh2[:, None]
        f = moe_ffn(h2, w_router[l], b_router[l], w_gu[l], b_gu[l], w_down[l], b_down[l])
        x = layer_norm(DEEPNORM_ALPHA * x + g2[:, None] * f, ln2_g[l], ln2_b[l])
    return x
```

```python
from contextlib import ExitStack
import numpy as np
import concourse.bass as bass
import concourse.mybir as mybir
from concourse.bass_utils import run_bass_kernel_spmd

F32 = mybir.dt.float32
BF16 = mybir.dt.bfloat16
AF = mybir.ActivationFunctionType
ALU = mybir.AluOpType
ENGS = ["tensor", "vector", "scalar", "gpsimd", "sync"]
N_DMA_SEMS = 12
EPOCH = 20000

D = 2048
T_OWN = 1024
NEXP = 32
CAP = 512
ALPHA = 2.0 ** 0.25
LN_EPS = 1e-5
GN_EPS = 64e-5
NSHIFT = 3360
NIN = 8480


class Prog:
    def __init__(self, same_engine_sync=True):
        self.ins = []
        self.last_w = {}
        self.readers = {}
        self.same_engine_sync = same_engine_sync
        self.dma_count = [0] * N_DMA_SEMS
        self.n_dma = 0
        self.n_dma_sw = 0

    def _deps(self, reads, writes):
        deps = set()
        for k in reads:
            if k in self.last_w:
                deps.add(self.last_w[k])
        for k in writes:
            if k in self.last_w:
                deps.add(self.last_w[k])
            last_rd = {}
            for r in self.readers.get(k, ()):
                rec = self.ins[r]
                if rec["dma"] is not None or rec["eng"] == "gpsimd":
                    deps.add(r)
                elif r > last_rd.get(rec["eng"], -1):
                    last_rd[rec["eng"]] = r
            deps.update(last_rd.values())
        return deps

    def _commit(self, iid, reads, writes):
        for k in reads:
            self.readers.setdefault(k, []).append(iid)
        for k in writes:
            self.last_w[k] = iid
            self.readers[k] = []

    def op(self, eng, fn, reads=(), writes=()):
        deps = self._deps(reads, writes)
        iid = len(self.ins)
        self.ins.append(dict(eng=eng, fn=fn, deps=deps, dma=None, target=False))
        self._commit(iid, reads, writes)
        return iid

    def dma(self, eng, fn, reads=(), writes=()):
        deps = self._deps(reads, writes)
        iid = len(self.ins)
        half = N_DMA_SEMS // 2
        if eng == "gpsimd":
            s = self.n_dma_sw % half
            self.n_dma_sw += 1
        else:
            s = half + self.n_dma % half
            self.n_dma += 1
        self.dma_count[s] += 1
        self.ins.append(dict(eng=eng, fn=fn, deps=deps, dma=(s, 16 * self.dma_count[s]), target=False))
        self._commit(iid, reads, writes)
        return iid

    def barrier(self, engines=ENGS):
        deps = set()
        last = {}
        for i, r in enumerate(self.ins):
            if r["fn"] is None:
                continue
            if r["dma"] is not None:
                deps.add(i)
            else:
                last[r["eng"]] = i
        deps.update(last.values())
        for e in engines:
            self.ins.append(dict(eng=e, fn=None, deps=set(deps), dma=None, target=False))
        self.last_w = {}
        self.readers = {}

    def emit(self, nc, stack):
        ins = self.ins
        ses = self.same_engine_sync
        for r in ins:
            for d in r["deps"]:
                t = ins[d]
                if t["dma"] is None:
                    if t["eng"] == r["eng"] and (t["eng"] == "tensor" or not ses):
                        continue
                    t["target"] = True
        cnt = {e: 0 for e in ENGS}
        for r in ins:
            if r["dma"] is None and r["target"]:
                cnt[r["eng"]] += 1
                r["seq"] = cnt[r["eng"]]
        n_ep = {e: max(1, -(-cnt[e] // EPOCH)) for e in ENGS}
        psem = {e: [stack.enter_context(nc.semaphore(f"p_{e}_{k}")) for k in range(n_ep[e])] for e in ENGS}
        dsem = [stack.enter_context(nc.semaphore(f"dma_{k}")) for k in range(N_DMA_SEMS)]
        per_eng = {e: [] for e in ENGS}
        for i, r in enumerate(ins):
            per_eng[r["eng"]].append(i)
        block = stack.enter_context(nc.Block())

        def run_engine(ename, e):
            waited = {}
            for i in per_eng[ename]:
                r = ins[i]
                need = {}
                for d in r["deps"]:
                    t = ins[d]
                    if t["dma"] is not None:
                        s, v = t["dma"]
                        key = ("d", s)
                    else:
                        if t["eng"] == ename and (ename == "tensor" or not ses):
                            continue
                        ep, v = divmod(t["seq"] - 1, EPOCH)
                        v += 1
                        key = (t["eng"], ep)
                    if v > need.get(key, 0):
                        need[key] = v
                for key, v in need.items():
                    if waited.get(key, 0) >= v:
                        continue
                    waited[key] = v
                    sem = dsem[key[1]] if key[0] == "d" else psem[key[0]][key[1]]
                    e.wait_ge(sem, v)
                if r["fn"] is None:
                    continue
                if r["dma"] is not None and r["dma"][1] > 16:
                    key = ("d", r["dma"][0])
                    if waited.get(key, 0) < r["dma"][1] - 16:
                        waited[key] = r["dma"][1] - 16
                        e.wait_ge(dsem[r["dma"][0]], r["dma"][1] - 16)
                bi = r["fn"](e)
                if r["dma"] is not None:
                    bi.then_inc(dsem[r["dma"][0]], 16)
                elif r["target"]:
                    bi.then_inc(psem[ename][(r["seq"] - 1) // EPOCH], 1)

        block.tensor(lambda e: run_engine("tensor", e))
        block.vector(lambda e: run_engine("vector", e))
        block.scalar(lambda e: run_engine("scalar", e))
        block.gpsimd(lambda e: run_engine("gpsimd", e))
        block.sync(lambda e: run_engine("sync", e))


G_R, G_K, G_V = 0, 1024, 2048
G_WDAD = 3072
G_GD = 3200
G_POOL = 3360
G_GA = 4384
G_GB = 6432


class _Stop(Exception):
    pass


def build_program(stage=2, debug=False, stop=None, n_exp=NEXP):
    def CP(name):
        if stop == name:
            raise _Stop()
    nc = bass.Bass("TRN2", target_bir_lowering=False)
    P = Prog()
    AX = mybir.AxisListType

    def OP(eng, method, reads, writes, **kw):
        P.op(eng, lambda e: getattr(e, method)(**kw), reads=reads, writes=writes)

    def MM(out, lhsT, rhs, start, stop, reads, writes):
        P.op("tensor", lambda e: e.matmul(out, lhsT=lhsT, rhs=rhs, start=start, stop=stop), reads=reads, writes=writes)

    def TR(out, in_, identity, reads, writes):
        P.op("tensor", lambda e: e.transpose(out=out, in_=in_, identity=identity), reads=reads, writes=writes)

    def LD(eng, dst, src, writes, reads=()):
        P.dma(eng, lambda e: e.dma_start(out=dst, in_=src), reads=reads, writes=writes)

    dt_in = lambda n, s, d=F32: nc.dram_tensor(n, s, d, kind="ExternalInput").ap()
    dt_out = lambda n, s, d=F32: nc.dram_tensor(n, s, d, kind="ExternalOutput").ap()
    xo = dt_in("xo", [T_OWN, D]); xp = dt_in("xp", [T_OWN, D])
    cT = dt_in("cT", [128, 16]); flag = dt_in("flag", [128, 1]); pcorr = dt_in("pcorr", [128, 4, 16])
    cmask = dt_in("cmask", [128, 6, 128]); segm = dt_in("segm", [128, 512])
    w_ada = dt_in("w_ada", [D, 6 * D]); b_ada_b = dt_in("b_ada_b", [128, 6 * D])
    w_in = dt_in("w_in", [D, NIN]); muT = dt_in("muT", [128, 27]); vecT = dt_in("vecT", [128, 7, 8])
    w_dup = dt_in("w_dup", [64, 1024]); w_iup = dt_in("w_iup", [64, 1024]); w_gup = dt_in("w_gup", [160, 1024])
    w_ro = dt_in("w_ro", [1024, D]); w_pool = dt_in("w_pool", [4, 256, 512]); pscT = dt_in("pscT", [128, 16])
    w_out = dt_in("w_out", [D, D]); lnb = dt_in("lnb", [4, 128, D])
    if stage >= 2:
        w_router = dt_in("w_router", [D, NEXP]); b_router_b = dt_in("b_router_b", [128, NEXP])
        w_gu = dt_in("w_gu", [NEXP, D, 2 * D]); b_guT = dt_in("b_guT", [NEXP, 128, 32])
        w_down = dt_in("w_down", [NEXP, D, D]); b_down = dt_in("b_down", [NEXP, D])
        iota_cap = dt_in("iota_cap", [128, CAP])
    out = dt_out("out", [T_OWN, D])
    x1_d = nc.dram_tensor("x1_d", [T_OWN, D], F32, kind="Internal").ap()
    h2_d = nc.dram_tensor("h2_d", [T_OWN, D], BF16, kind="Internal").ap()
    modb_d = nc.dram_tensor("modb_d", [4, 128, D], F32, kind="Internal").ap()
    lg_d = nc.dram_tensor("lg_d", [T_OWN, NEXP], F32, kind="ExternalOutput" if debug else "Internal").ap()
    dbg = {}
    if debug:
        dbg["yr"] = dt_out("dbg_yr", [128, 8, T_OWN]); dbg["mixin"] = dt_out("dbg_mixin", [128, 16, T_OWN])
        dbg["modb"] = dt_out("dbg_modb", [128, 2 * D]); dbg["ps"] = dt_out("dbg_ps", [128, 27, T_OWN])
        dbg["ld"] = dt_out("dbg_ld", [128, 8, T_OWN]); dbg["dT"] = dt_out("dbg_dT", [128, 8, T_OWN])
        dbg["y"] = dt_out("dbg_y", [128, 8, T_OWN])
        dbg["x1"] = dt_out("dbg_x1", [T_OWN, D])
        if stage >= 2:
            dbg["pm"] = dt_out("dbg_pm", [T_OWN, NEXP]); dbg["pos"] = dt_out("dbg_pos", [T_OWN, NEXP])

    with ExitStack() as st:
        sb = lambda n, s, d=F32: st.enter_context(nc.sbuf_tensor(n, s, d))
        cm = sb("cm", [128, 6, 128])
        ident, m_su, m_sl, m_ui, m_bd, ones_f = (cm[:, i, :] for i in range(6))
        cmb = sb("cmb", [128, 6, 128], BF16)
        ident_b = cmb[:, 0, :]
        epsG = sb("epsG", [128, 1]); epsL = sb("epsL", [128, 1])
        OP("gpsimd", "memset", [], ["epsG"], ap=epsG[:], constant=GN_EPS)
        OP("gpsimd", "memset", [], ["epsL"], ap=epsL[:], constant=LN_EPS)
        NSL = 4
        slab = [sb(f"slab{i}", [128, 4096], BF16) for i in range(NSL)]
        slab_i = [0]
        pbank = [st.enter_context(nc.psum_tensor(f"pb{i}", [128, 512], F32)) for i in range(7)]
        ptb = st.enter_context(nc.psum_tensor("ptb", [128, 1024], BF16))
        st_p = ExitStack()
        sbp = lambda n, s_, d=F32: st_p.enter_context(nc.sbuf_tensor(n, s_, d))
        seg = sbp("seg", [128, 512]); flg = sbp("flg", [128, 1]); pcr = sbp("pcr", [128, 4, 16])
        mu = sbp("mu", [128, 27]); omu = sbp("omu", [128, 27]); vec = sbp("vec", [128, 7, 8]); psc = sbp("psc", [128, 16])
        condB = sbp("condB", [128, 16, 128], BF16); condf = sbp("condf", [128, 16])
        sc1p = sbp("sc1p", [128, 16]); sh1 = sbp("sh1", [128, 16])
        wlo = sbp("wlo", [128, 3, 1024], BF16)

        def load_slab(src_ap, kc, cols):
            i = slab_i[0] % NSL
            slab_i[0] += 1
            view = slab[i][:, 0:kc * cols].rearrange("p (k c) -> p k c", k=kc)
            LD("gpsimd", view, src_ap, [f"slab{i}"])
            return view, f"slab{i}"

        for dst, src, key in [(cm[:], cmask, "cm"), (seg[:], segm, "seg"), (flg[:], flag, "flg"), (pcr[:], pcorr, "pcr"), (mu[:], muT, "mu"),
                              (vec[:], vecT, "vec"), (psc[:], pscT, "psc"), (condf[:], cT, "condf")]:
            LD("sync", dst, src, [key])
        LD("gpsimd", wlo[0:64, 0, :], w_dup, ["wlo"]); LD("gpsimd", wlo[64:128, 0, :], w_iup, ["wlo"])
        LD("gpsimd", wlo[:, 1, :], w_gup[0:128, :], ["wlo"]); LD("gpsimd", wlo[0:32, 2, :], w_gup[128:160, :], ["wlo"])
        OP("vector", "tensor_copy", ["cm"], ["cmb"], out=cmb[:], in_=cm[:])
        OP("vector", "tensor_scalar", ["mu"], ["omu"], out=omu[:], in0=mu[:], scalar1=-1.0, scalar2=1.0, op0=ALU.mult, op1=ALU.add)
        OP("scalar", "activation", ["condf"], ["condf"], out=condf[:], in_=condf[:], func=AF.Silu)
        for k in range(16):
            OP("vector", "tensor_copy", ["condf"], ["condB"], out=condB[:, k, :], in_=condf[:, k:k + 1].to_broadcast([128, 128]))

        with ExitStack() as st_a:
            badd = st_a.enter_context(nc.sbuf_tensor("badd", [128, 2, 256], F32))
            mod01 = st_a.enter_context(nc.sbuf_tensor("mod01", [128, 2 * D], F32))
            mtmp = st_a.enter_context(nc.sbuf_tensor("mtmp", [128, 16, 128], F32))

            def ada_slab(si, dst_ap, dst_key):
                c0 = si * 256
                view, key = load_slab(w_ada[:, c0:c0 + 256].rearrange("(k p) c -> p k c", p=128), 16, 256)
                bi = si % 2
                LD("sync", badd[:, bi, :], b_ada_b[:, c0:c0 + 256], [f"badd{bi}"])
                pbk = pbank[si % 2]
                for k in range(16):
                    MM(pbk[:, 0:256], condB[:, k, :], view[:, k, :], k == 0, k == 15, [key, "condB"], [f"pb{si % 2}"])
                OP("vector", "tensor_tensor", [f"pb{si % 2}", f"badd{bi}"], [dst_key], out=dst_ap, in0=pbk[:, 0:256], in1=badd[:, bi, :], op=ALU.add)

            for si in range(16):
                ada_slab(si, mod01[:, si * 256:(si + 1) * 256], f"mt{si // 8}")
            for j, (dst, dkey) in enumerate([(sh1, "sh1"), (sc1p, "sc1p")]):
                OP("vector", "tensor_tensor", [f"mt{j}", "cm"], ["mtmp"], out=mtmp[:], in0=mod01[:, j * D:(j + 1) * D].rearrange("p (c q) -> p c q", q=128),
                   in1=cm[:, 0:1, :].to_broadcast([128, 16, 128]), op=ALU.mult)
                OP("vector", "tensor_reduce", ["mtmp"], [dkey], out=dst[:], in_=mtmp[:], axis=AX.X, op=ALU.add)
            OP("vector", "tensor_scalar_add", ["sc1p"], ["sc1p"], out=sc1p[:], in0=sc1p[:], scalar1=1.0)
            if debug:
                LD("sync", dbg["modb"], mod01[:], [], ["mt0", "mt1"])
            order = {2: 0, 3: 1, 4: 2, 5: 3}
            for piece in range(2, 6):
                mt = mod01[:, (piece % 2) * D:(piece % 2 + 1) * D]
                mk = f"mt{piece % 2}"
                for s8 in range(8):
                    ada_slab(piece * 8 + s8, mt[:, s8 * 256:(s8 + 1) * 256], mk)
                if piece == 4:
                    OP("vector", "tensor_scalar_add", [mk], [mk], out=mt, in0=mt, scalar1=1.0)
                LD("sync", modb_d[order[piece]], mt, [], [mk])
            P.barrier()
        if stage == 0:
            st_p.close()
            P.emit(nc, st)
            return nc

        with ExitStack() as st_m:
            sbm = lambda n, s, d=F32: st_m.enter_context(nc.sbuf_tensor(n, s, d))
            hT = sbm("hT", [128, 16, 512], BF16)
            carry = sbm("carry", [128, 27])
            halo = sbm("halo", [128, 8, 16])
            dT = sbm("dT", [128, 8, 512], BF16); yrT = sbm("yrT", [128, 8, 512], BF16); mixT = sbm("mixT", [128, 16, 512], BF16)
            Hst = sbm("Hst", [128, 8, 128])
            import os
            if not os.environ.get("NO_MEMSET"):
                OP("vector", "memset", [], ["H%d" % i for i in range(8)], ap=Hst[:].rearrange("p a b -> p (a b)"), constant=0.0)
                OP("vector", "memset", [], ["carry"], ap=carry[:], constant=0.0)
                OP("vector", "memset", [], ["halo"], ap=halo[:].rearrange("p a b -> p (a b)"), constant=0.0)
            pslot_i = [0]

            def pslot():
                i = pslot_i[0] % 4
                pslot_i[0] += 1
                return pbank[2 + i][:, 0:128], f"pb{2 + i}"

            def in_proj(col0, ncols, ntok0, ntok, pbk, pkey):
                view, key = load_slab(w_in[:, col0:col0 + ncols].rearrange("(k p) c -> p k c", p=128), 16, ncols)
                for k in range(16):
                    MM(pbk[0:ncols, 0:ntok], view[:, k, :], hT[:, k, ntok0:ntok0 + ntok], k == 0, k == 15, [key, "hT"], [pkey])

            scopes = []
            try:
                for blk in range(4):
                    own = blk >= 2
                    xsrc = xo if own else xp
                    t0 = (blk % 2) * 512
                    tcol = (blk - 2) * 512
                    st1 = ExitStack(); scopes.append(st1)
                    sb1 = lambda n, s_, d=F32: st1.enter_context(nc.sbuf_tensor(f"{n}_b{blk}", s_, d))
                    xs = [sb1(f"xs{i}", [128, D]) for i in range(2)]
                    pr = sb1("pr", [128, 8, 512], BF16); pk = sb1("pk", [128, 8, 512], BF16); pv = sb1("pv", [128, 8, 512], BF16)
                    plo = sb1("plo", [128, 3, 512])
                    upool = sb1("upool", [128, 528])
                    ptmp = [sb1(f"ptmp{i}", [128, 528]) for i in range(2)]
                    tnh = sb1("tnh", [128, 512], BF16); sgd = sb1("sgd", [128, 2, 512], BF16)
                    f_ld = sb1("f_ld", [128, 512]); f_ic = sb1("f_ic", [128, 512]); f_L = sb1("f_L", [128, 512])
                    f_t0 = sb1("f_t0", [128, 512]); f_t1 = sb1("f_t1", [128, 512]); f_t2 = sb1("f_t2", [128, 512])
                    f_kk = sb1("f_kk", [128, 512]); f_km = sb1("f_km", [128, 512]); f_bb = sb1("f_bb", [128, 512])
                    ynT = sb1("ynT", [128, 512])
                    LC = sb1("LC", [128, 4]); WC = sb1("WC", [128, 4])
                    o_r = sb1("o_r", [128, 512], BF16); o_a = sb1("o_a", [128, 512], BF16); o_b = sb1("o_b", [128, 512], BF16)
                    o_k = sb1("o_k", [128, 512], BF16); o_bh = sb1("o_bh", [128, 512], BF16); o_kh = sb1("o_kh", [128, 512], BF16)
                    rF = sb1("rF", [128, 512])
                    tm = sb1("tm", [128, 4, 4, 128], BF16)
                    bonus = sb1("bonus", [128, 512]); gT = sb1("gT", [128, 512])
                    uM = [sb1(f"uM{i}", [128, 2, 128]) for i in range(4)]
                    uN = [sb1(f"uN{i}", [128, 128]) for i in range(4)]
                    uNb = [sb1(f"uNb{i}", [128, 128], BF16) for i in range(2)]
                    uA = [sb1(f"uA{i}", [128, 3, 128], BF16) for i in range(2)]
                    uAt = sb1("uAt", [128, 128], BF16); uX = sb1("uX", [128, 128], BF16); uU = sb1("uU", [128, 128], BF16)
                    uPT = sb1("uPT", [128, 128]); uQ = sb1("uQ", [128, 128]); uRh = sb1("uRh", [128, 128])
                    uY = sb1("uY", [128, 128]); uZ = sb1("uZ", [128, 128]); ust = sb1("ust", [128, 2, 6]); uag = sb1("uag", [128, 2, 2])
                    ga, gb, t1, t2 = f_ld, f_ic, f_kk, f_km
                    glist = []
                    for j in range(8):
                        glist.append((j, G_R + 128 * j, 128, pr, j, False))
                    for j in range(8):
                        glist.append((8 + j, G_K + 128 * j, 128, pk, j, True))
                    for j in range(8):
                        glist.append((16 + j, G_V + 128 * j, 128, pv, j, True))
                    glist += [(24, G_WDAD, 128, plo, 0, True), (25, G_GD, 128, plo, 1, False), (26, G_GD + 128, 32, plo, 2, False)]

                    CP(f'pre{blk}')
                    for half in range(2):
                        for i in range(2):
                            tt = half * 2 + i
                            LD("sync", xs[i][:], xsrc[t0 + tt * 128:t0 + (tt + 1) * 128, :], [f"xs{i}"])
                        for c in range(16):
                            for i in range(2):
                                TR(pbank[6][:, i * 128:(i + 1) * 128], xs[i][:, c * 128:(c + 1) * 128], ident, [f"xs{i}", "cm"], ["pb6"])
                            OP("scalar", "activation", ["pb6", "sc1p", "sh1"], ["hT"], out=hT[:, c, half * 256:(half + 1) * 256], in_=pbank[6][:, 0:256], func=AF.Identity,
                               scale=sc1p[:, c:c + 1], bias=sh1[:, c:c + 1])
                    CP(f'i{blk}')
                    for (gi, col0, ncols, dst, di, fullp) in glist:
                        pbk, pkey = pbank[gi % 2], f"pb{gi % 2}"
                        if not own and not fullp:
                            if blk == 1:
                                in_proj(col0, ncols, 496, 16, pbk, pkey)
                                OP("vector", "tensor_copy", [pkey], ["carry"], out=carry[0:ncols, gi:gi + 1], in_=pbk[0:ncols, 15:16])
                            continue
                        in_proj(col0, ncols, 0, 512, pbk, pkey)
                        dsl = dst[0:ncols, di, :]
                        OP("scalar", "activation", [pkey, "omu"], ["f_t0"], out=f_t0[0:ncols, :], in_=pbk[0:ncols, :], func=AF.Identity, scale=omu[0:ncols, gi:gi + 1])
                        OP("vector", "scalar_tensor_tensor", [pkey, "mu", "f_t0"], [f"p{gi}"], out=dsl[:, 1:512], in0=pbk[0:ncols, 0:511], scalar=mu[0:ncols, gi:gi + 1],
                           in1=f_t0[0:ncols, 1:512], op0=ALU.mult, op1=ALU.add)
                        OP("vector", "scalar_tensor_tensor", ["carry", "mu", "f_t0"], [f"p{gi}"], out=dsl[:, 0:1], in0=carry[0:ncols, gi:gi + 1], scalar=mu[0:ncols, gi:gi + 1],
                           in1=f_t0[0:ncols, 0:1], op0=ALU.mult, op1=ALU.add)
                        OP("vector", "tensor_copy", [pkey, "carry"], ["carry"], out=carry[0:ncols, gi:gi + 1], in_=pbk[0:ncols, 511:512])
                        if debug and own and dst is plo:
                            LD("sync", dbg["ps"][0:ncols, gi, tcol:tcol + 512], dsl, [], [f"p{gi}"])
                    if blk == 1:
                        OP("vector", "tensor_scalar_mul", ["carry", "flg"], ["carry"], out=carry[:], in0=carry[:], scalar1=flg[:, 0:1])
                    CP(f'ii{blk}')
                    OP("scalar", "activation", ["p24"], ["tnh"], out=tnh[0:64, :], in_=plo[0:64, 0, :], func=AF.Tanh)
                    OP("vector", "tensor_copy", ["p24"], ["tnh"], out=tnh[64:128, :], in_=plo[64:128, 0, :])
                    if own:
                        OP("scalar", "activation", ["p25"], ["sgd"], out=sgd[:, 0, :], in_=plo[:, 1, :], func=AF.Sigmoid)
                        OP("scalar", "activation", ["p26"], ["sgd"], out=sgd[0:32, 1, :], in_=plo[0:32, 2, :], func=AF.Sigmoid)
                    for hp in range(8):
                        cs = slice(hp * 128, (hp + 1) * 128)
                        V = lambda j: vec[:, j, hp:hp + 1]
                        pb6 = pbank[6]
                        kkey, vkey, rkey = f"p{8 + hp}", f"p{16 + hp}", f"p{hp}"
                        MM(pb6[:, :], wlo[0:64, 0, cs], tnh[0:64, :], True, True, ["wlo", "tnh"], ["pb6"])
                        OP("scalar", "activation", ["pb6", "vec"], ["f_ld"], out=f_ld[:], in_=pb6[:, :], func=AF.Sigmoid, bias=V(0))
                        MM(pb6[:, :], wlo[64:128, 0, cs], tnh[64:128, :], True, True, ["wlo", "tnh"], ["pb6"])
                        OP("scalar", "activation", ["pb6", "vec"], ["f_ic"], out=f_ic[:], in_=pb6[:, :], func=AF.Sigmoid, bias=V(1))
                        OP("vector", "tensor_scalar_mul", ["f_ld"], ["f_ld"], out=f_ld[:], in0=f_ld[:], scalar1=-0.6065306597126334)
                        OP("vector", "tensor_tensor_scan", ["seg", "f_ld"], ["f_L"], out=f_L[:], data0=seg[:], data1=f_ld[:], initial=0.0, op0=ALU.mult, op1=ALU.add)
                        OP("vector", "tensor_copy", ["f_L"], ["LC"], out=LC[:], in_=f_L[:].rearrange("p (c t) -> p c t", t=128)[:, :, 127])
                        OP("scalar", "activation", ["LC"], ["WC"], out=WC[:], in_=LC[:], func=AF.Exp)
                        OP("vector", "tensor_scalar_mul", [kkey, "vec"], ["f_kk"], out=f_kk[:], in0=pk[:, hp, :], scalar1=V(2))
                        OP("gpsimd", "tensor_tensor", ["f_kk"], ["f_t0"], out=f_t0[:], in0=f_kk[:], in1=f_kk[:], op=ALU.mult)
                        MM(pb6[:, :], m_bd, f_t0[:], True, True, ["cm", "f_t0"], ["pb6"])
                        OP("scalar", "activation", ["pb6"], ["f_t1"], out=f_t1[:], in_=pb6[:, :], func=AF.Sqrt)
                        OP("vector", "tensor_scalar_max", ["f_t1"], ["f_t1"], out=f_t1[:], in0=f_t1[:], scalar1=1e-12)
                        OP("vector", "reciprocal", ["f_t1"], ["f_t1"], out=f_t1[:], in_=f_t1[:])
                        OP("vector", "tensor_tensor", ["f_kk", "f_t1"], ["f_kk"], out=f_kk[:], in0=f_kk[:], in1=f_t1[:], op=ALU.mult)
                        OP("vector", "tensor_scalar", ["f_ic", "vec"], ["f_t2"], out=f_t2[:], in0=f_ic[:], scalar1=-1.0, scalar2=V(3), op0=ALU.add, op1=ALU.mult)
                        OP("vector", "scalar_tensor_tensor", ["f_t2", kkey], ["f_km"], out=f_km[:], in0=f_t2[:], scalar=1.0, in1=pk[:, hp, :], op0=ALU.add, op1=ALU.mult)
                        OP("gpsimd", "tensor_tensor", ["f_kk", "f_ic"], ["f_bb"], out=f_bb[:], in0=f_kk[:], in1=f_ic[:], op=ALU.mult)
                        OP("scalar", "activation", ["f_L"], ["f_t0"], out=f_t0[:], in_=f_L[:], func=AF.Exp)
                        if own:
                            OP("vector", "tensor_tensor", [rkey, "f_t0"], ["rF"], out=rF[:], in0=pr[:, hp, :], in1=f_t0[:], op=ALU.mult)
                            OP("gpsimd", "tensor_copy", ["rF"], ["o_r"], out=o_r[:], in_=rF[:])
                        OP("vector", "tensor_tensor", ["f_L", "f_ld"], ["f_t1"], out=f_t1[:], in0=f_L[:], in1=f_ld[:], op=ALU.subtract)
                        OP("scalar", "activation", ["f_t1"], ["f_t1"], out=f_t1[:], in_=f_t1[:], func=AF.Exp)
                        OP("vector", "scalar_tensor_tensor", ["f_kk", "f_t1"], ["o_a"], out=o_a[:], in0=f_kk[:], scalar=-1.0, in1=f_t1[:], op0=ALU.mult, op1=ALU.mult)
                        OP("scalar", "activation", ["f_L"], ["f_t2"], out=f_t2[:], in_=f_L[:], func=AF.Exp, scale=-1.0)
                        OP("vector", "tensor_tensor", ["f_bb", "f_t2"], ["o_b"], out=o_b[:], in0=f_bb[:], in1=f_t2[:], op=ALU.mult)
                        OP("gpsimd", "tensor_tensor", ["f_km", "f_t2"], ["o_k"], out=o_k[:], in0=f_km[:], in1=f_t2[:], op=ALU.mult)
                        for c in range(4):
                            OP("scalar", "activation", ["f_L", "LC"], ["f_t0"], out=f_t0[:, c * 128:(c + 1) * 128], in_=f_L[:, c * 128:(c + 1) * 128], func=AF.Exp, scale=-1.0, bias=LC[:, c:c + 1])
                        OP("vector", "tensor_tensor", ["f_bb", "f_t0"], ["o_bh"], out=o_bh[:], in0=f_bb[:], in1=f_t0[:], op=ALU.mult)
                        OP("gpsimd", "tensor_tensor", ["f_km", "f_t0"], ["o_kh"], out=o_kh[:], in0=f_km[:], in1=f_t0[:], op=ALU.mult)
                        CP(f'prep{blk}_{hp}')
                        if debug and own:
                            LD("sync", dbg["ld"][:, hp, tcol:tcol + 512], f_ld[:], [], ["f_ld"])
                        for c in range(4):
                            tc_ = slice(c * 128, (c + 1) * 128)
                            for wi, (src, skey) in enumerate([(o_a[:, tc_], "o_a"), (o_bh[:, tc_], "o_bh"), (o_kh[:, tc_], "o_kh"), (pv[:, hp, tc_], vkey)]):
                                TR(ptb[:, wi * 128:(wi + 1) * 128], src, ident_b, [skey, "cmb"], ["ptb"])
                            if c % 2:
                                OP("vector", "tensor_copy", ["ptb"], [f"tm{c}"], out=tm[:, c, :, :], in_=ptb[:, 0:512].rearrange("p (w q) -> p w q", q=128))
                            else:
                                OP("scalar", "activation", ["ptb"], [f"tm{c}"], out=tm[:, c, :, :], in_=ptb[:, 0:512].rearrange("p (w q) -> p w q", q=128), func=AF.Identity)
                        CP(f'tm{blk}_{hp}')
                        if own:
                            OP("vector", "scalar_tensor_tensor", [rkey, "vec", "f_km"], ["f_t1"], out=f_t1[:], in0=pr[:, hp, :], scalar=V(4), in1=f_km[:], op0=ALU.mult, op1=ALU.mult)
                            MM(pb6[:, :], m_bd, f_t1[:], True, True, ["cm", "f_t1"], ["pb6"])
                            OP("vector", "tensor_tensor", ["pb6", vkey], ["bonus"], out=bonus[:], in0=pb6[:, :], in1=pv[:, hp, :], op=ALU.mult)
                            MM(pb6[:, :], wlo[:, 1, cs], sgd[:, 0, :], True, False, ["wlo", "sgd"], ["pb6"])
                            MM(pb6[:, :], wlo[0:32, 2, cs], sgd[0:32, 1, :], False, True, ["wlo", "sgd"], ["pb6"])
                            OP("scalar", "activation", ["pb6"], ["gT"], out=gT[:], in_=pb6[:, :], func=AF.Identity)
                        for c in range(4):
                            tc_ = slice(c * 128, (c + 1) * 128)
                            tmk = f"tm{c}"
                            for h in range(2):
                                pbs = slice(h * 64, (h + 1) * 64)
                                bt, at, kt, rt = o_b[pbs, tc_], o_a[pbs, tc_], o_k[pbs, tc_], o_r[pbs, tc_]
                                M0, N0 = uM[2 * h], uN[2 * h]
                                mk0 = f"uM{2 * h}"
                                specs = [(bt, at, m_su, M0[:, 0, :], mk0, ["o_b", "o_a"]), (at, bt, m_sl, M0[:, 1, :], mk0, ["o_b", "o_a"]),
                                         (kt, at, m_su, uA[h][:, 2, :], f"uA{h}", ["o_k", "o_a"])]
                                if own:
                                    specs += [(bt, rt, m_ui, uA[h][:, 0, :], f"uA{h}", ["o_b", "o_r"]), (kt, rt, m_ui, uA[h][:, 1, :], f"uA{h}", ["o_k", "o_r"])]
                                for (l_, r_, msk, dst, dkey, rk) in specs:
                                    ps_, pk_ = pslot()
                                    MM(ps_, l_, r_, True, True, rk, [pk_])
                                    OP("vector", "tensor_tensor", [pk_, "cm"], [dkey], out=dst, in0=ps_, in1=msk, op=ALU.mult)
                                CP(f'amat{blk}_{hp}_{c}_{h}')
                                OP("gpsimd", "tensor_tensor", [mk0, "cm"], [f"uN{2 * h}"], out=N0[:], in0=M0[:, 0, :], in1=ident, op=ALU.add)
                                cur, curk, Nc, Nk = M0, mk0, N0, f"uN{2 * h}"
                                CP(f'n0_{blk}_{hp}_{c}_{h}')
                                for j in range(1, 7):
                                    nxt, nxtk = uM[2 * h + (j % 2)], f"uM{2 * h + (j % 2)}"
                                    Nn, Nnk = uN[2 * h + (j % 2)], f"uN{2 * h + (j % 2)}"
                                    rk = [curk, curk + "a", curk + "b"]
                                    if j < 6:
                                        ps_, pk_ = pslot()
                                        MM(ps_, cur[:, 1, :], cur[:, 0, :], True, True, rk, [pk_])
                                        OP("vector", "tensor_copy", [pk_], [nxtk + "a"], out=nxt[:, 0, :], in_=ps_)
                                    CP(f'l{j}a_{blk}_{hp}_{c}_{h}')
                                    ps2, pk2 = pslot()
                                    MM(ps2, cur[:, 0, :], cur[:, 1, :], True, True, rk, [pk2])
                                    OP("scalar", "activation", [pk2], [nxtk + "b"], out=nxt[:, 1, :], in_=ps2, func=AF.Identity)
                                    CP(f'l{j}b_{blk}_{hp}_{c}_{h}')
                                    ps3, pk3 = pslot()
                                    MM(ps3, nxt[:, 1, :], Nc[:], True, True, [nxtk + "b", Nk], [pk3])
                                    OP("vector", "tensor_tensor", [pk3, Nk], [Nnk], out=Nn[:], in0=ps3, in1=Nc[:], op=ALU.add)
                                    cur, curk, Nc, Nk = nxt, nxtk, Nn, Nnk
                                    CP(f'l{j}c_{blk}_{hp}_{c}_{h}')
                                CP(f'neu{blk}_{hp}_{c}_{h}')
                                OP("gpsimd", "tensor_copy", [Nk], [f"uNb{h}"], out=uNb[h][:], in_=Nc[:])
                                ps_, pk_ = pslot()
                                MM(ps_[:, 0:64], uNb[h][:], tm[:, c, 0, pbs], True, True, [f"uNb{h}", tmk], [pk_])
                                OP("vector", "tensor_copy", [pk_], [f"uAt{h}"], out=uAt[:, pbs], in_=ps_[:, 0:64])
                                ps_, pk_ = pslot()
                                MM(ps_[:, 0:64], uA[h][:, 2, :], tm[:, c, 3, pbs], True, True, [f"uA{h}", tmk], [pk_])
                                OP("scalar", "activation", [pk_], [f"uX{h}"], out=uX[:, pbs], in_=ps_[:, 0:64], func=AF.Identity)
                                ps_, pk_ = pslot()
                                MM(ps_[:, 0:64], uNb[h][:], uX[:, pbs], True, True, [f"uNb{h}", f"uX{h}"], [pk_])
                                OP("vector", "tensor_copy", [pk_], [f"uU{h}"], out=uU[:, pbs], in_=ps_[:, 0:64])
                            ps_, pk_ = pslot()
                            MM(ps_, uAt[:], tm[:, c, 1, :], True, True, ["uAt0", "uAt1", tmk], [pk_])
                            OP("vector", "tensor_tensor", [pk_, "cm"], ["uPT"], out=uPT[:], in0=ps_, in1=m_bd, op=ALU.mult)
                            OP("vector", "scalar_tensor_tensor", ["uPT", "cm", "WC"], ["uPT"], out=uPT[:], in0=ident, scalar=WC[:, c:c + 1], in1=uPT[:], op0=ALU.mult, op1=ALU.add)
                            ps_, pk_ = pslot()
                            MM(ps_, tm[:, c, 1, :], uU[:], True, False, ["uU0", "uU1", tmk], [pk_])
                            MM(ps_, tm[:, c, 2, :], tm[:, c, 3, :], False, True, [tmk], [pk_])
                            OP("vector", "tensor_tensor", [pk_, "cm"], ["uQ"], out=uQ[:], in0=ps_, in1=m_bd, op=ALU.mult)
                            if own:
                                for h in range(2):
                                    pbs = slice(h * 64, (h + 1) * 64)
                                    ps_, pk_ = pslot()
                                    MM(ps_, uAt[:], uA[h][:, 0, :], True, True, ["uAt0", "uAt1", f"uA{h}"], [pk_])
                                    OP("vector", "tensor_tensor", [pk_, "rF"], [f"uRh{h}"], out=uRh[pbs, :], in0=ps_[pbs, :], in1=rF[pbs, tc_], op=ALU.add)
                                psy, pky = pslot()
                                MM(psy, uRh[:], Hst[:, hp, :], True, True, ["uRh0", "uRh1", f"H{hp}"], [pky])
                                ps0, pk0 = pslot()
                                for h in range(2):
                                    pbs = slice(h * 64, (h + 1) * 64)
                                    MM(ps0[:, pbs], uA[h][:, 0, :], uU[:, pbs], True, False, [f"uA{h}", f"uU{h}"], [pk0])
                                    MM(ps0[:, pbs], uA[h][:, 1, :], tm[:, c, 3, pbs], False, True, [f"uA{h}", tmk], [pk0])
                                OP("scalar", "activation", [pk0], ["uY"], out=uY[:], in_=ps0, func=AF.Identity)
                                OP("vector", "tensor_tensor", [pky, "uY"], ["uY"], out=uY[:], in0=psy, in1=uY[:], op=ALU.add)
                                if debug:
                                    pass
                                for h in range(2):
                                    pbs = slice(h * 64, (h + 1) * 64)
                                    OP("vector", "bn_stats", ["uY"], [f"ust{h}"], out=ust[:, h, :], in_=uY[:, pbs])
                                    OP("vector", "bn_aggr", [f"ust{h}"], [f"uag{h}"], out=uag[:, h, :], in_=ust[:, h, :])
                                    OP("scalar", "activation", [f"uag{h}"], [f"uag{h}"], out=uag[:, h, 1:2], in_=uag[:, h, 1:2], func=AF.Sqrt, bias=epsG[:, 0:1])
                                    OP("vector", "reciprocal", [f"uag{h}"], [f"uag{h}"], out=uag[:, h, 1:2], in_=uag[:, h, 1:2])
                                    OP("vector", "tensor_scalar", ["uY", f"uag{h}"], [f"uZ{h}"], out=uZ[:, pbs], in0=uY[:, pbs], scalar1=uag[:, h, 0:1], scalar2=uag[:, h, 1:2],
                                       op0=ALU.subtract, op1=ALU.mult)
                                psz, pkz = pslot()
                                TR(psz, uZ[:], ident, ["uZ0", "uZ1", "cm"], [pkz])
                                OP("scalar", "activation", [pkz, "vec"], ["ynT"], out=ynT[:, tc_], in_=psz, func=AF.Identity, scale=vec[:, 5, hp:hp + 1], bias=vec[:, 6, hp:hp + 1])
                            psh, pkh = pslot()
                            MM(psh, uPT[:], Hst[:, hp, :], True, True, ["uPT", f"H{hp}"], [pkh])
                            OP("vector", "tensor_tensor", [pkh, "uQ"], [f"H{hp}"], out=Hst[:, hp, :], in0=psh, in1=uQ[:], op=ALU.add)
                            CP(f'unit{blk}_{hp}_{c}')
                            if blk == 1 and c == 3:
                                OP("vector", "tensor_scalar_mul", [f"H{hp}", "flg"], [f"H{hp}"], out=Hst[:, hp, :], in0=Hst[:, hp, :], scalar1=flg[:, 0:1])
                        if own:
                            if debug:
                                LD("sync", dbg["y"][:, hp, tcol:tcol + 512], ynT[:], [], ["ynT"])
                            OP("vector", "tensor_tensor", ["ynT", "bonus"], ["ynT"], out=ynT[:], in0=ynT[:], in1=bonus[:], op=ALU.add)
                            OP("vector", "tensor_tensor", ["ynT", "gT"], ["yrT"], out=yrT[:, hp, :], in0=ynT[:], in1=gT[:], op=ALU.mult)
                            if debug:
                                OP("vector", "tensor_tensor", ["ynT", "gT"], ["ynT"], out=ynT[:], in0=ynT[:], in1=gT[:], op=ALU.mult)
                                LD("sync", dbg["yr"][:, hp, tcol:tcol + 512], ynT[:], [], ["ynT"])
                    if not own:
                        if blk == 1:
                            for j in range(8):
                                pbk, pkey = pbank[j % 2], f"pb{j % 2}"
                                in_proj(G_POOL + 128 * j, 128, 496, 16, pbk, pkey)
                                OP("vector", "tensor_scalar_mul", [pkey, "flg"], ["halo"], out=halo[:, j, :], in0=pbk[:, 0:16], scalar1=flg[:, 0:1])
                        P.barrier()
                        st1.close()
                        continue
                    CP(f'rwkv{blk}')
                    for j in range(8):
                        g = j // 2
                        W = (2, 4, 8, 16)[g]
                        pbk, pkey = pbank[j % 2], f"pb{j % 2}"
                        in_proj(G_POOL + 128 * j, 128, 0, 512, pbk, pkey)
                        OP("scalar", "activation", [pkey], ["upool"], out=upool[:, 16:528], in_=pbk[:, :], func=AF.Identity)
                        OP("gpsimd", "tensor_copy", ["halo"], ["upool"], out=upool[:, 0:16], in_=halo[:, j, :])
                        src, skey = upool[:, :], "upool"
                        step, pi = 1, 0
                        while step < W:
                            dstt = ptmp[pi]
                            OP("gpsimd", "tensor_tensor", [skey], [f"ptmp{pi}"], out=dstt[:, step:528], in0=src[:, step:528], in1=src[:, 0:528 - step], op=ALU.add)
                            OP("gpsimd", "tensor_copy", [skey], [f"ptmp{pi}"], out=dstt[:, 0:step], in_=src[:, 0:step])
                            src, skey = dstt[:, :], f"ptmp{pi}"
                            pi ^= 1
                            step *= 2
                        OP("vector", "scalar_tensor_tensor", [skey, "upool"], ["dT"], out=dT[:, j, :], in0=src[:, 16:528], scalar=1.0 / W, in1=upool[:, 16:528], op0=ALU.mult, op1=ALU.subtract)
                        if blk == 2:
                            OP("vector", "tensor_tensor", [skey, "pcr"], ["f_kk"], out=t1[:, 0:16], in0=src[:, 16:32], in1=pcr[:, g, :], op=ALU.mult)
                            OP("vector", "tensor_tensor", ["f_kk", "upool"], ["dT"], out=dT[:, j, 0:16], in0=t1[:, 0:16], in1=upool[:, 16:32], op=ALU.subtract)
                        OP("gpsimd", "tensor_copy", ["upool"], ["halo"], out=halo[:, j, :], in_=upool[:, 512:528])
                    CP(f'pool{blk}')
                    for c in range(16):
                        in_proj(G_GA + 128 * c, 128, 0, 512, pbank[0], "pb0")
                        OP("scalar", "activation", ["pb0"], ["f_ld"], out=ga[:], in_=pbank[0][:, :], func=AF.Sigmoid)
                        in_proj(G_GB + 128 * c, 128, 0, 512, pbank[1], "pb1")
                        OP("scalar", "activation", ["pb1"], ["f_ic"], out=gb[:], in_=pbank[1][:, :], func=AF.Sigmoid)
                        vro, kro = load_slab(w_ro[:, c * 128:(c + 1) * 128].rearrange("(k p) c -> p k c", p=128), 8, 128)
                        for k in range(8):
                            MM(pbank[0][:, :], vro[:, k, :], yrT[:, k, :], k == 0, k == 7, [kro, "yrT"], ["pb0"])
                        g = c // 4
                        vpo, kpo = load_slab(w_pool[g, :, (c % 4) * 128:(c % 4 + 1) * 128].rearrange("(k p) c -> p k c", p=128), 2, 128)
                        for k in range(2):
                            MM(pbank[1][:, :], vpo[:, k, :], dT[:, 2 * g + k, :], k == 0, k == 1, [kpo, "dT"], ["pb1"])
                        OP("vector", "tensor_tensor", ["pb0", "f_ld"], ["f_kk"], out=t1[:], in0=pbank[0][:, :], in1=ga[:], op=ALU.mult)
                        OP("vector", "scalar_tensor_tensor", ["pb1", "psc", "f_ic"], ["f_km"], out=t2[:], in0=pbank[1][:, :], scalar=psc[:, c:c + 1], in1=gb[:], op0=ALU.mult, op1=ALU.mult)
                        OP("gpsimd", "tensor_tensor", ["f_kk", "f_km"], ["mixT"], out=mixT[:, c, :], in0=t1[:], in1=t2[:], op=ALU.add)
                        if debug:
                            OP("gpsimd", "tensor_tensor", ["f_kk", "f_km"], ["f_ld"], out=ga[:], in0=t1[:], in1=t2[:], op=ALU.add)
                            LD("sync", dbg["mixin"][:, c, tcol:tcol + 512], ga[:], [], ["f_ld"])
                    CP(f'comb{blk}')
                    P.barrier()
                    st1.close()
                    st2 = ExitStack(); scopes.append(st2)
                    sb2 = lambda n, s_, d=F32: st2.enter_context(nc.sbuf_tensor(f"{n}_c{blk}", s_, d))
                    xs = [sb2(f"xs{i}", [128, D]) for i in range(2)]
                    x1p = sb2("x1p", [128, D]); lnst = sb2("lnst", [128, 4, 6]); lnag = sb2("lnag", [128, 2]); h2t = sb2("h2t", [128, D], BF16)
                    lnw = sb2("lnw", [128, 2, D]); modb = sb2("modb", [128, 3, D])
                    LD("sync", lnw[:, 0, :], lnb[0], ["lnw"]); LD("sync", lnw[:, 1, :], lnb[1], ["lnw"])
                    for k_ in range(3):
                        LD("sync", modb[:, k_, :], modb_d[k_], ["modb"])
                    if stage >= 2:
                        h2T = sb2("h2T", [128, 16, 128]); wr = sb2("wr", [128, 16, NEXP]); brb = sb2("brb", [128, NEXP]); lgt = sb2("lgt", [128, NEXP])
                        LD("sync", wr[:], w_router.rearrange("(k p) e -> p k e", p=128), ["wr"])
                        LD("sync", brb[:], b_router_b, ["brb"])
                    for tt in range(4):
                        LD("sync", xs[0][:], xo[tcol + tt * 128:tcol + (tt + 1) * 128, :], ["xs0"])
                        OP("gpsimd", "tensor_scalar_mul", ["xs0"], ["xs0"], out=xs[0][:], in0=xs[0][:], scalar1=ALPHA)
                        for s8 in range(8):
                            vwo, kwo = load_slab(w_out[:, s8 * 256:(s8 + 1) * 256].rearrange("(k p) c -> p k c", p=128), 16, 256)
                            pbk, pkey = pbank[s8 % 2], f"pb{s8 % 2}"
                            for k in range(16):
                                MM(pbk[:, 0:256], mixT[:, k, tt * 128:(tt + 1) * 128], vwo[:, k, :], k == 0, k == 15, [kwo, "mixT"], [pkey])
                            dsl = slice(s8 * 256, (s8 + 1) * 256)
                            OP("vector", "tensor_tensor", [pkey, "modb"], ["x1p"], out=x1p[:, dsl], in0=pbk[:, 0:256], in1=modb[:, 0, dsl], op=ALU.mult)
                            OP("gpsimd", "tensor_tensor", ["x1p", "xs0"], ["x1p"], out=x1p[:, dsl], in0=x1p[:, dsl], in1=xs[0][:, dsl], op=ALU.add)
                        for q in range(4):
                            OP("vector", "bn_stats", ["x1p"], ["lnst"], out=lnst[:, q, :], in_=x1p[:, q * 512:(q + 1) * 512])
                        OP("vector", "bn_aggr", ["lnst"], ["lnag"], out=lnag[:], in_=lnst[:].rearrange("p a b -> p (a b)"))
                        OP("scalar", "activation", ["lnag"], ["lnag"], out=lnag[:, 1:2], in_=lnag[:, 1:2], func=AF.Sqrt, bias=epsL[:, 0:1])
                        OP("vector", "reciprocal", ["lnag"], ["lnag"], out=lnag[:, 1:2], in_=lnag[:, 1:2])
                        OP("vector", "tensor_scalar", ["x1p", "lnag"], ["x1p"], out=x1p[:], in0=x1p[:], scalar1=lnag[:, 0:1], scalar2=lnag[:, 1:2], op0=ALU.subtract, op1=ALU.mult)
                        OP("gpsimd", "tensor_tensor", ["x1p", "lnw"], ["x1p"], out=x1p[:], in0=x1p[:], in1=lnw[:, 0, :], op=ALU.mult)
                        OP("vector", "tensor_tensor", ["x1p", "lnw"], ["x1p"], out=x1p[:], in0=x1p[:], in1=lnw[:, 1, :], op=ALU.add)
                        LD("sync", x1_d[tcol + tt * 128:tcol + (tt + 1) * 128, :], x1p[:], [], ["x1p"])
                        if debug:
                            LD("sync", dbg["x1"][tcol + tt * 128:tcol + (tt + 1) * 128, :], x1p[:], [], ["x1p"])
                        OP("gpsimd", "tensor_tensor", ["x1p", "modb"], ["xs1"], out=xs[1][:], in0=x1p[:], in1=modb[:, 2, :], op=ALU.mult)
                        OP("vector", "tensor_tensor", ["xs1", "modb"], ["h2t"], out=h2t[:], in0=xs[1][:], in1=modb[:, 1, :], op=ALU.add)
                        if stage >= 2:
                            OP("gpsimd", "tensor_tensor", ["xs1", "modb"], ["xs1"], out=xs[1][:], in0=xs[1][:], in1=modb[:, 1, :], op=ALU.add)
                            for q4 in range(4):
                                for u_ in range(4):
                                    c_ = q4 * 4 + u_
                                    TR(pbank[6][:, u_ * 128:(u_ + 1) * 128], xs[1][:, c_ * 128:(c_ + 1) * 128], ident, ["xs1", "cm"], ["pb6"])
                                OP("scalar", "activation", ["pb6"], ["h2T"], out=h2T[:, q4 * 4:(q4 + 1) * 4, :], in_=pbank[6][:, :].rearrange("p (u q) -> p u q", q=128), func=AF.Identity)
                            for c_ in range(16):
                                MM(pbank[2][:, 0:NEXP], h2T[:, c_, :], wr[:, c_, :], c_ == 0, c_ == 15, ["h2T", "wr"], ["pb2"])
                            OP("vector", "tensor_tensor", ["pb2", "brb"], ["lgt"], out=lgt[:], in0=pbank[2][:, 0:NEXP], in1=brb[:], op=ALU.add)
                            LD("sync", lg_d[tcol + tt * 128:tcol + (tt + 1) * 128, :], lgt[:], [], ["lgt"])
                        LD("sync", h2_d[tcol + tt * 128:tcol + (tt + 1) * 128, :], h2t[:], [], ["h2t"])
                    P.barrier()
                    st2.close()
            except _Stop:
                P.barrier()
                for sc_ in reversed(scopes):
                    sc_.close()
            P.barrier()
        if stage == 1:
            st_p.close()
            with ExitStack() as st_o:
                ot = st_o.enter_context(nc.sbuf_tensor("ot", [128, D], F32))
                OP("gpsimd", "memset", [], ["ot"], ap=ot[:], constant=0.0)
                for tt in range(8):
                    LD("sync", out[tt * 128:(tt + 1) * 128, :], ot[:], [], ["ot"])
                P.barrier()
            P.emit(nc, st)
            return nc
        st_p.close()
        moe_phase(nc, P, st, dict(OP=OP, MM=MM, TR=TR, LD=LD, slab=slab, NSL=NSL, pbank=pbank, ptb=ptb, cm=cm, cmb=cmb, epsL=epsL,
                                  h2_d=h2_d, x1_d=x1_d, lg_d=lg_d, modb_d=modb_d, lnb=lnb, w_gu=w_gu, b_guT=b_guT, w_down=w_down, b_down=b_down,
                                  iota_cap=iota_cap, out=out, debug=debug, dbg=dbg, n_exp=n_exp))
        P.emit(nc, st)
    return nc


def moe_phase(nc, P, st, env):
    OP, MM, TR, LD = env["OP"], env["MM"], env["TR"], env["LD"]
    slab, NSL, pbank, ptb, cm, cmb, epsL = env["slab"], env["NSL"], env["pbank"], env["ptb"], env["cm"], env["cmb"], env["epsL"]
    h2_d, x1_d, lg_d, modb_d, lnb = env["h2_d"], env["x1_d"], env["lg_d"], env["modb_d"], env["lnb"]
    w_gu, b_guT, w_down, b_down, iota_cap, out = env["w_gu"], env["b_guT"], env["w_down"], env["b_down"], env["iota_cap"], env["out"]
    debug, dbg, n_exp = env["debug"], env["dbg"], env["n_exp"]
    ident, m_su, ones_f = cm[:, 0, :], cm[:, 1, :], cm[:, 5, :]
    ident_b = cmb[:, 0, :]
    NT = T_OWN // 128
    NR = CAP // 128
    with ExitStack() as sm:
        sb = lambda n, s_, d=F32: sm.enter_context(nc.sbuf_tensor(n, s_, d))
        h2 = sb("h2", [128, NT, D], BF16)
        acc = sb("acc", [128, NT, D])
        iot = sb("iot", [128, CAP])
        lg = sb("lg", [128, NT, NEXP]); pm = sb("pm", [128, NT, NEXP]); pos = sb("pos", [128, NT, NEXP]); msk = sb("msk", [128, NT, NEXP])
        mx8 = sb("mx8", [128, 8]); ssum = sb("ssum", [128, 1]); nmx = sb("nmx", [128, 1])
        bgu = [sb(f"bgu{i}", [128, 32]) for i in range(2)]
        bdn = sb("bdn", [32, D], BF16)
        pmT = sb("pmT", [32, T_OWN], BF16)
        sm2 = ExitStack()
        sb2 = lambda n, s_, d=F32: sm2.enter_context(nc.sbuf_tensor(n, s_, d))
        XT = sb2("XT", [128, 16, CAP], BF16)
        actT = sb2("actT", [128, 16, CAP], BF16)
        Sel = sb2("Sel", [128, NT, CAP], BF16)
        SelW = [sb2(f"SelW{i}", [128, CAP], BF16) for i in range(2)]
        SelP = sb2("SelP", [128, NR, T_OWN], BF16)
        Yb = [sb2(f"Yb{i}", [128, NR, 256], BF16) for i in range(2)]
        gc = sb2("gc", [128, CAP]); sg = sb2("sg", [128, CAP]); u1 = sb2("u1", [128, CAP])

        for i in range(NT):
            LD("sync", h2[:, i, :], h2_d[i * 128:(i + 1) * 128, :], [f"h2_{i}"])
        LD("sync", lg[:], lg_d.rearrange("(i p) e -> p i e", p=128), ["lg"])
        LD("sync", iot[:], iota_cap, ["iot"])
        LD("gpsimd", bdn[:], b_down, ["bdn"])
        for i in range(NT):
            OP("gpsimd", "memset", [], [f"acc{i}"], ap=acc[:, i, :], constant=0.0)
        for i in range(NT):
            OP("vector", "max", ["lg"], ["mx8"], out=mx8[:], in_=lg[:, i, :])
            OP("vector", "tensor_scalar", ["lg", "mx8"], ["msk"], out=msk[:, i, :], in0=lg[:, i, :], scalar1=mx8[:, 3:4], scalar2=None, op0=ALU.is_ge)
            OP("vector", "tensor_scalar_mul", ["mx8"], ["nmx"], out=nmx[:], in0=mx8[:, 0:1], scalar1=-1.0)
            OP("scalar", "activation", ["lg", "nmx"], ["pm"], out=pm[:, i, :], in_=lg[:, i, :], func=AF.Exp, bias=nmx[:, 0:1], scale=1.0)
            OP("vector", "tensor_tensor", ["pm", "msk"], ["pm"], out=pm[:, i, :], in0=pm[:, i, :], in1=msk[:, i, :], op=ALU.mult)
            OP("vector", "tensor_reduce", ["pm"], ["ssum"], out=ssum[:], in_=pm[:, i, :], axis=mybir.AxisListType.X, op=ALU.add)
            OP("vector", "reciprocal", ["ssum"], ["ssum"], out=ssum[:], in_=ssum[:])
            OP("vector", "tensor_scalar_mul", ["pm", "ssum"], ["pm"], out=pm[:, i, :], in0=pm[:, i, :], scalar1=ssum[:, 0:1])
        for i in range(NT):
            pb = pbank[2 + (i % 2)]
            pkey = f"pb{2 + (i % 2)}"
            MM(pb[:, 0:NEXP], m_su, msk[:, i, :], True, i == 0, ["cm", "msk"], [pkey])
            for i2 in range(i):
                MM(pb[:, 0:NEXP], ones_f, msk[:, i2, :], False, i2 == i - 1, ["cm", "msk"], [pkey])
            OP("vector", "tensor_tensor", [pkey, "msk"], ["pos"], out=pos[:, i, :], in0=pb[:, 0:NEXP], in1=msk[:, i, :], op=ALU.mult)
            OP("vector", "tensor_tensor", ["pos", "msk"], ["pos"], out=pos[:, i, :], in0=pos[:, i, :], in1=msk[:, i, :], op=ALU.add)
            OP("vector", "tensor_scalar_add", ["pos"], ["pos"], out=pos[:, i, :], in0=pos[:, i, :], scalar1=-1.0)
            OP("vector", "tensor_copy", ["pm"], ["SelW0"], out=SelW[0][:, 0:NEXP], in_=pm[:, i, :])
            TR(ptb[0:NEXP, 0:128], SelW[0][:, 0:NEXP], ident_b, ["SelW0", "cmb"], ["ptb"])
            OP("vector", "tensor_copy", ["ptb"], ["pmT"], out=pmT[:, i * 128:(i + 1) * 128], in_=ptb[0:NEXP, 0:128])
        if debug:
            LD("sync", dbg["pm"].rearrange("(i p) e -> p i e", p=128), pm[:], [], ["pm"])
            LD("sync", dbg["pos"].rearrange("(i p) e -> p i e", p=128), pos[:], [], ["pos"])

        slabs = []
        for e in range(n_exp):
            for j in range(16):
                slabs.append(w_gu[e, :, 256 * j:256 * (j + 1)].rearrange("(k p) c -> p k c", p=128))
            for s8 in range(8):
                slabs.append(w_down[e, :, 256 * s8:256 * (s8 + 1)].rearrange("(k p) c -> p k c", p=128))
        issued = [0]

        def get_slab(idx, look=2):
            while issued[0] <= min(idx + look, len(slabs) - 1):
                k_ = issued[0]
                b_ = k_ % NSL
                view = slab[b_][:, 0:4096].rearrange("p (k c) -> p k c", k=16)
                LD("gpsimd", view, slabs[k_], [f"slab{b_}"])
                issued[0] += 1
            b_ = idx % NSL
            return slab[b_][:, 0:4096].rearrange("p (k c) -> p k c", k=16), f"slab{b_}"

        sidx = 0
        for e in range(n_exp):
            bg = bgu[e % 2]
            bgk = f"bgu{e % 2}"
            LD("sync", bg[:], b_guT[e], [bgk])
            for i in range(NT):
                OP("vector", "tensor_scalar", ["iot", "pos"], [f"Sel{i}"], out=Sel[:, i, :], in0=iot[:], scalar1=pos[:, i, e:e + 1], scalar2=None, op0=ALU.is_equal)
                sw, swk = SelW[i % 2], f"SelW{i % 2}"
                OP("vector", "tensor_scalar", ["iot", "pos", "pm"], [swk], out=sw[:], in0=iot[:], scalar1=pos[:, i, e:e + 1], scalar2=pm[:, i, e:e + 1], op0=ALU.is_equal, op1=ALU.mult)
                for r in range(NR):
                    TR(ptb[:, r * 128:(r + 1) * 128], sw[:, r * 128:(r + 1) * 128], ident_b, [swk, "cmb"], ["ptb"])
                OP("scalar", "activation", ["ptb"], [f"SelP{i}"], out=SelP[:, :, i * 128:(i + 1) * 128], in_=ptb[:, 0:NR * 128].rearrange("p (r q) -> p r q", q=128), func=AF.Identity)
            for c in range(16):
                pb, pkey = pbank[c % 2], f"pb{c % 2}"
                for i in range(NT):
                    MM(pb[:, 0:CAP], h2[:, i, c * 128:(c + 1) * 128], Sel[:, i, :], i == 0, i == NT - 1, [f"h2_{i}", f"Sel{i}"], [pkey])
                OP("scalar", "activation", [pkey], [f"XT{c}"], out=XT[:, c, :], in_=pb[:, 0:CAP], func=AF.Identity)
            xkeys = [f"XT{c}" for c in range(16)]
            for j in range(16):
                view, skey = get_slab(sidx); sidx += 1
                for k in range(16):
                    MM(pbank[2][:, 0:CAP], view[:, k, 0:256:2], XT[:, k, :], k == 0, k == 15, [skey, f"XT{k}"], ["pb2"])
                for k in range(16):
                    MM(pbank[3][:, 0:CAP], view[:, k, 1:256:2], XT[:, k, :], k == 0, k == 15, [skey, f"XT{k}"], ["pb3"])
                OP("vector", "tensor_scalar", ["pb2", bgk], ["gc"], out=gc[:], in0=pbank[2][:, 0:CAP], scalar1=bg[:, j:j + 1], scalar2=7.0, op0=ALU.add, op1=ALU.min)
                OP("scalar", "activation", ["gc"], ["sg"], out=sg[:], in_=gc[:], func=AF.Sigmoid, scale=1.702)
                OP("vector", "tensor_scalar", ["pb3", bgk], ["u1"], out=u1[:], in0=pbank[3][:, 0:CAP], scalar1=bg[:, 16 + j:17 + j], scalar2=7.0, op0=ALU.add, op1=ALU.min)
                OP("vector", "tensor_scalar", ["u1"], ["u1"], out=u1[:], in0=u1[:], scalar1=-7.0, scalar2=1.0, op0=ALU.max, op1=ALU.add)
                OP("vector", "tensor_tensor", ["gc", "sg"], ["sg"], out=sg[:], in0=gc[:], in1=sg[:], op=ALU.mult)
                OP("vector", "tensor_tensor", ["u1", "sg"], [f"act{j}"], out=actT[:, j, :], in0=u1[:], in1=sg[:], op=ALU.mult)
            akeys = [f"act{j}" for j in range(16)]
            for s8 in range(8):
                view, skey = get_slab(sidx); sidx += 1
                yb, ybk = Yb[s8 % 2], f"Yb{s8 % 2}"
                dsl = slice(s8 * 256, (s8 + 1) * 256)
                for r in range(NR):
                    pb, pkey = pbank[r % 2], f"pb{r % 2}"
                    for j in range(16):
                        MM(pb[:, 0:256], actT[:, j, r * 128:(r + 1) * 128], view[:, j, :], j == 0, j == 15, [skey, f"act{j}"], [pkey])
                    OP("scalar", "activation", [pkey], [ybk], out=yb[:, r, :], in_=pb[:, 0:256], func=AF.Identity)
                for i in range(NT):
                    pb, pkey = pbank[4 + (i % 2)], f"pb{4 + (i % 2)}"
                    for r in range(NR):
                        MM(pb[:, 0:256], SelP[:, r, i * 128:(i + 1) * 128], yb[:, r, :], r == 0, r == NR - 1, [f"SelP{i}", ybk], [pkey])
                    OP("vector", "tensor_tensor", [pkey, f"acc{i}"], [f"acc{i}"], out=acc[:, i, dsl], in0=pb[:, 0:256], in1=acc[:, i, dsl], op=ALU.add)
        P.barrier()
        sm2.close()
        for i in range(NT):
            for s8 in range(8):
                dsl = slice(s8 * 256, (s8 + 1) * 256)
                pb, pkey = pbank[4 + (s8 % 2)], f"pb{4 + (s8 % 2)}"
                MM(pb[:, 0:256], pmT[0:n_exp, i * 128:(i + 1) * 128], bdn[0:n_exp, dsl], True, True, ["pmT", "bdn"], [pkey])
                OP("vector", "tensor_tensor", [pkey, f"acc{i}"], [f"acc{i}"], out=acc[:, i, dsl], in0=pb[:, 0:256], in1=acc[:, i, dsl], op=ALU.add)
        g2b = sb("g2b", [128, D]); l2g = sb("l2g", [128, D]); l2b = sb("l2b", [128, D])
        xt = [sb(f"xt{i}", [128, D]) for i in range(2)]
        st6 = sb("st6", [128, 4, 6]); ag = sb("ag", [128, 2])
        LD("sync", g2b[:], modb_d[3], ["g2b"]); LD("sync", l2g[:], lnb[2], ["l2g"]); LD("sync", l2b[:], lnb[3], ["l2b"])
        for i in range(NT):
            x_, xk = xt[i % 2], f"xt{i % 2}"
            ak = f"acc{i}"
            LD("sync", x_[:], x1_d[i * 128:(i + 1) * 128, :], [xk])
            OP("gpsimd", "tensor_tensor", [ak, "g2b"], [ak], out=acc[:, i, :], in0=acc[:, i, :], in1=g2b[:], op=ALU.mult)
            OP("vector", "scalar_tensor_tensor", [xk, ak], [ak], out=acc[:, i, :], in0=x_[:], scalar=ALPHA, in1=acc[:, i, :], op0=ALU.mult, op1=ALU.add)
            for q in range(4):
                OP("vector", "bn_stats", [ak], ["st6"], out=st6[:, q, :], in_=acc[:, i, q * 512:(q + 1) * 512])
            OP("vector", "bn_aggr", ["st6"], ["ag"], out=ag[:], in_=st6[:].rearrange("p a b -> p (a b)"))
            OP("scalar", "activation", ["ag"], ["ag"], out=ag[:, 1:2], in_=ag[:, 1:2], func=AF.Sqrt, bias=epsL[:, 0:1])
            OP("vector", "reciprocal", ["ag"], ["ag"], out=ag[:, 1:2], in_=ag[:, 1:2])
            OP("vector", "tensor_scalar", [ak, "ag"], [ak], out=acc[:, i, :], in0=acc[:, i, :], scalar1=ag[:, 0:1], scalar2=ag[:, 1:2], op0=ALU.subtract, op1=ALU.mult)
            OP("gpsimd", "tensor_tensor", [ak, "l2g"], [ak], out=acc[:, i, :], in0=acc[:, i, :], in1=l2g[:], op=ALU.mult)
            OP("vector", "tensor_tensor", [ak, "l2b"], [ak], out=acc[:, i, :], in0=acc[:, i, :], in1=l2b[:], op=ALU.add)
            LD("sync", out[i * 128:(i + 1) * 128, :], acc[:, i, :], [], [ak])
        P.barrier()


def prep_inputs(inp, stage=2, cores=range(8)):
    f = lambda a: np.ascontiguousarray(a, dtype=np.float32)
    x = inp["x"]; c = inp["c"]
    idx = np.arange(128)
    cmask = np.zeros((128, 6, 128), np.float32)
    cmask[:, 0] = np.eye(128)
    cmask[:, 1] = idx[:, None] < idx[None, :]
    cmask[:, 2] = idx[:, None] > idx[None, :]
    cmask[:, 3] = idx[:, None] <= idx[None, :]
    cmask[:, 4] = (idx[:, None] // 64) == (idx[None, :] // 64)
    cmask[:, 5] = 1.0
    segm = np.ones((128, 512), np.float32); segm[:, ::128] = 0.0
    mu = inp["mu_shift"][0]
    muT = np.zeros((128, 27), np.float32)
    for gi in range(24):
        muT[:, gi] = mu[128 * gi:128 * (gi + 1)]
    muT[:, 24] = mu[3072:3200]; muT[:, 25] = mu[3200:3328]; muT[:32, 26] = mu[3328:3360]
    vecs = [inp["w0"][0], inp["a0"][0], inp["k_k"][0], inp["k_a"][0], inp["r_k"][0].reshape(-1), inp["lnx_g"][0], inp["lnx_b"][0]]
    vecT = np.stack([v.reshape(8, 128).T for v in vecs], axis=1)
    shared = dict(
        cmask=cmask, segm=segm, w_ada=f(inp["w_ada"][0]), b_ada_b=f(np.broadcast_to(inp["b_ada"][0][None, :], (128, 6 * D))),
        w_in=f(inp["w_in"][0]), muT=muT, vecT=f(vecT), w_dup=f(inp["w_decay_up"][0]), w_iup=f(inp["w_iclr_up"][0]),
        w_gup=f(inp["w_glora_up"][0]), w_ro=f(inp["w_rwkv_out"][0]), w_pool=f(inp["w_pool"][0]),
        pscT=f(inp["pool_scale"][0].reshape(16, 128).T), w_out=f(inp["w_out"][0]),
        lnb=f(np.stack([np.broadcast_to(inp[k][0][None, :], (128, D)) for k in ("ln1_g", "ln1_b", "ln2_g", "ln2_b")])),
    )
    if stage >= 2:
        shared.update(
            w_router=f(inp["w_router"][0]), b_router_b=f(np.broadcast_to(inp["b_router"][0][None, :], (128, NEXP))),
            w_gu=f(inp["w_gu"][0]), w_down=f(inp["w_down"][0]), b_down=f(inp["b_down"][0]),
            b_guT=f(np.concatenate([inp["b_gu"][0][:, 0::2].reshape(NEXP, 16, 128), inp["b_gu"][0][:, 1::2].reshape(NEXP, 16, 128)], axis=1).transpose(0, 2, 1)),
            iota_cap=f(np.broadcast_to(np.arange(CAP, dtype=np.float32)[None, :], (128, CAP))),
        )
    maps = []
    for core in cores:
        b, half = core // 2, core % 2
        m = dict(shared)
        m["xo"] = f(x[b, half * T_OWN:(half + 1) * T_OWN])
        m["xp"] = f(x[b, 0:T_OWN])
        m["cT"] = f(c[b].reshape(16, 128).T)
        m["flag"] = np.full((128, 1), float(half), np.float32)
        pc = np.zeros((128, 4, 16), np.float32)
        for g, W in enumerate((2, 4, 8, 16)):
            pc[:, g, :] = 1.0 / W if half == 1 else 1.0 / np.minimum(np.arange(16) + 1, W)
        m["pcorr"] = pc
        maps.append(m)
    return maps


_NC_CACHE = {}


def kernel(**inputs):
    if "full" not in _NC_CACHE:
        _NC_CACHE["full"] = build_program(stage=2, debug=False)
    nc = _NC_CACHE["full"]
    maps = prep_inputs(inputs, stage=2)
    res = run_bass_kernel_spmd(nc, maps, core_ids=list(range(8)))
    out = np.zeros((4, 2048, D), np.float32)
    for core in range(8):
        b, half = core // 2, core % 2
        out[b, half * T_OWN:(half + 1) * T_OWN] = res.results[core]["out"]
    return out
```

```python
from contextlib import ExitStack
import numpy as np
import concourse.bass as bass
import concourse.mybir as mybir
from concourse.bass_utils import run_bass_kernel_spmd

F32 = mybir.dt.float32
BF16 = mybir.dt.bfloat16
AF = mybir.ActivationFunctionType
ALU = mybir.AluOpType
ENGS = ["tensor", "vector", "scalar", "gpsimd", "sync"]
N_DMA_SEMS = 12
EPOCH = 20000

D = 2048
T_OWN = 1024
NEXP = 32
CAP = 512
ALPHA = 2.0 ** 0.25
LN_EPS = 1e-5
GN_EPS = 64e-5
NSHIFT = 3360
NIN = 8480


class Prog:
    def __init__(self, same_engine_sync=True):
        self.ins = []
        self.last_w = {}
        self.readers = {}
        self.same_engine_sync = same_engine_sync
        self.dma_count = [0] * N_DMA_SEMS
        self.n_dma = 0
        self.n_dma_sw = 0

    def _deps(self, reads, writes):
        deps = set()
        for k in reads:
            if k in self.last_w:
                deps.add(self.last_w[k])
        for k in writes:
            if k in self.last_w:
                deps.add(self.last_w[k])
            for r in self.readers.get(k, ()):
                deps.add(r)
        return deps

    def _commit(self, iid, reads, writes):
        for k in reads:
            self.readers.setdefault(k, []).append(iid)
        for k in writes:
            self.last_w[k] = iid
            self.readers[k] = []

    def op(self, eng, fn, reads=(), writes=()):
        deps = self._deps(reads, writes)
        iid = len(self.ins)
        self.ins.append(dict(eng=eng, fn=fn, deps=deps, dma=None, target=False))
        self._commit(iid, reads, writes)
        return iid

    def dma(self, eng, fn, reads=(), writes=()):
        deps = self._deps(reads, writes)
        iid = len(self.ins)
        half = N_DMA_SEMS // 2
        if eng == "gpsimd":
            s = self.n_dma_sw % half
            self.n_dma_sw += 1
        else:
            s = half + self.n_dma % half
            self.n_dma += 1
        self.dma_count[s] += 1
        self.ins.append(dict(eng=eng, fn=fn, deps=deps, dma=(s, 16 * self.dma_count[s]), target=False))
        self._commit(iid, reads, writes)
        return iid

    def barrier(self, engines=ENGS):
        deps = set()
        last = {}
        for i, r in enumerate(self.ins):
            if r["fn"] is None:
                continue
            if r["dma"] is not None:
                deps.add(i)
            else:
                last[r["eng"]] = i
        deps.update(last.values())
        for e in engines:
            self.ins.append(dict(eng=e, fn=None, deps=set(deps), dma=None, target=False))
        self.last_w = {}
        self.readers = {}

    def emit(self, nc, stack):
        ins = self.ins
        ses = self.same_engine_sync
        for r in ins:
            for d in r["deps"]:
                t = ins[d]
                if t["dma"] is None:
                    if t["eng"] == r["eng"] and (t["eng"] == "tensor" or not ses):
                        continue
                    t["target"] = True
        cnt = {e: 0 for e in ENGS}
        for r in ins:
            if r["dma"] is None and r["target"]:
                cnt[r["eng"]] += 1
                r["seq"] = cnt[r["eng"]]
        n_ep = {e: max(1, -(-cnt[e] // EPOCH)) for e in ENGS}
        psem = {e: [stack.enter_context(nc.semaphore(f"p_{e}_{k}")) for k in range(n_ep[e])] for e in ENGS}
        dsem = [stack.enter_context(nc.semaphore(f"dma_{k}")) for k in range(N_DMA_SEMS)]
        per_eng = {e: [] for e in ENGS}
        for i, r in enumerate(ins):
            per_eng[r["eng"]].append(i)
        block = stack.enter_context(nc.Block())

        def run_engine(ename, e):
            waited = {}
            for i in per_eng[ename]:
                r = ins[i]
                need = {}
                for d in r["deps"]:
                    t = ins[d]
                    if t["dma"] is not None:
                        s, v = t["dma"]
                        key = ("d", s)
                    else:
                        if t["eng"] == ename and (ename == "tensor" or not ses):
                            continue
                        ep, v = divmod(t["seq"] - 1, EPOCH)
                        v += 1
                        key = (t["eng"], ep)
                    if v > need.get(key, 0):
                        need[key] = v
                for key, v in need.items():
                    if waited.get(key, 0) >= v:
                        continue
                    waited[key] = v
                    sem = dsem[key[1]] if key[0] == "d" else psem[key[0]][key[1]]
                    e.wait_ge(sem, v)
                if r["fn"] is None:
                    continue
                if r["dma"] is not None and r["dma"][1] > 16:
                    key = ("d", r["dma"][0])
                    if waited.get(key, 0) < r["dma"][1] - 16:
                        waited[key] = r["dma"][1] - 16
                        e.wait_ge(dsem[r["dma"][0]], r["dma"][1] - 16)
                bi = r["fn"](e)
                if r["dma"] is not None:
                    bi.then_inc(dsem[r["dma"][0]], 16)
                elif r["target"]:
                    bi.then_inc(psem[ename][(r["seq"] - 1) // EPOCH], 1)

        block.tensor(lambda e: run_engine("tensor", e))
        block.vector(lambda e: run_engine("vector", e))
        block.scalar(lambda e: run_engine("scalar", e))
        block.gpsimd(lambda e: run_engine("gpsimd", e))
        block.sync(lambda e: run_engine("sync", e))


G_R, G_K, G_V = 0, 1024, 2048
G_WDAD = 3072
G_GD = 3200
G_POOL = 3360
G_GA = 4384
G_GB = 6432


class _Stop(Exception):
    pass


def build_program(stage=2, debug=False, stop=None, n_exp=NEXP):
    def CP(name):
        if stop == name:
            raise _Stop()
    nc = bass.Bass("TRN2", target_bir_lowering=False)
    P = Prog()
    AX = mybir.AxisListType

    def OP(eng, method, reads, writes, **kw):
        P.op(eng, lambda e: getattr(e, method)(**kw), reads=reads, writes=writes)

    def MM(out, lhsT, rhs, start, stop, reads, writes):
        P.op("tensor", lambda e: e.matmul(out, lhsT=lhsT, rhs=rhs, start=start, stop=stop), reads=reads, writes=writes)

    def TR(out, in_, identity, reads, writes):
        P.op("tensor", lambda e: e.transpose(out=out, in_=in_, identity=identity), reads=reads, writes=writes)

    def LD(eng, dst, src, writes, reads=()):
        P.dma(eng, lambda e: e.dma_start(out=dst, in_=src), reads=reads, writes=writes)

    dt_in = lambda n, s, d=F32: nc.dram_tensor(n, s, d, kind="ExternalInput").ap()
    dt_out = lambda n, s, d=F32: nc.dram_tensor(n, s, d, kind="ExternalOutput").ap()
    xo = dt_in("xo", [T_OWN, D]); xp = dt_in("xp", [T_OWN, D])
    cT = dt_in("cT", [128, 16]); flag = dt_in("flag", [128, 1]); pcorr = dt_in("pcorr", [128, 4, 16])
    cmask = dt_in("cmask", [128, 6, 128]); segm = dt_in("segm", [128, 512])
    w_ada = dt_in("w_ada", [D, 6 * D]); b_ada_b = dt_in("b_ada_b", [128, 6 * D])
    w_in = dt_in("w_in", [D, NIN]); muT = dt_in("muT", [128, 27]); vecT = dt_in("vecT", [128, 7, 8])
    w_dup = dt_in("w_dup", [64, 1024]); w_iup = dt_in("w_iup", [64, 1024]); w_gup = dt_in("w_gup", [160, 1024])
    w_ro = dt_in("w_ro", [1024, D]); w_pool = dt_in("w_pool", [4, 256, 512]); pscT = dt_in("pscT", [128, 16])
    w_out = dt_in("w_out", [D, D]); lnb = dt_in("lnb", [4, 128, D])
    if stage >= 2:
        w_router = dt_in("w_router", [D, NEXP]); b_router_b = dt_in("b_router_b", [128, NEXP])
        w_gu = dt_in("w_gu", [NEXP, D, 2 * D]); b_guT = dt_in("b_guT", [NEXP, 128, 32])
        w_down = dt_in("w_down", [NEXP, D, D]); b_down = dt_in("b_down", [NEXP, D])
        iota_cap = dt_in("iota_cap", [128, CAP])
    out = dt_out("out", [T_OWN, D])
    x1_d = nc.dram_tensor("x1_d", [T_OWN, D], F32, kind="Internal").ap()
    h2_d = nc.dram_tensor("h2_d", [T_OWN, D], BF16, kind="Internal").ap()
    modb_d = nc.dram_tensor("modb_d", [4, 128, D], F32, kind="Internal").ap()
    lg_d = nc.dram_tensor("lg_d", [T_OWN, NEXP], F32, kind="ExternalOutput" if debug else "Internal").ap()
    dbg = {}
    if debug:
        dbg["yr"] = dt_out("dbg_yr", [128, 8, T_OWN]); dbg["mixin"] = dt_out("dbg_mixin", [128, 16, T_OWN])
        dbg["modb"] = dt_out("dbg_modb", [128, 2 * D]); dbg["ps"] = dt_out("dbg_ps", [128, 27, T_OWN])
        dbg["ld"] = dt_out("dbg_ld", [128, 8, T_OWN]); dbg["dT"] = dt_out("dbg_dT", [128, 8, T_OWN])
        dbg["y"] = dt_out("dbg_y", [128, 8, T_OWN])
        dbg["x1"] = dt_out("dbg_x1", [T_OWN, D])
        if stage >= 2:
            dbg["pm"] = dt_out("dbg_pm", [T_OWN, NEXP]); dbg["pos"] = dt_out("dbg_pos", [T_OWN, NEXP])

    with ExitStack() as st:
        sb = lambda n, s, d=F32: st.enter_context(nc.sbuf_tensor(n, s, d))
        cm = sb("cm", [128, 6, 128])
        ident, m_su, m_sl, m_ui, m_bd, ones_f = (cm[:, i, :] for i in range(6))
        cmb = sb("cmb", [128, 6, 128], BF16)
        ident_b = cmb[:, 0, :]
        epsG = sb("epsG", [128, 1]); epsL = sb("epsL", [128, 1])
        OP("gpsimd", "memset", [], ["epsG"], ap=epsG[:], constant=GN_EPS)
        OP("gpsimd", "memset", [], ["epsL"], ap=epsL[:], constant=LN_EPS)
        NSL = 4
        slab = [sb(f"slab{i}", [128, 4096], BF16) for i in range(NSL)]
        slab_i = [0]
        pbank = [st.enter_context(nc.psum_tensor(f"pb{i}", [128, 512], F32)) for i in range(7)]
        ptb = st.enter_context(nc.psum_tensor("ptb", [128, 1024], BF16))
        st_p = ExitStack()
        sbp = lambda n, s_, d=F32: st_p.enter_context(nc.sbuf_tensor(n, s_, d))
        seg = sbp("seg", [128, 512]); flg = sbp("flg", [128, 1]); pcr = sbp("pcr", [128, 4, 16])
        mu = sbp("mu", [128, 27]); omu = sbp("omu", [128, 27]); vec = sbp("vec", [128, 7, 8]); psc = sbp("psc", [128, 16])
        condB = sbp("condB", [128, 16, 128], BF16); condf = sbp("condf", [128, 16])
        sc1p = sbp("sc1p", [128, 16]); sh1 = sbp("sh1", [128, 16])
        wlo = sbp("wlo", [128, 3, 1024], BF16)

        pq = []
        LOOK = 2

        def plan(specs):
            pq.extend(dict(tag=t_, src=s_, kc=k_, cols=c_, buf=None) for (t_, s_, k_, c_) in specs)

        def _issue(src_ap, kc, cols):
            i = slab_i[0] % NSL
            slab_i[0] += 1
            view = slab[i][:, 0:kc * cols].rearrange("p (k c) -> p k c", k=kc)
            LD("gpsimd", view, src_ap, [f"slab{i}"])
            return view, f"slab{i}"

        def load_slab(src_ap, kc, cols, tag=None):
            if pq:
                assert tag is not None and pq[0]["tag"] == tag, (tag, pq[0]["tag"])
                for it in pq[:1 + LOOK]:
                    if it["buf"] is None:
                        it["buf"] = _issue(it["src"], it["kc"], it["cols"])
                return pq.pop(0)["buf"]
            return _issue(src_ap, kc, cols)

        def flush_plan():
            assert not pq, [it["tag"] for it in pq]

        for dst, src, key in [(cm[:], cmask, "cm"), (seg[:], segm, "seg"), (flg[:], flag, "flg"), (pcr[:], pcorr, "pcr"), (mu[:], muT, "mu"),
                              (vec[:], vecT, "vec"), (psc[:], pscT, "psc"), (condf[:], cT, "condf")]:
            LD("sync", dst, src, [key])
        LD("gpsimd", wlo[0:64, 0, :], w_dup, ["wlo"]); LD("gpsimd", wlo[64:128, 0, :], w_iup, ["wlo"])
        LD("gpsimd", wlo[:, 1, :], w_gup[0:128, :], ["wlo"]); LD("gpsimd", wlo[0:32, 2, :], w_gup[128:160, :], ["wlo"])
        OP("vector", "tensor_copy", ["cm"], ["cmb"], out=cmb[:], in_=cm[:])
        OP("vector", "tensor_scalar", ["mu"], ["omu"], out=omu[:], in0=mu[:], scalar1=-1.0, scalar2=1.0, op0=ALU.mult, op1=ALU.add)
        OP("scalar", "activation", ["condf"], ["condf"], out=condf[:], in_=condf[:], func=AF.Silu)
        for k in range(16):
            OP("vector", "tensor_copy", ["condf"], ["condB"], out=condB[:, k, :], in_=condf[:, k:k + 1].to_broadcast([128, 128]))

        with ExitStack() as st_a:
            badd = st_a.enter_context(nc.sbuf_tensor("badd", [128, 2, 256], F32))
            mod01 = st_a.enter_context(nc.sbuf_tensor("mod01", [128, 2 * D], F32))
            mtmp = st_a.enter_context(nc.sbuf_tensor("mtmp", [128, 16, 128], F32))

            def ada_slab(si, dst_ap, dst_key):
                c0 = si * 256
                view, key = load_slab(w_ada[:, c0:c0 + 256].rearrange("(k p) c -> p k c", p=128), 16, 256, tag=("ada", si))
                bi = si % 2
                LD("sync", badd[:, bi, :], b_ada_b[:, c0:c0 + 256], [f"badd{bi}"])
                pbk = pbank[si % 2]
                for k in range(16):
                    MM(pbk[:, 0:256], condB[:, k, :], view[:, k, :], k == 0, k == 15, [key, "condB"], [f"pb{si % 2}"])
                OP("vector", "tensor_tensor", [f"pb{si % 2}", f"badd{bi}"], [dst_key], out=dst_ap, in0=pbk[:, 0:256], in1=badd[:, bi, :], op=ALU.add)

            plan([(("ada", si_), w_ada[:, si_ * 256:si_ * 256 + 256].rearrange("(k p) c -> p k c", p=128), 16, 256) for si_ in range(48)])
            for si in range(16):
                ada_slab(si, mod01[:, si * 256:(si + 1) * 256], f"mt{si // 8}")
            for j, (dst, dkey) in enumerate([(sh1, "sh1"), (sc1p, "sc1p")]):
                OP("vector", "tensor_tensor", [f"mt{j}", "cm"], ["mtmp"], out=mtmp[:], in0=mod01[:, j * D:(j + 1) * D].rearrange("p (c q) -> p c q", q=128),
                   in1=cm[:, 0:1, :].to_broadcast([128, 16, 128]), op=ALU.mult)
                OP("vector", "tensor_reduce", ["mtmp"], [dkey], out=dst[:], in_=mtmp[:], axis=AX.X, op=ALU.add)
            OP("vector", "tensor_scalar_add", ["sc1p"], ["sc1p"], out=sc1p[:], in0=sc1p[:], scalar1=1.0)
            if debug:
                LD("sync", dbg["modb"], mod01[:], [], ["mt0", "mt1"])
            order = {2: 0, 3: 1, 4: 2, 5: 3}
            for piece in range(2, 6):
                mt = mod01[:, (piece % 2) * D:(piece % 2 + 1) * D]
                mk = f"mt{piece % 2}"
                for s8 in range(8):
                    ada_slab(piece * 8 + s8, mt[:, s8 * 256:(s8 + 1) * 256], mk)
                if piece == 4:
                    OP("vector", "tensor_scalar_add", [mk], [mk], out=mt, in0=mt, scalar1=1.0)
                LD("sync", modb_d[order[piece]], mt, [], [mk])
            P.barrier()
        if stage == 0:
            st_p.close()
            P.emit(nc, st)
            return nc

        with ExitStack() as st_m:
            sbm = lambda n, s, d=F32: st_m.enter_context(nc.sbuf_tensor(n, s, d))
            hT = sbm("hT", [128, 16, 512], BF16)
            carry = sbm("carry", [128, 27])
            halo = sbm("halo", [128, 8, 16])
            dT = sbm("dT", [128, 8, 512], BF16); yrT = sbm("yrT", [128, 8, 512], BF16); mixT = sbm("mixT", [128, 16, 512], BF16)
            Hst = sbm("Hst", [128, 8, 128])
            import os
            if not os.environ.get("NO_MEMSET"):
                OP("vector", "memset", [], ["H%d" % i for i in range(8)], ap=Hst[:].rearrange("p a b -> p (a b)"), constant=0.0)
                OP("vector", "memset", [], ["carry"], ap=carry[:], constant=0.0)
                OP("vector", "memset", [], ["halo"], ap=halo[:].rearrange("p a b -> p (a b)"), constant=0.0)
            pslot_i = [0]

            def pslot():
                i = pslot_i[0] % 4
                pslot_i[0] += 1
                return pbank[2 + i][:, 0:128], f"pb{2 + i}"

            def in_proj(col0, ncols, ntok0, ntok, pbk, pkey):
                view, key = load_slab(w_in[:, col0:col0 + ncols].rearrange("(k p) c -> p k c", p=128), 16, ncols, tag=("win", col0, ncols))
                for k in range(16):
                    MM(pbk[0:ncols, 0:ntok], view[:, k, :], hT[:, k, ntok0:ntok0 + ntok], k == 0, k == 15, [key, "hT"], [pkey])

            scopes = []
            try:
                for blk in range(4):
                    own = blk >= 2
                    xsrc = xo if own else xp
                    t0 = (blk % 2) * 512
                    tcol = (blk - 2) * 512
                    st1 = ExitStack(); scopes.append(st1)
                    sb1 = lambda n, s_, d=F32: st1.enter_context(nc.sbuf_tensor(f"{n}_b{blk}", s_, d))
                    xs = [sb1(f"xs{i}", [128, D]) for i in range(1)]
                    pr = sb1("pr", [128, 8, 512], BF16); pk = sb1("pk", [128, 8, 512], BF16); pv = sb1("pv", [128, 8, 512], BF16)
                    plo = sb1("plo", [128, 3, 512])
                    upool = sb1("upool", [128, 528])
                    ptmp = [sb1(f"ptmp{i}", [128, 528]) for i in range(2)]
                    tnh = sb1("tnh", [128, 512], BF16); sgd = sb1("sgd", [128, 2, 512], BF16)
                    f_ld = sb1("f_ld", [128, 512]); f_ic = sb1("f_ic", [128, 512]); f_L = sb1("f_L", [128, 512])
                    f_t0 = sb1("f_t0", [128, 512]); f_t1 = sb1("f_t1", [128, 512]); f_t2 = sb1("f_t2", [128, 512])
                    f_kk = sb1("f_kk", [128, 512]); f_km = sb1("f_km", [128, 512]); f_bb = sb1("f_bb", [128, 512])
                    ynT = sb1("ynT", [128, 512])
                    LC = sb1("LC", [128, 4]); WC = sb1("WC", [128, 4])
                    o_r = sb1("o_r", [128, 512], BF16); o_a = sb1("o_a", [128, 512], BF16); o_b = sb1("o_b", [128, 512], BF16)
                    o_k = sb1("o_k", [128, 512], BF16); o_bh = sb1("o_bh", [128, 512], BF16); o_kh = sb1("o_kh", [128, 512], BF16)
                    rF = sb1("rF", [128, 512])
                    tm = sb1("tm", [128, 4, 4, 128], BF16)
                    bonus = sb1("bonus", [128, 512]); gT = sb1("gT", [128, 512])
                    uM = [sb1(f"uM{i}", [128, 2, 128]) for i in range(8)]
                    uN = [sb1(f"uN{i}", [128, 128]) for i in range(8)]
                    uNb = [sb1(f"uNb{i}", [128, 128], BF16) for i in range(4)]
                    uA = [sb1(f"uA{i}", [128, 3, 128], BF16) for i in range(4)]
                    uAt = [sb1(f"uAt{i}", [128, 128], BF16) for i in range(2)]; uX = [sb1(f"uX{i}", [128, 128], BF16) for i in range(2)]
                    uU = [sb1(f"uU{i}", [128, 128], BF16) for i in range(2)]
                    uPT = sb1("uPT", [128, 128]); uQ = sb1("uQ", [128, 128]); uRh = sb1("uRh", [128, 128])
                    uY = sb1("uY", [128, 128]); uZ = sb1("uZ", [128, 128]); ust = sb1("ust", [128, 2, 6]); uag = sb1("uag", [128, 2, 2])
                    ga, gb, t1, t2 = f_ld, f_ic, f_kk, f_km
                    glist = []
                    for j in range(8):
                        glist.append((j, G_R + 128 * j, 128, pr, j, False))
                    for j in range(8):
                        glist.append((8 + j, G_K + 128 * j, 128, pk, j, True))
                    for j in range(8):
                        glist.append((16 + j, G_V + 128 * j, 128, pv, j, True))
                    glist += [(24, G_WDAD, 128, plo, 0, True), (25, G_GD, 128, plo, 1, False), (26, G_GD + 128, 32, plo, 2, False)]

                    CP(f'pre{blk}')
                    wsl = lambda c0_, n_: w_in[:, c0_:c0_ + n_].rearrange("(k p) c -> p k c", p=128)
                    bp = []
                    for (gi_, col0_, ncols_, dst_, di_, fullp_) in glist:
                        if (not own) and (not fullp_) and blk != 1:
                            continue
                        bp.append((("win", col0_, ncols_), wsl(col0_, ncols_), 16, ncols_))
                    if (not own) and blk == 1:
                        bp += [(("win", G_POOL + 128 * j_, 128), wsl(G_POOL + 128 * j_, 128), 16, 128) for j_ in range(8)]
                    if own:
                        bp += [(("win", G_POOL + 128 * j_, 128), wsl(G_POOL + 128 * j_, 128), 16, 128) for j_ in range(8)]
                        for c_ in range(16):
                            bp.append((("win", G_GA + 128 * c_, 128), wsl(G_GA + 128 * c_, 128), 16, 128))
                            bp.append((("win", G_GB + 128 * c_, 128), wsl(G_GB + 128 * c_, 128), 16, 128))
                            bp.append((("wro", c_), w_ro[:, c_ * 128:(c_ + 1) * 128].rearrange("(k p) c -> p k c", p=128), 8, 128))
                            bp.append((("wpool", c_), w_pool[c_ // 4, :, (c_ % 4) * 128:(c_ % 4 + 1) * 128].rearrange("(k p) c -> p k c", p=128), 2, 128))
                    plan(bp)
                    for tt in range(4):
                        LD("sync", xs[0][:], xsrc[t0 + tt * 128:t0 + (tt + 1) * 128, :], ["xs0"])
                        for c in range(16):
                            TR(pbank[6][:, (c % 4) * 128:(c % 4 + 1) * 128], xs[0][:, c * 128:(c + 1) * 128], ident, ["xs0", "cm"], ["pb6"])
                            if c % 4 == 3:
                                for u_ in range(4):
                                    cc_ = c - 3 + u_
                                    OP("scalar", "activation", ["pb6", "sc1p", "sh1"], ["hT"], out=hT[:, cc_, tt * 128:(tt + 1) * 128], in_=pbank[6][:, u_ * 128:(u_ + 1) * 128],
                                       func=AF.Identity, scale=sc1p[:, cc_:cc_ + 1], bias=sh1[:, cc_:cc_ + 1])
                    CP(f'i{blk}')
                    for (gi, col0, ncols, dst, di, fullp) in glist:
                        pbk, pkey = pbank[gi % 2], f"pb{gi % 2}"
                        if not own and not fullp:
                            if blk == 1:
                                in_proj(col0, ncols, 496, 16, pbk, pkey)
                                OP("vector", "tensor_copy", [pkey], ["carry"], out=carry[0:ncols, gi:gi + 1], in_=pbk[0:ncols, 15:16])
                            continue
                        in_proj(col0, ncols, 0, 512, pbk, pkey)
                        dsl = dst[0:ncols, di, :]
                        OP("scalar", "activation", [pkey, "omu"], ["f_t0"], out=f_t0[0:ncols, :], in_=pbk[0:ncols, :], func=AF.Identity, scale=omu[0:ncols, gi:gi + 1])
                        OP("vector", "scalar_tensor_tensor", [pkey, "mu", "f_t0"], [f"p{gi}"], out=dsl[:, 1:512], in0=pbk[0:ncols, 0:511], scalar=mu[0:ncols, gi:gi + 1],
                           in1=f_t0[0:ncols, 1:512], op0=ALU.mult, op1=ALU.add)
                        OP("vector", "scalar_tensor_tensor", ["carry", "mu", "f_t0"], [f"p{gi}"], out=dsl[:, 0:1], in0=carry[0:ncols, gi:gi + 1], scalar=mu[0:ncols, gi:gi + 1],
                           in1=f_t0[0:ncols, 0:1], op0=ALU.mult, op1=ALU.add)
                        OP("vector", "tensor_copy", [pkey, "carry"], ["carry"], out=carry[0:ncols, gi:gi + 1], in_=pbk[0:ncols, 511:512])
                        if debug and own and dst is plo:
                            LD("sync", dbg["ps"][0:ncols, gi, tcol:tcol + 512], dsl, [], [f"p{gi}"])
                    if blk == 1:
                        OP("vector", "tensor_scalar_mul", ["carry", "flg"], ["carry"], out=carry[:], in0=carry[:], scalar1=flg[:, 0:1])
                    CP(f'ii{blk}')
                    OP("scalar", "activation", ["p24"], ["tnh"], out=tnh[0:64, :], in_=plo[0:64, 0, :], func=AF.Tanh)
                    OP("vector", "tensor_copy", ["p24"], ["tnh"], out=tnh[64:128, :], in_=plo[64:128, 0, :])
                    if own:
                        OP("scalar", "activation", ["p25"], ["sgd"], out=sgd[:, 0, :], in_=plo[:, 1, :], func=AF.Sigmoid)
                        OP("scalar", "activation", ["p26"], ["sgd"], out=sgd[0:32, 1, :], in_=plo[0:32, 2, :], func=AF.Sigmoid)
                    for hp in range(8):
                        cs = slice(hp * 128, (hp + 1) * 128)
                        V = lambda j: vec[:, j, hp:hp + 1]
                        pb6 = pbank[6]
                        kkey, vkey, rkey = f"p{8 + hp}", f"p{16 + hp}", f"p{hp}"
                        MM(pb6[:, :], wlo[0:64, 0, cs], tnh[0:64, :], True, True, ["wlo", "tnh"], ["pb6"])
                        OP("scalar", "activation", ["pb6", "vec"], ["f_ld"], out=f_ld[:], in_=pb6[:, :], func=AF.Sigmoid, bias=V(0))
                        MM(pb6[:, :], wlo[64:128, 0, cs], tnh[64:128, :], True, True, ["wlo", "tnh"], ["pb6"])
                        OP("scalar", "activation", ["pb6", "vec"], ["f_ic"], out=f_ic[:], in_=pb6[:, :], func=AF.Sigmoid, bias=V(1))
                        OP("vector", "tensor_scalar_mul", ["f_ld"], ["f_ld"], out=f_ld[:], in0=f_ld[:], scalar1=-0.6065306597126334)
                        OP("vector", "tensor_tensor_scan", ["seg", "f_ld"], ["f_L"], out=f_L[:], data0=seg[:], data1=f_ld[:], initial=0.0, op0=ALU.mult, op1=ALU.add)
                        OP("vector", "tensor_copy", ["f_L"], ["LC"], out=LC[:], in_=f_L[:].rearrange("p (c t) -> p c t", t=128)[:, :, 127])
                        OP("scalar", "activation", ["LC"], ["WC"], out=WC[:], in_=LC[:], func=AF.Exp)
                        OP("vector", "tensor_scalar_mul", [kkey, "vec"], ["f_kk"], out=f_kk[:], in0=pk[:, hp, :], scalar1=V(2))
                        OP("gpsimd", "tensor_tensor", ["f_kk"], ["f_t0"], out=f_t0[:], in0=f_kk[:], in1=f_kk[:], op=ALU.mult)
                        MM(pb6[:, :], m_bd, f_t0[:], True, True, ["cm", "f_t0"], ["pb6"])
                        OP("scalar", "activation", ["pb6"], ["f_t1"], out=f_t1[:], in_=pb6[:, :], func=AF.Sqrt)
                        OP("vector", "tensor_scalar_max", ["f_t1"], ["f_t1"], out=f_t1[:], in0=f_t1[:], scalar1=1e-12)
                        OP("vector", "reciprocal", ["f_t1"], ["f_t1"], out=f_t1[:], in_=f_t1[:])
                        OP("vector", "tensor_tensor", ["f_kk", "f_t1"], ["f_kk"], out=f_kk[:], in0=f_kk[:], in1=f_t1[:], op=ALU.mult)
                        OP("vector", "tensor_scalar", ["f_ic", "vec"], ["f_t2"], out=f_t2[:], in0=f_ic[:], scalar1=-1.0, scalar2=V(3), op0=ALU.add, op1=ALU.mult)
                        OP("vector", "scalar_tensor_tensor", ["f_t2", kkey], ["f_km"], out=f_km[:], in0=f_t2[:], scalar=1.0, in1=pk[:, hp, :], op0=ALU.add, op1=ALU.mult)
                        OP("gpsimd", "tensor_tensor", ["f_kk", "f_ic"], ["f_bb"], out=f_bb[:], in0=f_kk[:], in1=f_ic[:], op=ALU.mult)
                        OP("scalar", "activation", ["f_L"], ["f_t0"], out=f_t0[:], in_=f_L[:], func=AF.Exp)
                        if own:
                            OP("vector", "tensor_tensor", [rkey, "f_t0"], ["rF"], out=rF[:], in0=pr[:, hp, :], in1=f_t0[:], op=ALU.mult)
                            OP("gpsimd", "tensor_copy", ["rF"], ["o_r"], out=o_r[:], in_=rF[:])
                        OP("vector", "tensor_tensor", ["f_L", "f_ld"], ["f_t1"], out=f_t1[:], in0=f_L[:], in1=f_ld[:], op=ALU.subtract)
                        OP("scalar", "activation", ["f_t1"], ["f_t1"], out=f_t1[:], in_=f_t1[:], func=AF.Exp)
                        OP("vector", "scalar_tensor_tensor", ["f_kk", "f_t1"], ["o_a"], out=o_a[:], in0=f_kk[:], scalar=-1.0, in1=f_t1[:], op0=ALU.mult, op1=ALU.mult)
                        OP("scalar", "activation", ["f_L"], ["f_t2"], out=f_t2[:], in_=f_L[:], func=AF.Exp, scale=-1.0)
                        OP("vector", "tensor_tensor", ["f_bb", "f_t2"], ["o_b"], out=o_b[:], in0=f_bb[:], in1=f_t2[:], op=ALU.mult)
                        OP("gpsimd", "tensor_tensor", ["f_km", "f_t2"], ["o_k"], out=o_k[:], in0=f_km[:], in1=f_t2[:], op=ALU.mult)
                        for c in range(4):
                            OP("scalar", "activation", ["f_L", "LC"], ["f_t0"], out=f_t0[:, c * 128:(c + 1) * 128], in_=f_L[:, c * 128:(c + 1) * 128], func=AF.Exp, scale=-1.0, bias=LC[:, c:c + 1])
                        OP("vector", "tensor_tensor", ["f_bb", "f_t0"], ["o_bh"], out=o_bh[:], in0=f_bb[:], in1=f_t0[:], op=ALU.mult)
                        OP("gpsimd", "tensor_tensor", ["f_km", "f_t0"], ["o_kh"], out=o_kh[:], in0=f_km[:], in1=f_t0[:], op=ALU.mult)
                        CP(f'prep{blk}_{hp}')
                        if debug and own:
                            LD("sync", dbg["ld"][:, hp, tcol:tcol + 512], f_ld[:], [], ["f_ld"])
                        for c in range(4):
                            tc_ = slice(c * 128, (c + 1) * 128)
                            for wi, (src, skey) in enumerate([(o_a[:, tc_], "o_a"), (o_bh[:, tc_], "o_bh"), (o_kh[:, tc_], "o_kh"), (pv[:, hp, tc_], vkey)]):
                                TR(ptb[:, wi * 128:(wi + 1) * 128], src, ident_b, [skey, "cmb"], ["ptb"])
                            if c % 2:
                                OP("vector", "tensor_copy", ["ptb"], [f"tm{c}"], out=tm[:, c, :, :], in_=ptb[:, 0:512].rearrange("p (w q) -> p w q", q=128))
                            else:
                                OP("scalar", "activation", ["ptb"], [f"tm{c}"], out=tm[:, c, :, :], in_=ptb[:, 0:512].rearrange("p (w q) -> p w q", q=128), func=AF.Identity)
                        CP(f'tm{blk}_{hp}')
                        if own:
                            OP("vector", "scalar_tensor_tensor", [rkey, "vec", "f_km"], ["f_t1"], out=f_t1[:], in0=pr[:, hp, :], scalar=V(4), in1=f_km[:], op0=ALU.mult, op1=ALU.mult)
                            MM(pb6[:, :], m_bd, f_t1[:], True, True, ["cm", "f_t1"], ["pb6"])
                            OP("vector", "tensor_tensor", ["pb6", vkey], ["bonus"], out=bonus[:], in0=pb6[:, :], in1=pv[:, hp, :], op=ALU.mult)
                            MM(pb6[:, :], wlo[:, 1, cs], sgd[:, 0, :], True, False, ["wlo", "sgd"], ["pb6"])
                            MM(pb6[:, :], wlo[0:32, 2, cs], sgd[0:32, 1, :], False, True, ["wlo", "sgd"], ["pb6"])
                            OP("scalar", "activation", ["pb6"], ["gT"], out=gT[:], in_=pb6[:, :], func=AF.Identity)

                        def head_chain(h, c, q, cs):
                            tc_ = slice(c * 128, (c + 1) * 128)
                            tmk = f"tm{c}"
                            pbs = slice(h * 64, (h + 1) * 64)
                            bt, at, kt, rt = o_b[pbs, tc_], o_a[pbs, tc_], o_k[pbs, tc_], o_r[pbs, tc_]
                            M0, N0 = uM[2 * q], uN[2 * q]
                            mk0 = f"uM{2 * q}"
                            uAq, uAk = uA[q], f"uA{q}"
                            specs = [(bt, at, m_su, M0[:, 0, :], mk0, ["o_b", "o_a"]), (at, bt, m_sl, M0[:, 1, :], mk0, ["o_b", "o_a"]),
                                     (kt, at, m_su, uAq[:, 2, :], uAk, ["o_k", "o_a"])]
                            if own:
                                specs += [(bt, rt, m_ui, uAq[:, 0, :], uAk, ["o_b", "o_r"]), (kt, rt, m_ui, uAq[:, 1, :], uAk, ["o_k", "o_r"])]
                            for (l_, r_, msk, dst, dkey, rk) in specs:
                                ps_, pk_ = pslot()
                                MM(ps_, l_, r_, True, True, rk, [pk_])
                                OP("vector", "tensor_tensor", [pk_, "cm"], [dkey], out=dst, in0=ps_, in1=msk, op=ALU.mult)
                                yield
                            OP("gpsimd", "tensor_tensor", [mk0, "cm"], [f"uN{2 * q}"], out=N0[:], in0=M0[:, 0, :], in1=ident, op=ALU.add)
                            cur, curk, Nc, Nk = M0, mk0, N0, f"uN{2 * q}"
                            for j in range(1, 7):
                                nxt, nxtk = uM[2 * q + (j % 2)], f"uM{2 * q + (j % 2)}"
                                Nn, Nnk = uN[2 * q + (j % 2)], f"uN{2 * q + (j % 2)}"
                                rk = [curk, curk + "a", curk + "b"]
                                if j < 6:
                                    ps_, pk_ = pslot()
                                    MM(ps_, cur[:, 1, :], cur[:, 0, :], True, True, rk, [pk_])
                                    OP("vector", "tensor_copy", [pk_], [nxtk + "a"], out=nxt[:, 0, :], in_=ps_)
                                ps2, pk2 = pslot()
                                MM(ps2, cur[:, 0, :], cur[:, 1, :], True, True, rk, [pk2])
                                OP("scalar", "activation", [pk2], [nxtk + "b"], out=nxt[:, 1, :], in_=ps2, func=AF.Identity)
                                yield
                                ps3, pk3 = pslot()
                                MM(ps3, nxt[:, 1, :], Nc[:], True, True, [nxtk + "b", Nk], [pk3])
                                OP("vector", "tensor_tensor", [pk3, Nk], [Nnk], out=Nn[:], in0=ps3, in1=Nc[:], op=ALU.add)
                                cur, curk, Nc, Nk = nxt, nxtk, Nn, Nnk
                                yield
                            OP("gpsimd", "tensor_copy", [Nk], [f"uNb{q}"], out=uNb[q][:], in_=Nc[:])
                            ps_, pk_ = pslot()
                            MM(ps_[:, 0:64], uNb[q][:], tm[:, c, 0, pbs], True, True, [f"uNb{q}", tmk], [pk_])
                            OP("vector", "tensor_copy", [pk_], [f"uAt{cs}_{h}"], out=uAt[cs][:, pbs], in_=ps_[:, 0:64])
                            ps_, pk_ = pslot()
                            MM(ps_[:, 0:64], uAq[:, 2, :], tm[:, c, 3, pbs], True, True, [uAk, tmk], [pk_])
                            OP("scalar", "activation", [pk_], [f"uX{cs}_{h}"], out=uX[cs][:, pbs], in_=ps_[:, 0:64], func=AF.Identity)
                            yield
                            ps_, pk_ = pslot()
                            MM(ps_[:, 0:64], uNb[q][:], uX[cs][:, pbs], True, True, [f"uNb{q}", f"uX{cs}_{h}"], [pk_])
                            OP("vector", "tensor_copy", [pk_], [f"uU{cs}_{h}"], out=uU[cs][:, pbs], in_=ps_[:, 0:64])
                            yield

                        def pair_level(c, cs):
                            tc_ = slice(c * 128, (c + 1) * 128)
                            tmk = f"tm{c}"
                            atk = [f"uAt{cs}_0", f"uAt{cs}_1"]
                            uuk = [f"uU{cs}_0", f"uU{cs}_1"]
                            ps_, pk_ = pslot()
                            MM(ps_, uAt[cs][:], tm[:, c, 1, :], True, True, atk + [tmk], [pk_])
                            OP("vector", "tensor_tensor", [pk_, "cm"], ["uPT"], out=uPT[:], in0=ps_, in1=m_bd, op=ALU.mult)
                            OP("vector", "scalar_tensor_tensor", ["uPT", "cm", "WC"], ["uPT"], out=uPT[:], in0=ident, scalar=WC[:, c:c + 1], in1=uPT[:], op0=ALU.mult, op1=ALU.add)
                            ps_, pk_ = pslot()
                            MM(ps_, tm[:, c, 1, :], uU[cs][:], True, False, uuk + [tmk], [pk_])
                            MM(ps_, tm[:, c, 2, :], tm[:, c, 3, :], False, True, [tmk], [pk_])
                            OP("vector", "tensor_tensor", [pk_, "cm"], ["uQ"], out=uQ[:], in0=ps_, in1=m_bd, op=ALU.mult)
                            if own:
                                for h in range(2):
                                    pbs = slice(h * 64, (h + 1) * 64)
                                    q = 2 * cs + h
                                    ps_, pk_ = pslot()
                                    MM(ps_, uAt[cs][:], uA[q][:, 0, :], True, True, atk + [f"uA{q}"], [pk_])
                                    OP("vector", "tensor_tensor", [pk_, "rF"], [f"uRh{h}"], out=uRh[pbs, :], in0=ps_[pbs, :], in1=rF[pbs, tc_], op=ALU.add)
                                psy, pky = pslot()
                                MM(psy, uRh[:], Hst[:, hp, :], True, True, ["uRh0", "uRh1", f"H{hp}"], [pky])
                                ps0, pk0 = pslot()
                                for h in range(2):
                                    pbs = slice(h * 64, (h + 1) * 64)
                                    q = 2 * cs + h
                                    MM(ps0[:, pbs], uA[q][:, 0, :], uU[cs][:, pbs], True, False, [f"uA{q}", f"uU{cs}_{h}"], [pk0])
                                    MM(ps0[:, pbs], uA[q][:, 1, :], tm[:, c, 3, pbs], False, True, [f"uA{q}", tmk], [pk0])
                                OP("scalar", "activation", [pk0], ["uY"], out=uY[:], in_=ps0, func=AF.Identity)
                                OP("vector", "tensor_tensor", [pky, "uY"], ["uY"], out=uY[:], in0=psy, in1=uY[:], op=ALU.add)
                                for h in range(2):
                                    pbs = slice(h * 64, (h + 1) * 64)
                                    OP("vector", "bn_stats", ["uY"], [f"ust{h}"], out=ust[:, h, :], in_=uY[:, pbs])
                                    OP("vector", "bn_aggr", [f"ust{h}"], [f"uag{h}"], out=uag[:, h, :], in_=ust[:, h, :])
                                    OP("scalar", "activation", [f"uag{h}"], [f"uag{h}"], out=uag[:, h, 1:2], in_=uag[:, h, 1:2], func=AF.Sqrt, bias=epsG[:, 0:1])
                                    OP("vector", "reciprocal", [f"uag{h}"], [f"uag{h}"], out=uag[:, h, 1:2], in_=uag[:, h, 1:2])
                                    OP("vector", "tensor_scalar", ["uY", f"uag{h}"], [f"uZ{h}"], out=uZ[:, pbs], in0=uY[:, pbs], scalar1=uag[:, h, 0:1], scalar2=uag[:, h, 1:2],
                                       op0=ALU.subtract, op1=ALU.mult)
                                psz, pkz = pslot()
                                TR(psz, uZ[:], ident, ["uZ0", "uZ1", "cm"], [pkz])
                                OP("scalar", "activation", [pkz, "vec"], ["ynT"], out=ynT[:, tc_], in_=psz, func=AF.Identity, scale=vec[:, 5, hp:hp + 1], bias=vec[:, 6, hp:hp + 1])
                            psh, pkh = pslot()
                            MM(psh, uPT[:], Hst[:, hp, :], True, True, ["uPT", f"H{hp}"], [pkh])
                            OP("vector", "tensor_tensor", [pkh, "uQ"], [f"H{hp}"], out=Hst[:, hp, :], in0=psh, in1=uQ[:], op=ALU.add)
                            if blk == 1 and c == 3:
                                OP("vector", "tensor_scalar_mul", [f"H{hp}", "flg"], [f"H{hp}"], out=Hst[:, hp, :], in0=Hst[:, hp, :], scalar1=flg[:, 0:1])

                        for cp_ in (0, 2):
                            chains = [head_chain(h, cp_ + cs, 2 * cs + h, cs) for cs in range(2) for h in range(2)]
                            live = list(chains)
                            while live:
                                for g_ in list(live):
                                    try:
                                        next(g_)
                                    except StopIteration:
                                        live.remove(g_)
                            pair_level(cp_, 0)
                            pair_level(cp_ + 1, 1)

                        if own:
                            if debug:
                                LD("sync", dbg["y"][:, hp, tcol:tcol + 512], ynT[:], [], ["ynT"])
                            OP("vector", "tensor_tensor", ["ynT", "bonus"], ["ynT"], out=ynT[:], in0=ynT[:], in1=bonus[:], op=ALU.add)
                            OP("vector", "tensor_tensor", ["ynT", "gT"], ["yrT"], out=yrT[:, hp, :], in0=ynT[:], in1=gT[:], op=ALU.mult)
                            if debug:
                                OP("vector", "tensor_tensor", ["ynT", "gT"], ["ynT"], out=ynT[:], in0=ynT[:], in1=gT[:], op=ALU.mult)
                                LD("sync", dbg["yr"][:, hp, tcol:tcol + 512], ynT[:], [], ["ynT"])
                    if not own:
                        if blk == 1:
                            for j in range(8):
                                pbk, pkey = pbank[j % 2], f"pb{j % 2}"
                                in_proj(G_POOL + 128 * j, 128, 496, 16, pbk, pkey)
                                OP("vector", "tensor_scalar_mul", [pkey, "flg"], ["halo"], out=halo[:, j, :], in0=pbk[:, 0:16], scalar1=flg[:, 0:1])
                        P.barrier()
                        st1.close()
                        continue
                    CP(f'rwkv{blk}')
                    for j in range(8):
                        g = j // 2
                        W = (2, 4, 8, 16)[g]
                        pbk, pkey = pbank[j % 2], f"pb{j % 2}"
                        in_proj(G_POOL + 128 * j, 128, 0, 512, pbk, pkey)
                        OP("scalar", "activation", [pkey], ["upool"], out=upool[:, 16:528], in_=pbk[:, :], func=AF.Identity)
                        OP("gpsimd", "tensor_copy", ["halo"], ["upool"], out=upool[:, 0:16], in_=halo[:, j, :])
                        src, skey = upool[:, :], "upool"
                        step, pi = 1, 0
                        while step < W:
                            dstt = ptmp[pi]
                            OP("gpsimd", "tensor_tensor", [skey], [f"ptmp{pi}"], out=dstt[:, step:528], in0=src[:, step:528], in1=src[:, 0:528 - step], op=ALU.add)
                            OP("gpsimd", "tensor_copy", [skey], [f"ptmp{pi}"], out=dstt[:, 0:step], in_=src[:, 0:step])
                            src, skey = dstt[:, :], f"ptmp{pi}"
                            pi ^= 1
                            step *= 2
                        OP("vector", "scalar_tensor_tensor", [skey, "upool"], ["dT"], out=dT[:, j, :], in0=src[:, 16:528], scalar=1.0 / W, in1=upool[:, 16:528], op0=ALU.mult, op1=ALU.subtract)
                        if blk == 2:
                            OP("vector", "tensor_tensor", [skey, "pcr"], ["f_kk"], out=t1[:, 0:16], in0=src[:, 16:32], in1=pcr[:, g, :], op=ALU.mult)
                            OP("vector", "tensor_tensor", ["f_kk", "upool"], ["dT"], out=dT[:, j, 0:16], in0=t1[:, 0:16], in1=upool[:, 16:32], op=ALU.subtract)
                        OP("gpsimd", "tensor_copy", ["upool"], ["halo"], out=halo[:, j, :], in_=upool[:, 512:528])
                    CP(f'pool{blk}')
                    for c in range(16):
                        in_proj(G_GA + 128 * c, 128, 0, 512, pbank[0], "pb0")
                        OP("scalar", "activation", ["pb0"], ["f_ld"], out=ga[:], in_=pbank[0][:, :], func=AF.Sigmoid)
                        in_proj(G_GB + 128 * c, 128, 0, 512, pbank[1], "pb1")
                        OP("scalar", "activation", ["pb1"], ["f_ic"], out=gb[:], in_=pbank[1][:, :], func=AF.Sigmoid)
                        vro, kro = load_slab(w_ro[:, c * 128:(c + 1) * 128].rearrange("(k p) c -> p k c", p=128), 8, 128, tag=("wro", c))
                        for k in range(8):
                            MM(pbank[0][:, :], vro[:, k, :], yrT[:, k, :], k == 0, k == 7, [kro, "yrT"], ["pb0"])
                        g = c // 4
                        vpo, kpo = load_slab(w_pool[g, :, (c % 4) * 128:(c % 4 + 1) * 128].rearrange("(k p) c -> p k c", p=128), 2, 128, tag=("wpool", c))
                        for k in range(2):
                            MM(pbank[1][:, :], vpo[:, k, :], dT[:, 2 * g + k, :], k == 0, k == 1, [kpo, "dT"], ["pb1"])
                        OP("vector", "tensor_tensor", ["pb0", "f_ld"], ["f_kk"], out=t1[:], in0=pbank[0][:, :], in1=ga[:], op=ALU.mult)
                        OP("vector", "scalar_tensor_tensor", ["pb1", "psc", "f_ic"], ["f_km"], out=t2[:], in0=pbank[1][:, :], scalar=psc[:, c:c + 1], in1=gb[:], op0=ALU.mult, op1=ALU.mult)
                        OP("gpsimd", "tensor_tensor", ["f_kk", "f_km"], ["mixT"], out=mixT[:, c, :], in0=t1[:], in1=t2[:], op=ALU.add)
                        if debug:
                            OP("gpsimd", "tensor_tensor", ["f_kk", "f_km"], ["f_ld"], out=ga[:], in0=t1[:], in1=t2[:], op=ALU.add)
                            LD("sync", dbg["mixin"][:, c, tcol:tcol + 512], ga[:], [], ["f_ld"])
                    CP(f'comb{blk}')
                    P.barrier()
                    st1.close()
                    st2 = ExitStack(); scopes.append(st2)
                    sb2 = lambda n, s_, d=F32: st2.enter_context(nc.sbuf_tensor(f"{n}_c{blk}", s_, d))
                    xs = [sb2(f"xs{i}", [128, D]) for i in range(2)]
                    x1p = sb2("x1p", [128, D]); lnst = sb2("lnst", [128, 4, 6]); lnag = sb2("lnag", [128, 2]); h2t = sb2("h2t", [128, D], BF16)
                    lnw = sb2("lnw", [128, 2, D]); modb = sb2("modb", [128, 3, D])
                    LD("sync", lnw[:, 0, :], lnb[0], ["lnw"]); LD("sync", lnw[:, 1, :], lnb[1], ["lnw"])
                    for k_ in range(3):
                        LD("sync", modb[:, k_, :], modb_d[k_], ["modb"])
                    if stage >= 2:
                        h2T = sb2("h2T", [128, 16, 128]); wr = sb2("wr", [128, 16, NEXP]); brb = sb2("brb", [128, NEXP]); lgt = sb2("lgt", [128, NEXP])
                        LD("sync", wr[:], w_router.rearrange("(k p) e -> p k e", p=128), ["wr"])
                        LD("sync", brb[:], b_router_b, ["brb"])
                    plan([(("wout", tt_, s8_), w_out[:, s8_ * 256:(s8_ + 1) * 256].rearrange("(k p) c -> p k c", p=128), 16, 256) for tt_ in range(4) for s8_ in range(8)])
                    for tt in range(4):
                        LD("sync", xs[0][:], xo[tcol + tt * 128:tcol + (tt + 1) * 128, :], ["xs0"])
                        OP("gpsimd", "tensor_scalar_mul", ["xs0"], ["xs0"], out=xs[0][:], in0=xs[0][:], scalar1=ALPHA)
                        for s8 in range(8):
                            vwo, kwo = load_slab(w_out[:, s8 * 256:(s8 + 1) * 256].rearrange("(k p) c -> p k c", p=128), 16, 256, tag=("wout", tt, s8))
                            pbk, pkey = pbank[s8 % 2], f"pb{s8 % 2}"
                            for k in range(16):
                                MM(pbk[:, 0:256], mixT[:, k, tt * 128:(tt + 1) * 128], vwo[:, k, :], k == 0, k == 15, [kwo, "mixT"], [pkey])
                            dsl = slice(s8 * 256, (s8 + 1) * 256)
                            OP("vector", "tensor_tensor", [pkey, "modb"], ["x1p"], out=x1p[:, dsl], in0=pbk[:, 0:256], in1=modb[:, 0, dsl], op=ALU.mult)
                            OP("gpsimd", "tensor_tensor", ["x1p", "xs0"], ["x1p"], out=x1p[:, dsl], in0=x1p[:, dsl], in1=xs[0][:, dsl], op=ALU.add)
                        for q in range(4):
                            OP("vector", "bn_stats", ["x1p"], ["lnst"], out=lnst[:, q, :], in_=x1p[:, q * 512:(q + 1) * 512])
                        OP("vector", "bn_aggr", ["lnst"], ["lnag"], out=lnag[:], in_=lnst[:].rearrange("p a b -> p (a b)"))
                        OP("scalar", "activation", ["lnag"], ["lnag"], out=lnag[:, 1:2], in_=lnag[:, 1:2], func=AF.Sqrt, bias=epsL[:, 0:1])
                        OP("vector", "reciprocal", ["lnag"], ["lnag"], out=lnag[:, 1:2], in_=lnag[:, 1:2])
                        OP("vector", "tensor_scalar", ["x1p", "lnag"], ["x1p"], out=x1p[:], in0=x1p[:], scalar1=lnag[:, 0:1], scalar2=lnag[:, 1:2], op0=ALU.subtract, op1=ALU.mult)
                        OP("gpsimd", "tensor_tensor", ["x1p", "lnw"], ["x1p"], out=x1p[:], in0=x1p[:], in1=lnw[:, 0, :], op=ALU.mult)
                        OP("vector", "tensor_tensor", ["x1p", "lnw"], ["x1p"], out=x1p[:], in0=x1p[:], in1=lnw[:, 1, :], op=ALU.add)
                        LD("sync", x1_d[tcol + tt * 128:tcol + (tt + 1) * 128, :], x1p[:], [], ["x1p"])
                        if debug:
                            LD("sync", dbg["x1"][tcol + tt * 128:tcol + (tt + 1) * 128, :], x1p[:], [], ["x1p"])
                        OP("gpsimd", "tensor_tensor", ["x1p", "modb"], ["xs1"], out=xs[1][:], in0=x1p[:], in1=modb[:, 2, :], op=ALU.mult)
                        OP("vector", "tensor_tensor", ["xs1", "modb"], ["h2t"], out=h2t[:], in0=xs[1][:], in1=modb[:, 1, :], op=ALU.add)
                        if stage >= 2:
                            OP("gpsimd", "tensor_tensor", ["xs1", "modb"], ["xs1"], out=xs[1][:], in0=xs[1][:], in1=modb[:, 1, :], op=ALU.add)
                            for q4 in range(4):
                                for u_ in range(4):
                                    c_ = q4 * 4 + u_
                                    TR(pbank[6][:, u_ * 128:(u_ + 1) * 128], xs[1][:, c_ * 128:(c_ + 1) * 128], ident, ["xs1", "cm"], ["pb6"])
                                OP("scalar", "activation", ["pb6"], ["h2T"], out=h2T[:, q4 * 4:(q4 + 1) * 4, :], in_=pbank[6][:, :].rearrange("p (u q) -> p u q", q=128), func=AF.Identity)
                            for c_ in range(16):
                                MM(pbank[2][:, 0:NEXP], h2T[:, c_, :], wr[:, c_, :], c_ == 0, c_ == 15, ["h2T", "wr"], ["pb2"])
                            OP("vector", "tensor_tensor", ["pb2", "brb"], ["lgt"], out=lgt[:], in0=pbank[2][:, 0:NEXP], in1=brb[:], op=ALU.add)
                            LD("sync", lg_d[tcol + tt * 128:tcol + (tt + 1) * 128, :], lgt[:], [], ["lgt"])
                        LD("sync", h2_d[tcol + tt * 128:tcol + (tt + 1) * 128, :], h2t[:], [], ["h2t"])
                    P.barrier()
                    st2.close()
            except _Stop:
                P.barrier()
                for sc_ in reversed(scopes):
                    sc_.close()
            P.barrier()
        if stage == 1:
            st_p.close()
            with ExitStack() as st_o:
                ot = st_o.enter_context(nc.sbuf_tensor("ot", [128, D], F32))
                OP("gpsimd", "memset", [], ["ot"], ap=ot[:], constant=0.0)
                for tt in range(8):
                    LD("sync", out[tt * 128:(tt + 1) * 128, :], ot[:], [], ["ot"])
                P.barrier()
            P.emit(nc, st)
            return nc
        st_p.close()
        moe_phase(nc, P, st, dict(OP=OP, MM=MM, TR=TR, LD=LD, slab=slab, NSL=NSL, pbank=pbank, ptb=ptb, cm=cm, cmb=cmb, epsL=epsL,
                                  h2_d=h2_d, x1_d=x1_d, lg_d=lg_d, modb_d=modb_d, lnb=lnb, w_gu=w_gu, b_guT=b_guT, w_down=w_down, b_down=b_down,
                                  iota_cap=iota_cap, out=out, debug=debug, dbg=dbg, n_exp=n_exp))
        P.emit(nc, st)
    return nc


def moe_phase(nc, P, st, env):
    OP, MM, TR, LD = env["OP"], env["MM"], env["TR"], env["LD"]
    slab, NSL, pbank, ptb, cm, cmb, epsL = env["slab"], env["NSL"], env["pbank"], env["ptb"], env["cm"], env["cmb"], env["epsL"]
    h2_d, x1_d, lg_d, modb_d, lnb = env["h2_d"], env["x1_d"], env["lg_d"], env["modb_d"], env["lnb"]
    w_gu, b_guT, w_down, b_down, iota_cap, out = env["w_gu"], env["b_guT"], env["w_down"], env["b_down"], env["iota_cap"], env["out"]
    debug, dbg, n_exp = env["debug"], env["dbg"], env["n_exp"]
    ident, m_su, ones_f = cm[:, 0, :], cm[:, 1, :], cm[:, 5, :]
    ident_b = cmb[:, 0, :]
    NT = T_OWN // 128
    NR = CAP // 128
    with ExitStack() as sm:
        sb = lambda n, s_, d=F32: sm.enter_context(nc.sbuf_tensor(n, s_, d))
        h2 = sb("h2", [128, NT, D], BF16)
        acc = sb("acc", [128, NT, D])
        iot = sb("iot", [128, CAP])
        lg = sb("lg", [128, NT, NEXP]); pm = sb("pm", [128, NT, NEXP]); pos = sb("pos", [128, NT, NEXP]); msk = sb("msk", [128, NT, NEXP])
        mx8 = sb("mx8", [128, 8]); ssum = sb("ssum", [128, 1]); nmx = sb("nmx", [128, 1])
        bgu = [sb(f"bgu{i}", [128, 32]) for i in range(2)]
        bdn = sb("bdn", [32, D], BF16)
        pmT = sb("pmT", [32, T_OWN], BF16)
        sm2 = ExitStack()
        sb2 = lambda n, s_, d=F32: sm2.enter_context(nc.sbuf_tensor(n, s_, d))
        XT = sb2("XT", [128, 16, CAP], BF16)
        actT = sb2("actT", [128, 16, CAP], BF16)
        Sel = sb2("Sel", [128, NT, CAP], BF16)
        SelW = [sb2(f"SelW{i}", [128, CAP], BF16) for i in range(2)]
        SelP = sb2("SelP", [128, NR, T_OWN], BF16)
        Yb = [sb2(f"Yb{i}", [128, NR, 256], BF16) for i in range(2)]
        gc = sb2("gc", [128, CAP]); sg = sb2("sg", [128, CAP]); u1 = sb2("u1", [128, CAP])

        for i in range(NT):
            LD("sync", h2[:, i, :], h2_d[i * 128:(i + 1) * 128, :], [f"h2_{i}"])
        LD("sync", lg[:], lg_d.rearrange("(i p) e -> p i e", p=128), ["lg"])
        LD("sync", iot[:], iota_cap, ["iot"])
        LD("gpsimd", bdn[:], b_down, ["bdn"])
        for i in range(NT):
            OP("gpsimd", "memset", [], [f"acc{i}"], ap=acc[:, i, :], constant=0.0)
        for i in range(NT):
            OP("vector", "max", ["lg"], ["mx8"], out=mx8[:], in_=lg[:, i, :])
            OP("vector", "tensor_scalar", ["lg", "mx8"], ["msk"], out=msk[:, i, :], in0=lg[:, i, :], scalar1=mx8[:, 3:4], scalar2=None, op0=ALU.is_ge)
            OP("vector", "tensor_scalar_mul", ["mx8"], ["nmx"], out=nmx[:], in0=mx8[:, 0:1], scalar1=-1.0)
            OP("scalar", "activation", ["lg", "nmx"], ["pm"], out=pm[:, i, :], in_=lg[:, i, :], func=AF.Exp, bias=nmx[:, 0:1], scale=1.0)
            OP("vector", "tensor_tensor", ["pm", "msk"], ["pm"], out=pm[:, i, :], in0=pm[:, i, :], in1=msk[:, i, :], op=ALU.mult)
            OP("vector", "tensor_reduce", ["pm"], ["ssum"], out=ssum[:], in_=pm[:, i, :], axis=mybir.AxisListType.X, op=ALU.add)
            OP("vector", "reciprocal", ["ssum"], ["ssum"], out=ssum[:], in_=ssum[:])
            OP("vector", "tensor_scalar_mul", ["pm", "ssum"], ["pm"], out=pm[:, i, :], in0=pm[:, i, :], scalar1=ssum[:, 0:1])
        for i in range(NT):
            pb = pbank[2 + (i % 2)]
            pkey = f"pb{2 + (i % 2)}"
            MM(pb[:, 0:NEXP], m_su, msk[:, i, :], True, i == 0, ["cm", "msk"], [pkey])
            for i2 in range(i):
                MM(pb[:, 0:NEXP], ones_f, msk[:, i2, :], False, i2 == i - 1, ["cm", "msk"], [pkey])
            OP("vector", "tensor_tensor", [pkey, "msk"], ["pos"], out=pos[:, i, :], in0=pb[:, 0:NEXP], in1=msk[:, i, :], op=ALU.mult)
            OP("vector", "tensor_tensor", ["pos", "msk"], ["pos"], out=pos[:, i, :], in0=pos[:, i, :], in1=msk[:, i, :], op=ALU.add)
            OP("vector", "tensor_scalar_add", ["pos"], ["pos"], out=pos[:, i, :], in0=pos[:, i, :], scalar1=-1.0)
            OP("vector", "tensor_copy", ["pm"], ["SelW0"], out=SelW[0][:, 0:NEXP], in_=pm[:, i, :])
            TR(ptb[0:NEXP, 0:128], SelW[0][:, 0:NEXP], ident_b, ["SelW0", "cmb"], ["ptb"])
            OP("vector", "tensor_copy", ["ptb"], ["pmT"], out=pmT[:, i * 128:(i + 1) * 128], in_=ptb[0:NEXP, 0:128])
        if debug:
            LD("sync", dbg["pm"].rearrange("(i p) e -> p i e", p=128), pm[:], [], ["pm"])
            LD("sync", dbg["pos"].rearrange("(i p) e -> p i e", p=128), pos[:], [], ["pos"])

        slabs = []
        for e in range(n_exp):
            for j in range(16):
                slabs.append(w_gu[e, :, 256 * j:256 * (j + 1)].rearrange("(k p) c -> p k c", p=128))
            for s8 in range(8):
                slabs.append(w_down[e, :, 256 * s8:256 * (s8 + 1)].rearrange("(k p) c -> p k c", p=128))
        issued = [0]

        def get_slab(idx, look=2):
            while issued[0] <= min(idx + look, len(slabs) - 1):
                k_ = issued[0]
                b_ = k_ % NSL
                view = slab[b_][:, 0:4096].rearrange("p (k c) -> p k c", k=16)
                LD("gpsimd", view, slabs[k_], [f"slab{b_}"])
                issued[0] += 1
            b_ = idx % NSL
            return slab[b_][:, 0:4096].rearrange("p (k c) -> p k c", k=16), f"slab{b_}"

        sidx = 0
        for e in range(n_exp):
            bg = bgu[e % 2]
            bgk = f"bgu{e % 2}"
            LD("sync", bg[:], b_guT[e], [bgk])
            for i in range(NT):
                OP("vector", "tensor_scalar", ["iot", "pos"], [f"Sel{i}"], out=Sel[:, i, :], in0=iot[:], scalar1=pos[:, i, e:e + 1], scalar2=None, op0=ALU.is_equal)
                sw, swk = SelW[i % 2], f"SelW{i % 2}"
                OP("vector", "tensor_scalar", ["iot", "pos", "pm"], [swk], out=sw[:], in0=iot[:], scalar1=pos[:, i, e:e + 1], scalar2=pm[:, i, e:e + 1], op0=ALU.is_equal, op1=ALU.mult)
                for r in range(NR):
                    TR(ptb[:, r * 128:(r + 1) * 128], sw[:, r * 128:(r + 1) * 128], ident_b, [swk, "cmb"], ["ptb"])
                OP("scalar", "activation", ["ptb"], [f"SelP{i}"], out=SelP[:, :, i * 128:(i + 1) * 128], in_=ptb[:, 0:NR * 128].rearrange("p (r q) -> p r q", q=128), func=AF.Identity)
            for c in range(16):
                pb, pkey = pbank[c % 2], f"pb{c % 2}"
                for i in range(NT):
                    MM(pb[:, 0:CAP], h2[:, i, c * 128:(c + 1) * 128], Sel[:, i, :], i == 0, i == NT - 1, [f"h2_{i}", f"Sel{i}"], [pkey])
                OP("scalar", "activation", [pkey], [f"XT{c}"], out=XT[:, c, :], in_=pb[:, 0:CAP], func=AF.Identity)
            xkeys = [f"XT{c}" for c in range(16)]
            for j in range(16):
                view, skey = get_slab(sidx); sidx += 1
                for k in range(16):
                    MM(pbank[2][:, 0:CAP], view[:, k, 0:256:2], XT[:, k, :], k == 0, k == 15, [skey, f"XT{k}"], ["pb2"])
                for k in range(16):
                    MM(pbank[3][:, 0:CAP], view[:, k, 1:256:2], XT[:, k, :], k == 0, k == 15, [skey, f"XT{k}"], ["pb3"])
                OP("vector", "tensor_scalar", ["pb2", bgk], ["gc"], out=gc[:], in0=pbank[2][:, 0:CAP], scalar1=bg[:, j:j + 1], scalar2=7.0, op0=ALU.add, op1=ALU.min)
                OP("scalar", "activation", ["gc"], ["sg"], out=sg[:], in_=gc[:], func=AF.Sigmoid, scale=1.702)
                OP("vector", "tensor_scalar", ["pb3", bgk], ["u1"], out=u1[:], in0=pbank[3][:, 0:CAP], scalar1=bg[:, 16 + j:17 + j], scalar2=7.0, op0=ALU.add, op1=ALU.min)
                OP("vector", "tensor_scalar", ["u1"], ["u1"], out=u1[:], in0=u1[:], scalar1=-7.0, scalar2=1.0, op0=ALU.max, op1=ALU.add)
                OP("vector", "tensor_tensor", ["gc", "sg"], ["sg"], out=sg[:], in0=gc[:], in1=sg[:], op=ALU.mult)
                OP("vector", "tensor_tensor", ["u1", "sg"], [f"act{j}"], out=actT[:, j, :], in0=u1[:], in1=sg[:], op=ALU.mult)
            akeys = [f"act{j}" for j in range(16)]
            for s8 in range(8):
                view, skey = get_slab(sidx); sidx += 1
                yb, ybk = Yb[s8 % 2], f"Yb{s8 % 2}"
                dsl = slice(s8 * 256, (s8 + 1) * 256)
                for r in range(NR):
                    pb, pkey = pbank[r % 2], f"pb{r % 2}"
                    for j in range(16):
                        MM(pb[:, 0:256], actT[:, j, r * 128:(r + 1) * 128], view[:, j, :], j == 0, j == 15, [skey, f"act{j}"], [pkey])
                    OP("scalar", "activation", [pkey], [ybk], out=yb[:, r, :], in_=pb[:, 0:256], func=AF.Identity)
                for i in range(NT):
                    pb, pkey = pbank[4 + (i % 2)], f"pb{4 + (i % 2)}"
                    for r in range(NR):
                        MM(pb[:, 0:256], SelP[:, r, i * 128:(i + 1) * 128], yb[:, r, :], r == 0, r == NR - 1, [f"SelP{i}", ybk], [pkey])
                    OP("vector", "tensor_tensor", [pkey, f"acc{i}"], [f"acc{i}"], out=acc[:, i, dsl], in0=pb[:, 0:256], in1=acc[:, i, dsl], op=ALU.add)
        P.barrier()
        sm2.close()
        for i in range(NT):
            for s8 in range(8):
                dsl = slice(s8 * 256, (s8 + 1) * 256)
                pb, pkey = pbank[4 + (s8 % 2)], f"pb{4 + (s8 % 2)}"
                MM(pb[:, 0:256], pmT[0:n_exp, i * 128:(i + 1) * 128], bdn[0:n_exp, dsl], True, True, ["pmT", "bdn"], [pkey])
                OP("vector", "tensor_tensor", [pkey, f"acc{i}"], [f"acc{i}"], out=acc[:, i, dsl], in0=pb[:, 0:256], in1=acc[:, i, dsl], op=ALU.add)
        g2b = sb("g2b", [128, D]); l2g = sb("l2g", [128, D]); l2b = sb("l2b", [128, D])
        xt = [sb(f"xt{i}", [128, D]) for i in range(2)]
        st6 = sb("st6", [128, 4, 6]); ag = sb("ag", [128, 2])
        LD("sync", g2b[:], modb_d[3], ["g2b"]); LD("sync", l2g[:], lnb[2], ["l2g"]); LD("sync", l2b[:], lnb[3], ["l2b"])
        for i in range(NT):
            x_, xk = xt[i % 2], f"xt{i % 2}"
            ak = f"acc{i}"
            LD("sync", x_[:], x1_d[i * 128:(i + 1) * 128, :], [xk])
            OP("gpsimd", "tensor_tensor", [ak, "g2b"], [ak], out=acc[:, i, :], in0=acc[:, i, :], in1=g2b[:], op=ALU.mult)
            OP("vector", "scalar_tensor_tensor", [xk, ak], [ak], out=acc[:, i, :], in0=x_[:], scalar=ALPHA, in1=acc[:, i, :], op0=ALU.mult, op1=ALU.add)
            for q in range(4):
                OP("vector", "bn_stats", [ak], ["st6"], out=st6[:, q, :], in_=acc[:, i, q * 512:(q + 1) * 512])
            OP("vector", "bn_aggr", ["st6"], ["ag"], out=ag[:], in_=st6[:].rearrange("p a b -> p (a b)"))
            OP("scalar", "activation", ["ag"], ["ag"], out=ag[:, 1:2], in_=ag[:, 1:2], func=AF.Sqrt, bias=epsL[:, 0:1])
            OP("vector", "reciprocal", ["ag"], ["ag"], out=ag[:, 1:2], in_=ag[:, 1:2])
            OP("vector", "tensor_scalar", [ak, "ag"], [ak], out=acc[:, i, :], in0=acc[:, i, :], scalar1=ag[:, 0:1], scalar2=ag[:, 1:2], op0=ALU.subtract, op1=ALU.mult)
            OP("gpsimd", "tensor_tensor", [ak, "l2g"], [ak], out=acc[:, i, :], in0=acc[:, i, :], in1=l2g[:], op=ALU.mult)
            OP("vector", "tensor_tensor", [ak, "l2b"], [ak], out=acc[:, i, :], in0=acc[:, i, :], in1=l2b[:], op=ALU.add)
            LD("sync", out[i * 128:(i + 1) * 128, :], acc[:, i, :], [], [ak])
        P.barrier()


def prep_inputs(inp, stage=2, cores=range(8)):
    f = lambda a: np.ascontiguousarray(a, dtype=np.float32)
    x = inp["x"]; c = inp["c"]
    idx = np.arange(128)
    cmask = np.zeros((128, 6, 128), np.float32)
    cmask[:, 0] = np.eye(128)
    cmask[:, 1] = idx[:, None] < idx[None, :]
    cmask[:, 2] = idx[:, None] > idx[None, :]
    cmask[:, 3] = idx[:, None] <= idx[None, :]
    cmask[:, 4] = (idx[:, None] // 64) == (idx[None, :] // 64)
    cmask[:, 5] = 1.0
    segm = np.ones((128, 512), np.float32); segm[:, ::128] = 0.0
    mu = inp["mu_shift"][0]
    muT = np.zeros((128, 27), np.float32)
    for gi in range(24):
        muT[:, gi] = mu[128 * gi:128 * (gi + 1)]
    muT[:, 24] = mu[3072:3200]; muT[:, 25] = mu[3200:3328]; muT[:32, 26] = mu[3328:3360]
    vecs = [inp["w0"][0], inp["a0"][0], inp["k_k"][0], inp["k_a"][0], inp["r_k"][0].reshape(-1), inp["lnx_g"][0], inp["lnx_b"][0]]
    vecT = np.stack([v.reshape(8, 128).T for v in vecs], axis=1)
    shared = dict(
        cmask=cmask, segm=segm, w_ada=f(inp["w_ada"][0]), b_ada_b=f(np.broadcast_to(inp["b_ada"][0][None, :], (128, 6 * D))),
        w_in=f(inp["w_in"][0]), muT=muT, vecT=f(vecT), w_dup=f(inp["w_decay_up"][0]), w_iup=f(inp["w_iclr_up"][0]),
        w_gup=f(inp["w_glora_up"][0]), w_ro=f(inp["w_rwkv_out"][0]), w_pool=f(inp["w_pool"][0]),
        pscT=f(inp["pool_scale"][0].reshape(16, 128).T), w_out=f(inp["w_out"][0]),
        lnb=f(np.stack([np.broadcast_to(inp[k][0][None, :], (128, D)) for k in ("ln1_g", "ln1_b", "ln2_g", "ln2_b")])),
    )
    if stage >= 2:
        shared.update(
            w_router=f(inp["w_router"][0]), b_router_b=f(np.broadcast_to(inp["b_router"][0][None, :], (128, NEXP))),
            w_gu=f(inp["w_gu"][0]), w_down=f(inp["w_down"][0]), b_down=f(inp["b_down"][0]),
            b_guT=f(np.concatenate([inp["b_gu"][0][:, 0::2].reshape(NEXP, 16, 128), inp["b_gu"][0][:, 1::2].reshape(NEXP, 16, 128)], axis=1).transpose(0, 2, 1)),
            iota_cap=f(np.broadcast_to(np.arange(CAP, dtype=np.float32)[None, :], (128, CAP))),
        )
    maps = []
    for core in cores:
        b, half = core // 2, core % 2
        m = dict(shared)
        m["xo"] = f(x[b, half * T_OWN:(half + 1) * T_OWN])
        m["xp"] = f(x[b, 0:T_OWN])
        m["cT"] = f(c[b].reshape(16, 128).T)
        m["flag"] = np.full((128, 1), float(half), np.float32)
        pc = np.zeros((128, 4, 16), np.float32)
        for g, W in enumerate((2, 4, 8, 16)):
            pc[:, g, :] = 1.0 / W if half == 1 else 1.0 / np.minimum(np.arange(16) + 1, W)
        m["pcorr"] = pc
        maps.append(m)
    return maps


_NC_CACHE = {}


def kernel(**inputs):
    if "full" not in _NC_CACHE:
        _NC_CACHE["full"] = build_program(stage=2, debug=False)
    nc = _NC_CACHE["full"]
    maps = prep_inputs(inputs, stage=2)
    res = run_bass_kernel_spmd(nc, maps, core_ids=list(range(8)))
    out = np.zeros((4, 2048, D), np.float32)
    for core in range(8):
        b, half = core // 2, core % 2
        out[b, half * T_OWN:(half + 1) * T_OWN] = res.results[core]["out"]
    return out
```

```python
from contextlib import ExitStack
import numpy as np
import concourse.bass as bass
import concourse.mybir as mybir
from concourse.bass_utils import run_bass_kernel_spmd

F32 = mybir.dt.float32
BF16 = mybir.dt.bfloat16
AF = mybir.ActivationFunctionType
ALU = mybir.AluOpType
ENGS = ["tensor", "vector", "scalar", "gpsimd", "sync"]
N_DMA_SEMS = 12
EPOCH = 20000

D = 2048
T_OWN = 1024
NEXP = 32
CAP = 512
ALPHA = 2.0 ** 0.25
LN_EPS = 1e-5
GN_EPS = 64e-5
NSHIFT = 3360
NIN = 8480


class Prog:
    def __init__(self, same_engine_sync=True):
        self.ins = []
        self.last_w = {}
        self.readers = {}
        self.same_engine_sync = same_engine_sync
        self.dma_count = [0] * N_DMA_SEMS
        self.n_dma = 0
        self.n_dma_sw = 0

    def _deps(self, reads, writes):
        deps = set()
        for k in reads:
            if k in self.last_w:
                deps.add(self.last_w[k])
        for k in writes:
            if k in self.last_w:
                deps.add(self.last_w[k])
            for r in self.readers.get(k, ()):
                deps.add(r)
        return deps

    def _commit(self, iid, reads, writes):
        for k in reads:
            self.readers.setdefault(k, []).append(iid)
        for k in writes:
            self.last_w[k] = iid
            self.readers[k] = []

    def op(self, eng, fn, reads=(), writes=()):
        deps = self._deps(reads, writes)
        iid = len(self.ins)
        self.ins.append(dict(eng=eng, fn=fn, deps=deps, dma=None, target=False))
        self._commit(iid, reads, writes)
        return iid

    def dma(self, eng, fn, reads=(), writes=()):
        deps = self._deps(reads, writes)
        iid = len(self.ins)
        half = N_DMA_SEMS // 2
        if eng == "gpsimd":
            s = self.n_dma_sw % half
            self.n_dma_sw += 1
        else:
            s = half + self.n_dma % half
            self.n_dma += 1
        self.dma_count[s] += 1
        self.ins.append(dict(eng=eng, fn=fn, deps=deps, dma=(s, 16 * self.dma_count[s]), target=False))
        self._commit(iid, reads, writes)
        return iid

    def barrier(self, engines=ENGS):
        deps = set()
        last = {}
        for i, r in enumerate(self.ins):
            if r["fn"] is None:
                continue
            if r["dma"] is not None:
                deps.add(i)
            else:
                last[r["eng"]] = i
        deps.update(last.values())
        for e in engines:
            self.ins.append(dict(eng=e, fn=None, deps=set(deps), dma=None, target=False))
        self.last_w = {}
        self.readers = {}

    def emit(self, nc, stack):
        ins = self.ins
        ses = self.same_engine_sync
        for r in ins:
            for d in r["deps"]:
                t = ins[d]
                if t["dma"] is None:
                    if t["eng"] == r["eng"] and (t["eng"] == "tensor" or not ses):
                        continue
                    t["target"] = True
        cnt = {e: 0 for e in ENGS}
        for r in ins:
            if r["dma"] is None and r["target"]:
                cnt[r["eng"]] += 1
                r["seq"] = cnt[r["eng"]]
        n_ep = {e: max(1, -(-cnt[e] // EPOCH)) for e in ENGS}
        psem = {e: [stack.enter_context(nc.semaphore(f"p_{e}_{k}")) for k in range(n_ep[e])] for e in ENGS}
        dsem = [stack.enter_context(nc.semaphore(f"dma_{k}")) for k in range(N_DMA_SEMS)]
        per_eng = {e: [] for e in ENGS}
        for i, r in enumerate(ins):
            per_eng[r["eng"]].append(i)
        block = stack.enter_context(nc.Block())

        def run_engine(ename, e):
            waited = {}
            for i in per_eng[ename]:
                r = ins[i]
                need = {}
                for d in r["deps"]:
                    t = ins[d]
                    if t["dma"] is not None:
                        s, v = t["dma"]
                        key = ("d", s)
                    else:
                        if t["eng"] == ename and (ename == "tensor" or not ses):
                            continue
                        ep, v = divmod(t["seq"] - 1, EPOCH)
                        v += 1
                        key = (t["eng"], ep)
                    if v > need.get(key, 0):
                        need[key] = v
                for key, v in need.items():
                    if waited.get(key, 0) >= v:
                        continue
                    waited[key] = v
                    sem = dsem[key[1]] if key[0] == "d" else psem[key[0]][key[1]]
                    e.wait_ge(sem, v)
                if r["fn"] is None:
                    continue
                if r["dma"] is not None and r["dma"][1] > 16:
                    key = ("d", r["dma"][0])
                    if waited.get(key, 0) < r["dma"][1] - 16:
                        waited[key] = r["dma"][1] - 16
                        e.wait_ge(dsem[r["dma"][0]], r["dma"][1] - 16)
                bi = r["fn"](e)
                if r["dma"] is not None:
                    bi.then_inc(dsem[r["dma"][0]], 16)
                elif r["target"]:
                    bi.then_inc(psem[ename][(r["seq"] - 1) // EPOCH], 1)

        block.tensor(lambda e: run_engine("tensor", e))
        block.vector(lambda e: run_engine("vector", e))
        block.scalar(lambda e: run_engine("scalar", e))
        block.gpsimd(lambda e: run_engine("gpsimd", e))
        block.sync(lambda e: run_engine("sync", e))


G_R, G_K, G_V = 0, 1024, 2048
G_WDAD = 3072
G_GD = 3200
G_POOL = 3360
G_GA = 4384
G_GB = 6432


class _Stop(Exception):
    pass


def build_program(stage=2, debug=False, stop=None, n_exp=NEXP):
    def CP(name):
        if stop == name:
            raise _Stop()
    nc = bass.Bass("TRN2", target_bir_lowering=False)
    P = Prog()
    AX = mybir.AxisListType

    def OP(eng, method, reads, writes, **kw):
        P.op(eng, lambda e: getattr(e, method)(**kw), reads=reads, writes=writes)

    def MM(out, lhsT, rhs, start, stop, reads, writes):
        P.op("tensor", lambda e: e.matmul(out, lhsT=lhsT, rhs=rhs, start=start, stop=stop), reads=reads, writes=writes)

    def TR(out, in_, identity, reads, writes):
        P.op("tensor", lambda e: e.transpose(out=out, in_=in_, identity=identity), reads=reads, writes=writes)

    def LD(eng, dst, src, writes, reads=()):
        P.dma(eng, lambda e: e.dma_start(out=dst, in_=src), reads=reads, writes=writes)

    dt_in = lambda n, s, d=F32: nc.dram_tensor(n, s, d, kind="ExternalInput").ap()
    dt_out = lambda n, s, d=F32: nc.dram_tensor(n, s, d, kind="ExternalOutput").ap()
    xo = dt_in("xo", [T_OWN, D]); xp = dt_in("xp", [T_OWN, D])
    cT = dt_in("cT", [128, 16]); flag = dt_in("flag", [128, 1]); pcorr = dt_in("pcorr", [128, 4, 16])
    cmask = dt_in("cmask", [128, 6, 128]); segm = dt_in("segm", [128, 512])
    w_ada = dt_in("w_ada", [D, 6 * D]); b_ada_b = dt_in("b_ada_b", [128, 6 * D])
    w_in = dt_in("w_in", [D, NIN]); muT = dt_in("muT", [128, 27]); vecT = dt_in("vecT", [128, 7, 8])
    w_dup = dt_in("w_dup", [64, 1024]); w_iup = dt_in("w_iup", [64, 1024]); w_gup = dt_in("w_gup", [160, 1024])
    w_ro = dt_in("w_ro", [1024, D]); w_pool = dt_in("w_pool", [4, 256, 512]); pscT = dt_in("pscT", [128, 16])
    w_out = dt_in("w_out", [D, D]); lnb = dt_in("lnb", [4, 128, D])
    if stage >= 2:
        w_router = dt_in("w_router", [D, NEXP]); b_router_b = dt_in("b_router_b", [128, NEXP])
        w_gu = dt_in("w_gu", [NEXP, D, 2 * D]); b_guT = dt_in("b_guT", [NEXP, 128, 32])
        w_down = dt_in("w_down", [NEXP, D, D]); b_down = dt_in("b_down", [NEXP, D])
        iota_cap = dt_in("iota_cap", [128, CAP])
    out = dt_out("out", [T_OWN, D])
    x1_d = nc.dram_tensor("x1_d", [T_OWN, D], F32, kind="Internal").ap()
    h2_d = nc.dram_tensor("h2_d", [T_OWN, D], BF16, kind="Internal").ap()
    modb_d = nc.dram_tensor("modb_d", [4, 128, D], F32, kind="Internal").ap()
    lg_d = nc.dram_tensor("lg_d", [T_OWN, NEXP], F32, kind="ExternalOutput" if debug else "Internal").ap()
    dbg = {}
    if debug:
        dbg["yr"] = dt_out("dbg_yr", [128, 8, T_OWN]); dbg["mixin"] = dt_out("dbg_mixin", [128, 16, T_OWN])
        dbg["modb"] = dt_out("dbg_modb", [128, 2 * D]); dbg["ps"] = dt_out("dbg_ps", [128, 27, T_OWN])
        dbg["ld"] = dt_out("dbg_ld", [128, 8, T_OWN]); dbg["dT"] = dt_out("dbg_dT", [128, 8, T_OWN])
        dbg["y"] = dt_out("dbg_y", [128, 8, T_OWN])
        dbg["x1"] = dt_out("dbg_x1", [T_OWN, D])
        if stage >= 2:
            dbg["pm"] = dt_out("dbg_pm", [T_OWN, NEXP]); dbg["pos"] = dt_out("dbg_pos", [T_OWN, NEXP])

    with ExitStack() as st:
        sb = lambda n, s, d=F32: st.enter_context(nc.sbuf_tensor(n, s, d))
        cm = sb("cm", [128, 6, 128])
        ident, m_su, m_sl, m_ui, m_bd, ones_f = (cm[:, i, :] for i in range(6))
        cmb = sb("cmb", [128, 6, 128], BF16)
        ident_b = cmb[:, 0, :]
        epsG = sb("epsG", [128, 1]); epsL = sb("epsL", [128, 1])
        OP("gpsimd", "memset", [], ["epsG"], ap=epsG[:], constant=GN_EPS)
        OP("gpsimd", "memset", [], ["epsL"], ap=epsL[:], constant=LN_EPS)
        NSL = 4
        slab = [sb(f"slab{i}", [128, 4096], BF16) for i in range(NSL)]
        slab_i = [0]
        pbank = [st.enter_context(nc.psum_tensor(f"pb{i}", [128, 512], F32)) for i in range(7)]
        ptb = st.enter_context(nc.psum_tensor("ptb", [128, 1024], BF16))
        st_p = ExitStack()
        sbp = lambda n, s_, d=F32: st_p.enter_context(nc.sbuf_tensor(n, s_, d))
        seg = sbp("seg", [128, 512]); flg = sbp("flg", [128, 1]); pcr = sbp("pcr", [128, 4, 16])
        mu = sbp("mu", [128, 27]); omu = sbp("omu", [128, 27]); vec = sbp("vec", [128, 7, 8]); psc = sbp("psc", [128, 16])
        condB = sbp("condB", [128, 16, 128], BF16); condf = sbp("condf", [128, 16])
        sc1p = sbp("sc1p", [128, 16]); sh1 = sbp("sh1", [128, 16])
        wlo = sbp("wlo", [128, 3, 1024], BF16)

        pq = []
        LOOK = 2

        def plan(specs):
            pq.extend(dict(tag=t_, src=s_, kc=k_, cols=c_, buf=None) for (t_, s_, k_, c_) in specs)

        def _issue(src_ap, kc, cols):
            i = slab_i[0] % NSL
            slab_i[0] += 1
            view = slab[i][:, 0:kc * cols].rearrange("p (k c) -> p k c", k=kc)
            LD("gpsimd", view, src_ap, [f"slab{i}"])
            return view, f"slab{i}"

        def load_slab(src_ap, kc, cols, tag=None):
            if pq:
                assert tag is not None and pq[0]["tag"] == tag, (tag, pq[0]["tag"])
                for it in pq[:1 + LOOK]:
                    if it["buf"] is None:
                        it["buf"] = _issue(it["src"], it["kc"], it["cols"])
                return pq.pop(0)["buf"]
            return _issue(src_ap, kc, cols)

        def flush_plan():
            assert not pq, [it["tag"] for it in pq]

        for dst, src, key in [(cm[:], cmask, "cm"), (seg[:], segm, "seg"), (flg[:], flag, "flg"), (pcr[:], pcorr, "pcr"), (mu[:], muT, "mu"),
                              (vec[:], vecT, "vec"), (psc[:], pscT, "psc"), (condf[:], cT, "condf")]:
            LD("sync", dst, src, [key])
        LD("gpsimd", wlo[0:64, 0, :], w_dup, ["wlo"]); LD("gpsimd", wlo[64:128, 0, :], w_iup, ["wlo"])
        LD("gpsimd", wlo[:, 1, :], w_gup[0:128, :], ["wlo"]); LD("gpsimd", wlo[0:32, 2, :], w_gup[128:160, :], ["wlo"])
        OP("vector", "tensor_copy", ["cm"], ["cmb"], out=cmb[:], in_=cm[:])
        OP("vector", "tensor_scalar", ["mu"], ["omu"], out=omu[:], in0=mu[:], scalar1=-1.0, scalar2=1.0, op0=ALU.mult, op1=ALU.add)
        OP("scalar", "activation", ["condf"], ["condf"], out=condf[:], in_=condf[:], func=AF.Silu)
        for k in range(16):
            OP("vector", "tensor_copy", ["condf"], ["condB"], out=condB[:, k, :], in_=condf[:, k:k + 1].to_broadcast([128, 128]))

        with ExitStack() as st_a:
            badd = st_a.enter_context(nc.sbuf_tensor("badd", [128, 2, 256], F32))
            mod01 = st_a.enter_context(nc.sbuf_tensor("mod01", [128, 2 * D], F32))
            mtmp = st_a.enter_context(nc.sbuf_tensor("mtmp", [128, 16, 128], F32))

            def ada_slab(si, dst_ap, dst_key):
                c0 = si * 256
                view, key = load_slab(w_ada[:, c0:c0 + 256].rearrange("(k p) c -> p k c", p=128), 16, 256, tag=("ada", si))
                bi = si % 2
                LD("sync", badd[:, bi, :], b_ada_b[:, c0:c0 + 256], [f"badd{bi}"])
                pbk = pbank[si % 2]
                for k in range(16):
                    MM(pbk[:, 0:256], condB[:, k, :], view[:, k, :], k == 0, k == 15, [key, "condB"], [f"pb{si % 2}"])
                OP("vector", "tensor_tensor", [f"pb{si % 2}", f"badd{bi}"], [dst_key], out=dst_ap, in0=pbk[:, 0:256], in1=badd[:, bi, :], op=ALU.add)

            plan([(("ada", si_), w_ada[:, si_ * 256:si_ * 256 + 256].rearrange("(k p) c -> p k c", p=128), 16, 256) for si_ in range(48)])
            for si in range(16):
                ada_slab(si, mod01[:, si * 256:(si + 1) * 256], f"mt{si // 8}")
            for j, (dst, dkey) in enumerate([(sh1, "sh1"), (sc1p, "sc1p")]):
                OP("vector", "tensor_tensor", [f"mt{j}", "cm"], ["mtmp"], out=mtmp[:], in0=mod01[:, j * D:(j + 1) * D].rearrange("p (c q) -> p c q", q=128),
                   in1=cm[:, 0:1, :].to_broadcast([128, 16, 128]), op=ALU.mult)
                OP("vector", "tensor_reduce", ["mtmp"], [dkey], out=dst[:], in_=mtmp[:], axis=AX.X, op=ALU.add)
            OP("vector", "tensor_scalar_add", ["sc1p"], ["sc1p"], out=sc1p[:], in0=sc1p[:], scalar1=1.0)
            if debug:
                LD("sync", dbg["modb"], mod01[:], [], ["mt0", "mt1"])
            order = {2: 0, 3: 1, 4: 2, 5: 3}
            for piece in range(2, 6):
                mt = mod01[:, (piece % 2) * D:(piece % 2 + 1) * D]
                mk = f"mt{piece % 2}"
                for s8 in range(8):
                    ada_slab(piece * 8 + s8, mt[:, s8 * 256:(s8 + 1) * 256], mk)
                if piece == 4:
                    OP("vector", "tensor_scalar_add", [mk], [mk], out=mt, in0=mt, scalar1=1.0)
                LD("sync", modb_d[order[piece]], mt, [], [mk])
            P.barrier()
        if stage == 0:
            st_p.close()
            P.emit(nc, st)
            return nc

        with ExitStack() as st_m:
            sbm = lambda n, s, d=F32: st_m.enter_context(nc.sbuf_tensor(n, s, d))
            hT = sbm("hT", [128, 16, 512], BF16)
            carry = sbm("carry", [128, 27])
            halo = sbm("halo", [128, 8, 16])
            dT = sbm("dT", [128, 8, 512], BF16); yrT = sbm("yrT", [128, 8, 512], BF16); mixT = sbm("mixT", [128, 16, 512], BF16)
            Hst = sbm("Hst", [128, 8, 128])
            import os
            if not os.environ.get("NO_MEMSET"):
                OP("vector", "memset", [], ["H%d" % i for i in range(8)], ap=Hst[:].rearrange("p a b -> p (a b)"), constant=0.0)
                OP("vector", "memset", [], ["carry"], ap=carry[:], constant=0.0)
                OP("vector", "memset", [], ["halo"], ap=halo[:].rearrange("p a b -> p (a b)"), constant=0.0)
            pslot_i = [0]

            def pslot():
                i = pslot_i[0] % 4
                pslot_i[0] += 1
                return pbank[2 + i][:, 0:128], f"pb{2 + i}"

            def in_proj(col0, ncols, ntok0, ntok, pbk, pkey):
                view, key = load_slab(w_in[:, col0:col0 + ncols].rearrange("(k p) c -> p k c", p=128), 16, ncols, tag=("win", col0, ncols))
                for k in range(16):
                    MM(pbk[0:ncols, 0:ntok], view[:, k, :], hT[:, k, ntok0:ntok0 + ntok], k == 0, k == 15, [key, "hT"], [pkey])

            scopes = []
            try:
                for blk in range(4):
                    own = blk >= 2
                    xsrc = xo if own else xp
                    t0 = (blk % 2) * 512
                    tcol = (blk - 2) * 512
                    st1 = ExitStack(); scopes.append(st1)
                    sb1 = lambda n, s_, d=F32: st1.enter_context(nc.sbuf_tensor(f"{n}_b{blk}", s_, d))
                    xs = [sb1(f"xs{i}", [128, D]) for i in range(1)]
                    pr = sb1("pr", [128, 8, 512], BF16); pk = sb1("pk", [128, 8, 512], BF16); pv = sb1("pv", [128, 8, 512], BF16)
                    plo = sb1("plo", [128, 3, 512])
                    upool = sb1("upool", [128, 528])
                    ptmp = [sb1(f"ptmp{i}", [128, 528]) for i in range(2)]
                    tnh = sb1("tnh", [128, 512], BF16); sgd = sb1("sgd", [128, 2, 512], BF16)
                    f_ld = sb1("f_ld", [128, 512]); f_ic = sb1("f_ic", [128, 512]); f_L = sb1("f_L", [128, 512])
                    f_t0 = sb1("f_t0", [128, 512]); f_t1 = sb1("f_t1", [128, 512]); f_t2 = sb1("f_t2", [128, 512])
                    f_kk = sb1("f_kk", [128, 512]); f_km = sb1("f_km", [128, 512]); f_bb = sb1("f_bb", [128, 512])
                    ynT = sb1("ynT", [128, 512])
                    LC = sb1("LC", [128, 4]); WC = sb1("WC", [128, 4])
                    o_r = sb1("o_r", [128, 512], BF16); o_a = sb1("o_a", [128, 512], BF16); o_b = sb1("o_b", [128, 512], BF16)
                    o_k = sb1("o_k", [128, 512], BF16); o_bh = sb1("o_bh", [128, 512], BF16); o_kh = sb1("o_kh", [128, 512], BF16)
                    rF = sb1("rF", [128, 512])
                    tm = sb1("tm", [128, 4, 4, 128], BF16)
                    bonus = sb1("bonus", [128, 512]); gT = sb1("gT", [128, 512])
                    uM = [sb1(f"uM{i}", [128, 2, 128]) for i in range(8)]
                    uN = [sb1(f"uN{i}", [128, 128]) for i in range(8)]
                    uNb = [sb1(f"uNb{i}", [128, 128], BF16) for i in range(4)]
                    uA = [sb1(f"uA{i}", [128, 3, 128], BF16) for i in range(4)]
                    uAt = [sb1(f"uAt{i}", [128, 128], BF16) for i in range(2)]; uX = [sb1(f"uX{i}", [128, 128], BF16) for i in range(2)]
                    uU = [sb1(f"uU{i}", [128, 128], BF16) for i in range(2)]
                    uPT = sb1("uPT", [128, 128]); uQ = sb1("uQ", [128, 128]); uRh = sb1("uRh", [128, 128])
                    uY = sb1("uY", [128, 128]); uZ = sb1("uZ", [128, 128]); ust = sb1("ust", [128, 2, 6]); uag = sb1("uag", [128, 2, 2])
                    ga, gb, t1, t2 = f_ld, f_ic, f_kk, f_km
                    glist = []
                    for j in range(8):
                        glist.append((j, G_R + 128 * j, 128, pr, j, False))
                    for j in range(8):
                        glist.append((8 + j, G_K + 128 * j, 128, pk, j, True))
                    for j in range(8):
                        glist.append((16 + j, G_V + 128 * j, 128, pv, j, True))
                    glist += [(24, G_WDAD, 128, plo, 0, True), (25, G_GD, 128, plo, 1, False), (26, G_GD + 128, 32, plo, 2, False)]

                    CP(f'pre{blk}')
                    wsl = lambda c0_, n_: w_in[:, c0_:c0_ + n_].rearrange("(k p) c -> p k c", p=128)
                    bp = []
                    for (gi_, col0_, ncols_, dst_, di_, fullp_) in glist:
                        if (not own) and (not fullp_) and blk != 1:
                            continue
                        bp.append((("win", col0_, ncols_), wsl(col0_, ncols_), 16, ncols_))
                    if (not own) and blk == 1:
                        bp += [(("win", G_POOL + 128 * j_, 128), wsl(G_POOL + 128 * j_, 128), 16, 128) for j_ in range(8)]
                    if own:
                        bp += [(("win", G_POOL + 128 * j_, 128), wsl(G_POOL + 128 * j_, 128), 16, 128) for j_ in range(8)]
                        for c_ in range(16):
                            bp.append((("win", G_GA + 128 * c_, 128), wsl(G_GA + 128 * c_, 128), 16, 128))
                            bp.append((("win", G_GB + 128 * c_, 128), wsl(G_GB + 128 * c_, 128), 16, 128))
                            bp.append((("wro", c_), w_ro[:, c_ * 128:(c_ + 1) * 128].rearrange("(k p) c -> p k c", p=128), 8, 128))
                            bp.append((("wpool", c_), w_pool[c_ // 4, :, (c_ % 4) * 128:(c_ % 4 + 1) * 128].rearrange("(k p) c -> p k c", p=128), 2, 128))
                    plan(bp)
                    for tt in range(4):
                        LD("sync", xs[0][:], xsrc[t0 + tt * 128:t0 + (tt + 1) * 128, :], ["xs0"])
                        for c in range(16):
                            TR(pbank[6][:, (c % 4) * 128:(c % 4 + 1) * 128], xs[0][:, c * 128:(c + 1) * 128], ident, ["xs0", "cm"], ["pb6"])
                            if c % 4 == 3:
                                for u_ in range(4):
                                    cc_ = c - 3 + u_
                                    OP("scalar", "activation", ["pb6", "sc1p", "sh1"], ["hT"], out=hT[:, cc_, tt * 128:(tt + 1) * 128], in_=pbank[6][:, u_ * 128:(u_ + 1) * 128],
                                       func=AF.Identity, scale=sc1p[:, cc_:cc_ + 1], bias=sh1[:, cc_:cc_ + 1])
                    CP(f'i{blk}')
                    for (gi, col0, ncols, dst, di, fullp) in glist:
                        pbk, pkey = pbank[gi % 2], f"pb{gi % 2}"
                        if not own and not fullp:
                            if blk == 1:
                                in_proj(col0, ncols, 496, 16, pbk, pkey)
                                OP("vector", "tensor_copy", [pkey], ["carry"], out=carry[0:ncols, gi:gi + 1], in_=pbk[0:ncols, 15:16])
                            continue
                        in_proj(col0, ncols, 0, 512, pbk, pkey)
                        dsl = dst[0:ncols, di, :]
                        OP("scalar", "activation", [pkey, "omu"], ["f_t0"], out=f_t0[0:ncols, :], in_=pbk[0:ncols, :], func=AF.Identity, scale=omu[0:ncols, gi:gi + 1])
                        OP("vector", "scalar_tensor_tensor", [pkey, "mu", "f_t0"], [f"p{gi}"], out=dsl[:, 1:512], in0=pbk[0:ncols, 0:511], scalar=mu[0:ncols, gi:gi + 1],
                           in1=f_t0[0:ncols, 1:512], op0=ALU.mult, op1=ALU.add)
                        OP("vector", "scalar_tensor_tensor", ["carry", "mu", "f_t0"], [f"p{gi}"], out=dsl[:, 0:1], in0=carry[0:ncols, gi:gi + 1], scalar=mu[0:ncols, gi:gi + 1],
                           in1=f_t0[0:ncols, 0:1], op0=ALU.mult, op1=ALU.add)
                        OP("vector", "tensor_copy", [pkey, "carry"], ["carry"], out=carry[0:ncols, gi:gi + 1], in_=pbk[0:ncols, 511:512])
                        if debug and own and dst is plo:
                            LD("sync", dbg["ps"][0:ncols, gi, tcol:tcol + 512], dsl, [], [f"p{gi}"])
                    if blk == 1:
                        OP("vector", "tensor_scalar_mul", ["carry", "flg"], ["carry"], out=carry[:], in0=carry[:], scalar1=flg[:, 0:1])
                    CP(f'ii{blk}')
                    OP("scalar", "activation", ["p24"], ["tnh"], out=tnh[0:64, :], in_=plo[0:64, 0, :], func=AF.Tanh)
                    OP("vector", "tensor_copy", ["p24"], ["tnh"], out=tnh[64:128, :], in_=plo[64:128, 0, :])
                    if own:
                        OP("scalar", "activation", ["p25"], ["sgd"], out=sgd[:, 0, :], in_=plo[:, 1, :], func=AF.Sigmoid)
                        OP("scalar", "activation", ["p26"], ["sgd"], out=sgd[0:32, 1, :], in_=plo[0:32, 2, :], func=AF.Sigmoid)
                    for hp in range(8):
                        cs = slice(hp * 128, (hp + 1) * 128)
                        V = lambda j: vec[:, j, hp:hp + 1]
                        pb6 = pbank[6]
                        kkey, vkey, rkey = f"p{8 + hp}", f"p{16 + hp}", f"p{hp}"
                        MM(pb6[:, :], wlo[0:64, 0, cs], tnh[0:64, :], True, True, ["wlo", "tnh"], ["pb6"])
                        OP("scalar", "activation", ["pb6", "vec"], ["f_ld"], out=f_ld[:], in_=pb6[:, :], func=AF.Sigmoid, bias=V(0))
                        MM(pb6[:, :], wlo[64:128, 0, cs], tnh[64:128, :], True, True, ["wlo", "tnh"], ["pb6"])
                        OP("scalar", "activation", ["pb6", "vec"], ["f_ic"], out=f_ic[:], in_=pb6[:, :], func=AF.Sigmoid, bias=V(1))
                        OP("vector", "tensor_scalar_mul", ["f_ld"], ["f_ld"], out=f_ld[:], in0=f_ld[:], scalar1=-0.6065306597126334)
                        OP("vector", "tensor_tensor_scan", ["seg", "f_ld"], ["f_L"], out=f_L[:], data0=seg[:], data1=f_ld[:], initial=0.0, op0=ALU.mult, op1=ALU.add)
                        OP("vector", "tensor_copy", ["f_L"], ["LC"], out=LC[:], in_=f_L[:].rearrange("p (c t) -> p c t", t=128)[:, :, 127])
                        OP("scalar", "activation", ["LC"], ["WC"], out=WC[:], in_=LC[:], func=AF.Exp)
                        OP("vector", "tensor_scalar_mul", [kkey, "vec"], ["f_kk"], out=f_kk[:], in0=pk[:, hp, :], scalar1=V(2))
                        OP("gpsimd", "tensor_tensor", ["f_kk"], ["f_t0"], out=f_t0[:], in0=f_kk[:], in1=f_kk[:], op=ALU.mult)
                        MM(pb6[:, :], m_bd, f_t0[:], True, True, ["cm", "f_t0"], ["pb6"])
                        OP("scalar", "activation", ["pb6"], ["f_t1"], out=f_t1[:], in_=pb6[:, :], func=AF.Sqrt)
                        OP("vector", "tensor_scalar_max", ["f_t1"], ["f_t1"], out=f_t1[:], in0=f_t1[:], scalar1=1e-12)
                        OP("vector", "reciprocal", ["f_t1"], ["f_t1"], out=f_t1[:], in_=f_t1[:])
                        OP("vector", "tensor_tensor", ["f_kk", "f_t1"], ["f_kk"], out=f_kk[:], in0=f_kk[:], in1=f_t1[:], op=ALU.mult)
                        OP("vector", "tensor_scalar", ["f_ic", "vec"], ["f_t2"], out=f_t2[:], in0=f_ic[:], scalar1=-1.0, scalar2=V(3), op0=ALU.add, op1=ALU.mult)
                        OP("vector", "scalar_tensor_tensor", ["f_t2", kkey], ["f_km"], out=f_km[:], in0=f_t2[:], scalar=1.0, in1=pk[:, hp, :], op0=ALU.add, op1=ALU.mult)
                        OP("gpsimd", "tensor_tensor", ["f_kk", "f_ic"], ["f_bb"], out=f_bb[:], in0=f_kk[:], in1=f_ic[:], op=ALU.mult)
                        OP("scalar", "activation", ["f_L"], ["f_t0"], out=f_t0[:], in_=f_L[:], func=AF.Exp)
                        if own:
                            OP("vector", "tensor_tensor", [rkey, "f_t0"], ["rF"], out=rF[:], in0=pr[:, hp, :], in1=f_t0[:], op=ALU.mult)
                            OP("gpsimd", "tensor_copy", ["rF"], ["o_r"], out=o_r[:], in_=rF[:])
                        OP("vector", "tensor_tensor", ["f_L", "f_ld"], ["f_t1"], out=f_t1[:], in0=f_L[:], in1=f_ld[:], op=ALU.subtract)
                        OP("scalar", "activation", ["f_t1"], ["f_t1"], out=f_t1[:], in_=f_t1[:], func=AF.Exp)
                        OP("vector", "scalar_tensor_tensor", ["f_kk", "f_t1"], ["o_a"], out=o_a[:], in0=f_kk[:], scalar=-1.0, in1=f_t1[:], op0=ALU.mult, op1=ALU.mult)
                        OP("scalar", "activation", ["f_L"], ["f_t2"], out=f_t2[:], in_=f_L[:], func=AF.Exp, scale=-1.0)
                        OP("vector", "tensor_tensor", ["f_bb", "f_t2"], ["o_b"], out=o_b[:], in0=f_bb[:], in1=f_t2[:], op=ALU.mult)
                        OP("gpsimd", "tensor_tensor", ["f_km", "f_t2"], ["o_k"], out=o_k[:], in0=f_km[:], in1=f_t2[:], op=ALU.mult)
                        for c in range(4):
                            OP("scalar", "activation", ["f_L", "LC"], ["f_t0"], out=f_t0[:, c * 128:(c + 1) * 128], in_=f_L[:, c * 128:(c + 1) * 128], func=AF.Exp, scale=-1.0, bias=LC[:, c:c + 1])
                        OP("vector", "tensor_tensor", ["f_bb", "f_t0"], ["o_bh"], out=o_bh[:], in0=f_bb[:], in1=f_t0[:], op=ALU.mult)
                        OP("gpsimd", "tensor_tensor", ["f_km", "f_t0"], ["o_kh"], out=o_kh[:], in0=f_km[:], in1=f_t0[:], op=ALU.mult)
                        CP(f'prep{blk}_{hp}')
                        if debug and own:
                            LD("sync", dbg["ld"][:, hp, tcol:tcol + 512], f_ld[:], [], ["f_ld"])
                        for c in range(4):
                            tc_ = slice(c * 128, (c + 1) * 128)
                            for wi, (src, skey) in enumerate([(o_a[:, tc_], "o_a"), (o_bh[:, tc_], "o_bh"), (o_kh[:, tc_], "o_kh"), (pv[:, hp, tc_], vkey)]):
                                TR(ptb[:, wi * 128:(wi + 1) * 128], src, ident_b, [skey, "cmb"], ["ptb"])
                            if c % 2:
                                OP("vector", "tensor_copy", ["ptb"], [f"tm{c}"], out=tm[:, c, :, :], in_=ptb[:, 0:512].rearrange("p (w q) -> p w q", q=128))
                            else:
                                OP("scalar", "activation", ["ptb"], [f"tm{c}"], out=tm[:, c, :, :], in_=ptb[:, 0:512].rearrange("p (w q) -> p w q", q=128), func=AF.Identity)
                        CP(f'tm{blk}_{hp}')
                        if own:
                            OP("vector", "scalar_tensor_tensor", [rkey, "vec", "f_km"], ["f_t1"], out=f_t1[:], in0=pr[:, hp, :], scalar=V(4), in1=f_km[:], op0=ALU.mult, op1=ALU.mult)
                            MM(pb6[:, :], m_bd, f_t1[:], True, True, ["cm", "f_t1"], ["pb6"])
                            OP("vector", "tensor_tensor", ["pb6", vkey], ["bonus"], out=bonus[:], in0=pb6[:, :], in1=pv[:, hp, :], op=ALU.mult)
                            MM(pb6[:, :], wlo[:, 1, cs], sgd[:, 0, :], True, False, ["wlo", "sgd"], ["pb6"])
                            MM(pb6[:, :], wlo[0:32, 2, cs], sgd[0:32, 1, :], False, True, ["wlo", "sgd"], ["pb6"])
                            OP("scalar", "activation", ["pb6"], ["gT"], out=gT[:], in_=pb6[:, :], func=AF.Identity)

                        def head_chain(h, c, q, cs):
                            tc_ = slice(c * 128, (c + 1) * 128)
                            tmk = f"tm{c}"
                            pbs = slice(h * 64, (h + 1) * 64)
                            bt, at, kt, rt = o_b[pbs, tc_], o_a[pbs, tc_], o_k[pbs, tc_], o_r[pbs, tc_]
                            M0, N0 = uM[2 * q], uN[2 * q]
                            mk0 = f"uM{2 * q}"
                            uAq, uAk = uA[q], f"uA{q}"
                            specs = [(bt, at, m_su, M0[:, 0, :], mk0, ["o_b", "o_a"]), (at, bt, m_sl, M0[:, 1, :], mk0, ["o_b", "o_a"]),
                                     (kt, at, m_su, uAq[:, 2, :], uAk, ["o_k", "o_a"])]
                            if own:
                                specs += [(bt, rt, m_ui, uAq[:, 0, :], uAk, ["o_b", "o_r"]), (kt, rt, m_ui, uAq[:, 1, :], uAk, ["o_k", "o_r"])]
                            for (l_, r_, msk, dst, dkey, rk) in specs:
                                ps_, pk_ = pslot()
                                MM(ps_, l_, r_, True, True, rk, [pk_])
                                OP("vector", "tensor_tensor", [pk_, "cm"], [dkey], out=dst, in0=ps_, in1=msk, op=ALU.mult)
                                yield
                            OP("gpsimd", "tensor_tensor", [mk0, "cm"], [f"uN{2 * q}"], out=N0[:], in0=M0[:, 0, :], in1=ident, op=ALU.add)
                            cur, curk, Nc, Nk = M0, mk0, N0, f"uN{2 * q}"
                            for j in range(1, 7):
                                nxt, nxtk = uM[2 * q + (j % 2)], f"uM{2 * q + (j % 2)}"
                                Nn, Nnk = uN[2 * q + (j % 2)], f"uN{2 * q + (j % 2)}"
                                rk = [curk, curk + "a", curk + "b"]
                                if j < 6:
                                    ps_, pk_ = pslot()
                                    MM(ps_, cur[:, 1, :], cur[:, 0, :], True, True, rk, [pk_])
                                    OP("scalar", "activation", [pk_], [nxtk + "a"], out=nxt[:, 0, :], in_=ps_, func=AF.Identity)
                                ps2, pk2 = pslot()
                                MM(ps2, cur[:, 0, :], cur[:, 1, :], True, True, rk, [pk2])
                                OP("scalar", "activation", [pk2], [nxtk + "b"], out=nxt[:, 1, :], in_=ps2, func=AF.Identity)
                                yield
                                ps3, pk3 = pslot()
                                MM(ps3, nxt[:, 1, :], Nc[:], True, True, [nxtk + "b", Nk], [pk3])
                                OP("vector", "tensor_tensor", [pk3, Nk], [Nnk], out=Nn[:], in0=ps3, in1=Nc[:], op=ALU.add)
                                cur, curk, Nc, Nk = nxt, nxtk, Nn, Nnk
                                yield
                            OP("gpsimd", "tensor_copy", [Nk], [f"uNb{q}"], out=uNb[q][:], in_=Nc[:])
                            ps_, pk_ = pslot()
                            MM(ps_[:, 0:64], uNb[q][:], tm[:, c, 0, pbs], True, True, [f"uNb{q}", tmk], [pk_])
                            OP("vector", "tensor_copy", [pk_], [f"uAt{cs}_{h}"], out=uAt[cs][:, pbs], in_=ps_[:, 0:64])
                            ps_, pk_ = pslot()
                            MM(ps_[:, 0:64], uAq[:, 2, :], tm[:, c, 3, pbs], True, True, [uAk, tmk], [pk_])
                            OP("scalar", "activation", [pk_], [f"uX{cs}_{h}"], out=uX[cs][:, pbs], in_=ps_[:, 0:64], func=AF.Identity)
                            yield
                            ps_, pk_ = pslot()
                            MM(ps_[:, 0:64], uNb[q][:], uX[cs][:, pbs], True, True, [f"uNb{q}", f"uX{cs}_{h}"], [pk_])
                            OP("vector", "tensor_copy", [pk_], [f"uU{cs}_{h}"], out=uU[cs][:, pbs], in_=ps_[:, 0:64])
                            yield

                        def pair_level(c, cs):
                            tc_ = slice(c * 128, (c + 1) * 128)
                            tmk = f"tm{c}"
                            atk = [f"uAt{cs}_0", f"uAt{cs}_1"]
                            uuk = [f"uU{cs}_0", f"uU{cs}_1"]
                            ps_, pk_ = pslot()
                            MM(ps_, uAt[cs][:], tm[:, c, 1, :], True, True, atk + [tmk], [pk_])
                            OP("vector", "tensor_tensor", [pk_, "cm"], ["uPT"], out=uPT[:], in0=ps_, in1=m_bd, op=ALU.mult)
                            OP("vector", "scalar_tensor_tensor", ["uPT", "cm", "WC"], ["uPT"], out=uPT[:], in0=ident, scalar=WC[:, c:c + 1], in1=uPT[:], op0=ALU.mult, op1=ALU.add)
                            ps_, pk_ = pslot()
                            MM(ps_, tm[:, c, 1, :], uU[cs][:], True, False, uuk + [tmk], [pk_])
                            MM(ps_, tm[:, c, 2, :], tm[:, c, 3, :], False, True, [tmk], [pk_])
                            OP("vector", "tensor_tensor", [pk_, "cm"], ["uQ"], out=uQ[:], in0=ps_, in1=m_bd, op=ALU.mult)
                            if own:
                                for h in range(2):
                                    pbs = slice(h * 64, (h + 1) * 64)
                                    q = 2 * cs + h
                                    ps_, pk_ = pslot()
                                    MM(ps_, uAt[cs][:], uA[q][:, 0, :], True, True, atk + [f"uA{q}"], [pk_])
                                    OP("vector", "tensor_tensor", [pk_, "rF"], [f"uRh{h}"], out=uRh[pbs, :], in0=ps_[pbs, :], in1=rF[pbs, tc_], op=ALU.add)
                                psy, pky = pslot()
                                MM(psy, uRh[:], Hst[:, hp, :], True, True, ["uRh0", "uRh1", f"H{hp}"], [pky])
                                ps0, pk0 = pslot()
                                for h in range(2):
                                    pbs = slice(h * 64, (h + 1) * 64)
                                    q = 2 * cs + h
                                    MM(ps0[:, pbs], uA[q][:, 0, :], uU[cs][:, pbs], True, False, [f"uA{q}", f"uU{cs}_{h}"], [pk0])
                                    MM(ps0[:, pbs], uA[q][:, 1, :], tm[:, c, 3, pbs], False, True, [f"uA{q}", tmk], [pk0])
                                OP("scalar", "activation", [pk0], ["uY"], out=uY[:], in_=ps0, func=AF.Identity)
                                OP("vector", "tensor_tensor", [pky, "uY"], ["uY"], out=uY[:], in0=psy, in1=uY[:], op=ALU.add)
                                for h in range(2):
                                    pbs = slice(h * 64, (h + 1) * 64)
                                    OP("vector", "bn_stats", ["uY"], [f"ust{h}"], out=ust[:, h, :], in_=uY[:, pbs])
                                    OP("vector", "bn_aggr", [f"ust{h}"], [f"uag{h}"], out=uag[:, h, :], in_=ust[:, h, :])
                                    OP("scalar", "activation", [f"uag{h}"], [f"uag{h}"], out=uag[:, h, 1:2], in_=uag[:, h, 1:2], func=AF.Sqrt, bias=epsG[:, 0:1])
                                    OP("vector", "reciprocal", [f"uag{h}"], [f"uag{h}"], out=uag[:, h, 1:2], in_=uag[:, h, 1:2])
                                    OP("vector", "tensor_scalar", ["uY", f"uag{h}"], [f"uZ{h}"], out=uZ[:, pbs], in0=uY[:, pbs], scalar1=uag[:, h, 0:1], scalar2=uag[:, h, 1:2],
                                       op0=ALU.subtract, op1=ALU.mult)
                                psz, pkz = pslot()
                                TR(psz, uZ[:], ident, ["uZ0", "uZ1", "cm"], [pkz])
                                OP("scalar", "activation", [pkz, "vec"], ["ynT"], out=ynT[:, tc_], in_=psz, func=AF.Identity, scale=vec[:, 5, hp:hp + 1], bias=vec[:, 6, hp:hp + 1])
                            psh, pkh = pslot()
                            MM(psh, uPT[:], Hst[:, hp, :], True, True, ["uPT", f"H{hp}"], [pkh])
                            OP("vector", "tensor_tensor", [pkh, "uQ"], [f"H{hp}"], out=Hst[:, hp, :], in0=psh, in1=uQ[:], op=ALU.add)
                            if blk == 1 and c == 3:
                                OP("vector", "tensor_scalar_mul", [f"H{hp}", "flg"], [f"H{hp}"], out=Hst[:, hp, :], in0=Hst[:, hp, :], scalar1=flg[:, 0:1])

                        for cp_ in (0, 2):
                            chains = [head_chain(h, cp_ + cs, 2 * cs + h, cs) for cs in range(2) for h in range(2)]
                            live = list(chains)
                            while live:
                                for g_ in list(live):
                                    try:
                                        next(g_)
                                    except StopIteration:
                                        live.remove(g_)
                            pair_level(cp_, 0)
                            pair_level(cp_ + 1, 1)

                        if own:
                            if debug:
                                LD("sync", dbg["y"][:, hp, tcol:tcol + 512], ynT[:], [], ["ynT"])
                            OP("vector", "tensor_tensor", ["ynT", "bonus"], ["ynT"], out=ynT[:], in0=ynT[:], in1=bonus[:], op=ALU.add)
                            OP("vector", "tensor_tensor", ["ynT", "gT"], ["yrT"], out=yrT[:, hp, :], in0=ynT[:], in1=gT[:], op=ALU.mult)
                            if debug:
                                OP("vector", "tensor_tensor", ["ynT", "gT"], ["ynT"], out=ynT[:], in0=ynT[:], in1=gT[:], op=ALU.mult)
                                LD("sync", dbg["yr"][:, hp, tcol:tcol + 512], ynT[:], [], ["ynT"])
                    if not own:
                        if blk == 1:
                            for j in range(8):
                                pbk, pkey = pbank[j % 2], f"pb{j % 2}"
                                in_proj(G_POOL + 128 * j, 128, 496, 16, pbk, pkey)
                                OP("vector", "tensor_scalar_mul", [pkey, "flg"], ["halo"], out=halo[:, j, :], in0=pbk[:, 0:16], scalar1=flg[:, 0:1])
                        P.barrier()
                        st1.close()
                        continue
                    CP(f'rwkv{blk}')
                    for j in range(8):
                        g = j // 2
                        W = (2, 4, 8, 16)[g]
                        pbk, pkey = pbank[j % 2], f"pb{j % 2}"
                        in_proj(G_POOL + 128 * j, 128, 0, 512, pbk, pkey)
                        OP("scalar", "activation", [pkey], ["upool"], out=upool[:, 16:528], in_=pbk[:, :], func=AF.Identity)
                        OP("gpsimd", "tensor_copy", ["halo"], ["upool"], out=upool[:, 0:16], in_=halo[:, j, :])
                        src, skey = upool[:, :], "upool"
                        step, pi = 1, 0
                        while step < W:
                            dstt = ptmp[pi]
                            OP("gpsimd", "tensor_tensor", [skey], [f"ptmp{pi}"], out=dstt[:, step:528], in0=src[:, step:528], in1=src[:, 0:528 - step], op=ALU.add)
                            OP("gpsimd", "tensor_copy", [skey], [f"ptmp{pi}"], out=dstt[:, 0:step], in_=src[:, 0:step])
                            src, skey = dstt[:, :], f"ptmp{pi}"
                            pi ^= 1
                            step *= 2
                        OP("vector", "scalar_tensor_tensor", [skey, "upool"], ["dT"], out=dT[:, j, :], in0=src[:, 16:528], scalar=1.0 / W, in1=upool[:, 16:528], op0=ALU.mult, op1=ALU.subtract)
                        if blk == 2:
                            OP("vector", "tensor_tensor", [skey, "pcr"], ["f_kk"], out=t1[:, 0:16], in0=src[:, 16:32], in1=pcr[:, g, :], op=ALU.mult)
                            OP("vector", "tensor_tensor", ["f_kk", "upool"], ["dT"], out=dT[:, j, 0:16], in0=t1[:, 0:16], in1=upool[:, 16:32], op=ALU.subtract)
                        OP("gpsimd", "tensor_copy", ["upool"], ["halo"], out=halo[:, j, :], in_=upool[:, 512:528])
                    CP(f'pool{blk}')
                    for c in range(16):
                        in_proj(G_GA + 128 * c, 128, 0, 512, pbank[0], "pb0")
                        OP("scalar", "activation", ["pb0"], ["f_ld"], out=ga[:], in_=pbank[0][:, :], func=AF.Sigmoid)
                        in_proj(G_GB + 128 * c, 128, 0, 512, pbank[1], "pb1")
                        OP("scalar", "activation", ["pb1"], ["f_ic"], out=gb[:], in_=pbank[1][:, :], func=AF.Sigmoid)
                        vro, kro = load_slab(w_ro[:, c * 128:(c + 1) * 128].rearrange("(k p) c -> p k c", p=128), 8, 128, tag=("wro", c))
                        for k in range(8):
                            MM(pbank[0][:, :], vro[:, k, :], yrT[:, k, :], k == 0, k == 7, [kro, "yrT"], ["pb0"])
                        g = c // 4
                        vpo, kpo = load_slab(w_pool[g, :, (c % 4) * 128:(c % 4 + 1) * 128].rearrange("(k p) c -> p k c", p=128), 2, 128, tag=("wpool", c))
                        for k in range(2):
                            MM(pbank[1][:, :], vpo[:, k, :], dT[:, 2 * g + k, :], k == 0, k == 1, [kpo, "dT"], ["pb1"])
                        OP("vector", "tensor_tensor", ["pb0", "f_ld"], ["f_kk"], out=t1[:], in0=pbank[0][:, :], in1=ga[:], op=ALU.mult)
                        OP("vector", "scalar_tensor_tensor", ["pb1", "psc", "f_ic"], ["f_km"], out=t2[:], in0=pbank[1][:, :], scalar=psc[:, c:c + 1], in1=gb[:], op0=ALU.mult, op1=ALU.mult)
                        OP("gpsimd", "tensor_tensor", ["f_kk", "f_km"], ["mixT"], out=mixT[:, c, :], in0=t1[:], in1=t2[:], op=ALU.add)
                        if debug:
                            OP("gpsimd", "tensor_tensor", ["f_kk", "f_km"], ["f_ld"], out=ga[:], in0=t1[:], in1=t2[:], op=ALU.add)
                            LD("sync", dbg["mixin"][:, c, tcol:tcol + 512], ga[:], [], ["f_ld"])
                    CP(f'comb{blk}')
                    P.barrier()
                    st1.close()
                    st2 = ExitStack(); scopes.append(st2)
                    sb2 = lambda n, s_, d=F32: st2.enter_context(nc.sbuf_tensor(f"{n}_c{blk}", s_, d))
                    xs = [sb2(f"xs{i}", [128, D]) for i in range(2)]
                    x1p4 = sb2("x1p4", [128, 4, D]); lnst = sb2("lnst", [128, 4, 6]); lnag = sb2("lnag", [128, 2]); h2t = sb2("h2t", [128, D], BF16)
                    lnw = sb2("lnw", [128, 2, D]); modb = sb2("modb", [128, 3, D])
                    LD("sync", lnw[:, 0, :], lnb[0], ["lnw"]); LD("sync", lnw[:, 1, :], lnb[1], ["lnw"])
                    for k_ in range(3):
                        LD("sync", modb[:, k_, :], modb_d[k_], ["modb"])
                    if stage >= 2:
                        h2T = sb2("h2T", [128, 16, 128]); wr = sb2("wr", [128, 16, NEXP]); brb = sb2("brb", [128, NEXP]); lgt = sb2("lgt", [128, NEXP])
                        LD("sync", wr[:], w_router.rearrange("(k p) e -> p k e", p=128), ["wr"])
                        LD("sync", brb[:], b_router_b, ["brb"])
                    plan([(("wout", 0, s8_), w_out[:, s8_ * 256:(s8_ + 1) * 256].rearrange("(k p) c -> p k c", p=128), 16, 256) for s8_ in range(8)])
                    for s8 in range(8):
                        vwo, kwo = load_slab(w_out[:, s8 * 256:(s8 + 1) * 256].rearrange("(k p) c -> p k c", p=128), 16, 256, tag=("wout", 0, s8))
                        dsl = slice(s8 * 256, (s8 + 1) * 256)
                        for tt in range(4):
                            pbk, pkey = pbank[tt % 2], f"pb{tt % 2}"
                            for k in range(16):
                                MM(pbk[:, 0:256], mixT[:, k, tt * 128:(tt + 1) * 128], vwo[:, k, :], k == 0, k == 15, [kwo, "mixT"], [pkey])
                            OP("vector", "tensor_tensor", [pkey, "modb"], [f"x1p{tt}"], out=x1p4[:, tt, dsl], in0=pbk[:, 0:256], in1=modb[:, 0, dsl], op=ALU.mult)
                    for tt in range(4):
                        x1p = x1p4[:, tt, :]
                        LD("sync", xs[0][:], xo[tcol + tt * 128:tcol + (tt + 1) * 128, :], ["xs0"])
                        OP("gpsimd", "tensor_scalar_mul", ["xs0"], ["xs0"], out=xs[0][:], in0=xs[0][:], scalar1=ALPHA)
                        OP("gpsimd", "tensor_tensor", [f"x1p{tt}", "xs0"], ["x1p"], out=x1p, in0=x1p, in1=xs[0][:], op=ALU.add)
                        for q in range(4):
                            OP("vector", "bn_stats", ["x1p"], ["lnst"], out=lnst[:, q, :], in_=x1p[:, q * 512:(q + 1) * 512])
                        OP("vector", "bn_aggr", ["lnst"], ["lnag"], out=lnag[:], in_=lnst[:].rearrange("p a b -> p (a b)"))
                        OP("scalar", "activation", ["lnag"], ["lnag"], out=lnag[:, 1:2], in_=lnag[:, 1:2], func=AF.Sqrt, bias=epsL[:, 0:1])
                        OP("vector", "reciprocal", ["lnag"], ["lnag"], out=lnag[:, 1:2], in_=lnag[:, 1:2])
                        OP("vector", "tensor_scalar", ["x1p", "lnag"], ["x1p"], out=x1p, in0=x1p, scalar1=lnag[:, 0:1], scalar2=lnag[:, 1:2], op0=ALU.subtract, op1=ALU.mult)
                        OP("gpsimd", "tensor_tensor", ["x1p", "lnw"], ["x1p"], out=x1p, in0=x1p, in1=lnw[:, 0, :], op=ALU.mult)
                        OP("vector", "tensor_tensor", ["x1p", "lnw"], ["x1p"], out=x1p, in0=x1p, in1=lnw[:, 1, :], op=ALU.add)
                        LD("sync", x1_d[tcol + tt * 128:tcol + (tt + 1) * 128, :], x1p, [], ["x1p"])
                        if debug:
                            LD("sync", dbg["x1"][tcol + tt * 128:tcol + (tt + 1) * 128, :], x1p, [], ["x1p"])
                        OP("gpsimd", "tensor_tensor", ["x1p", "modb"], ["xs1"], out=xs[1][:], in0=x1p, in1=modb[:, 2, :], op=ALU.mult)
                        OP("vector", "tensor_tensor", ["xs1", "modb"], ["h2t"], out=h2t[:], in0=xs[1][:], in1=modb[:, 1, :], op=ALU.add)
                        if stage >= 2:
                            OP("gpsimd", "tensor_tensor", ["xs1", "modb"], ["xs1"], out=xs[1][:], in0=xs[1][:], in1=modb[:, 1, :], op=ALU.add)
                            for q4 in range(4):
                                for u_ in range(4):
                                    c_ = q4 * 4 + u_
                                    TR(pbank[6][:, u_ * 128:(u_ + 1) * 128], xs[1][:, c_ * 128:(c_ + 1) * 128], ident, ["xs1", "cm"], ["pb6"])
                                OP("scalar", "activation", ["pb6"], ["h2T"], out=h2T[:, q4 * 4:(q4 + 1) * 4, :], in_=pbank[6][:, :].rearrange("p (u q) -> p u q", q=128), func=AF.Identity)
                            for c_ in range(16):
                                MM(pbank[2][:, 0:NEXP], h2T[:, c_, :], wr[:, c_, :], c_ == 0, c_ == 15, ["h2T", "wr"], ["pb2"])
                            OP("vector", "tensor_tensor", ["pb2", "brb"], ["lgt"], out=lgt[:], in0=pbank[2][:, 0:NEXP], in1=brb[:], op=ALU.add)
                            LD("sync", lg_d[tcol + tt * 128:tcol + (tt + 1) * 128, :], lgt[:], [], ["lgt"])
                        LD("sync", h2_d[tcol + tt * 128:tcol + (tt + 1) * 128, :], h2t[:], [], ["h2t"])
                    P.barrier()
                    st2.close()
            except _Stop:
                P.barrier()
                for sc_ in reversed(scopes):
                    sc_.close()
            P.barrier()
        if stage == 1:
            st_p.close()
            with ExitStack() as st_o:
                ot = st_o.enter_context(nc.sbuf_tensor("ot", [128, D], F32))
                OP("gpsimd", "memset", [], ["ot"], ap=ot[:], constant=0.0)
                for tt in range(8):
                    LD("sync", out[tt * 128:(tt + 1) * 128, :], ot[:], [], ["ot"])
                P.barrier()
            P.emit(nc, st)
            return nc
        st_p.close()
        moe_phase(nc, P, st, dict(OP=OP, MM=MM, TR=TR, LD=LD, slab=slab, NSL=NSL, pbank=pbank, ptb=ptb, cm=cm, cmb=cmb, epsL=epsL,
                                  h2_d=h2_d, x1_d=x1_d, lg_d=lg_d, modb_d=modb_d, lnb=lnb, w_gu=w_gu, b_guT=b_guT, w_down=w_down, b_down=b_down,
                                  iota_cap=iota_cap, out=out, debug=debug, dbg=dbg, n_exp=n_exp))
        P.emit(nc, st)
    return nc


def moe_phase(nc, P, st, env):
    OP, MM, TR, LD = env["OP"], env["MM"], env["TR"], env["LD"]
    slab, NSL, pbank, ptb, cm, cmb, epsL = env["slab"], env["NSL"], env["pbank"], env["ptb"], env["cm"], env["cmb"], env["epsL"]
    h2_d, x1_d, lg_d, modb_d, lnb = env["h2_d"], env["x1_d"], env["lg_d"], env["modb_d"], env["lnb"]
    w_gu, b_guT, w_down, b_down, iota_cap, out = env["w_gu"], env["b_guT"], env["w_down"], env["b_down"], env["iota_cap"], env["out"]
    debug, dbg, n_exp = env["debug"], env["dbg"], env["n_exp"]
    ident, m_su, ones_f = cm[:, 0, :], cm[:, 1, :], cm[:, 5, :]
    ident_b = cmb[:, 0, :]
    NT = T_OWN // 128
    NR = CAP // 128
    with ExitStack() as sm:
        sb = lambda n, s_, d=F32: sm.enter_context(nc.sbuf_tensor(n, s_, d))
        h2 = sb("h2", [128, NT, D], BF16)
        acc = sb("acc", [128, NT, D])
        iot = sb("iot", [128, CAP])
        lg = sb("lg", [128, NT, NEXP]); pm = sb("pm", [128, NT, NEXP]); pos = sb("pos", [128, NT, NEXP]); msk = sb("msk", [128, NT, NEXP])
        mx8 = sb("mx8", [128, 8]); ssum = sb("ssum", [128, 1]); nmx = sb("nmx", [128, 1])
        bgu = [sb(f"bgu{i}", [128, 32]) for i in range(2)]
        bdn = sb("bdn", [32, D], BF16)
        pmT = sb("pmT", [32, T_OWN], BF16)
        sm2 = ExitStack()
        sb2 = lambda n, s_, d=F32: sm2.enter_context(nc.sbuf_tensor(n, s_, d))
        XT = sb2("XT", [128, 16, CAP], BF16)
        actT = sb2("actT", [128, 16, CAP], BF16)
        Sel = sb2("Sel", [128, NT, CAP], BF16)
        SelW = [sb2(f"SelW{i}", [128, CAP], BF16) for i in range(2)]
        SelP = sb2("SelP", [128, NR, T_OWN], BF16)
        Yb = [sb2(f"Yb{i}", [128, NR, 256], BF16) for i in range(2)]
        gc = sb2("gc", [128, CAP]); sg = sb2("sg", [128, CAP]); u1 = sb2("u1", [128, CAP])

        for i in range(NT):
            LD("sync", h2[:, i, :], h2_d[i * 128:(i + 1) * 128, :], [f"h2_{i}"])
        LD("sync", lg[:], lg_d.rearrange("(i p) e -> p i e", p=128), ["lg"])
        LD("sync", iot[:], iota_cap, ["iot"])
        LD("gpsimd", bdn[:], b_down, ["bdn"])
        for i in range(NT):
            OP("gpsimd", "memset", [], [f"acc{i}"], ap=acc[:, i, :], constant=0.0)
        for i in range(NT):
            OP("vector", "max", ["lg"], ["mx8"], out=mx8[:], in_=lg[:, i, :])
            OP("vector", "tensor_scalar", ["lg", "mx8"], ["msk"], out=msk[:, i, :], in0=lg[:, i, :], scalar1=mx8[:, 3:4], scalar2=None, op0=ALU.is_ge)
            OP("vector", "tensor_scalar_mul", ["mx8"], ["nmx"], out=nmx[:], in0=mx8[:, 0:1], scalar1=-1.0)
            OP("scalar", "activation", ["lg", "nmx"], ["pm"], out=pm[:, i, :], in_=lg[:, i, :], func=AF.Exp, bias=nmx[:, 0:1], scale=1.0)
            OP("vector", "tensor_tensor", ["pm", "msk"], ["pm"], out=pm[:, i, :], in0=pm[:, i, :], in1=msk[:, i, :], op=ALU.mult)
            OP("vector", "tensor_reduce", ["pm"], ["ssum"], out=ssum[:], in_=pm[:, i, :], axis=mybir.AxisListType.X, op=ALU.add)
            OP("vector", "reciprocal", ["ssum"], ["ssum"], out=ssum[:], in_=ssum[:])
            OP("vector", "tensor_scalar_mul", ["pm", "ssum"], ["pm"], out=pm[:, i, :], in0=pm[:, i, :], scalar1=ssum[:, 0:1])
        for i in range(NT):
            pb = pbank[2 + (i % 2)]
            pkey = f"pb{2 + (i % 2)}"
            MM(pb[:, 0:NEXP], m_su, msk[:, i, :], True, i == 0, ["cm", "msk"], [pkey])
            for i2 in range(i):
                MM(pb[:, 0:NEXP], ones_f, msk[:, i2, :], False, i2 == i - 1, ["cm", "msk"], [pkey])
            OP("vector", "tensor_tensor", [pkey, "msk"], ["pos"], out=pos[:, i, :], in0=pb[:, 0:NEXP], in1=msk[:, i, :], op=ALU.mult)
            OP("vector", "tensor_tensor", ["pos", "msk"], ["pos"], out=pos[:, i, :], in0=pos[:, i, :], in1=msk[:, i, :], op=ALU.add)
            OP("vector", "tensor_scalar_add", ["pos"], ["pos"], out=pos[:, i, :], in0=pos[:, i, :], scalar1=-1.0)
            OP("vector", "tensor_copy", ["pm"], ["SelW0"], out=SelW[0][:, 0:NEXP], in_=pm[:, i, :])
            TR(ptb[0:NEXP, 0:128], SelW[0][:, 0:NEXP], ident_b, ["SelW0", "cmb"], ["ptb"])
            OP("vector", "tensor_copy", ["ptb"], ["pmT"], out=pmT[:, i * 128:(i + 1) * 128], in_=ptb[0:NEXP, 0:128])
        if debug:
            LD("sync", dbg["pm"].rearrange("(i p) e -> p i e", p=128), pm[:], [], ["pm"])
            LD("sync", dbg["pos"].rearrange("(i p) e -> p i e", p=128), pos[:], [], ["pos"])

        slabs = []
        for e in range(n_exp):
            for j in range(16):
                slabs.append(w_gu[e, :, 256 * j:256 * (j + 1)].rearrange("(k p) c -> p k c", p=128))
            for s8 in range(8):
                slabs.append(w_down[e, :, 256 * s8:256 * (s8 + 1)].rearrange("(k p) c -> p k c", p=128))
        issued = [0]

        def get_slab(idx, look=2):
            while issued[0] <= min(idx + look, len(slabs) - 1):
                k_ = issued[0]
                b_ = k_ % NSL
                view = slab[b_][:, 0:4096].rearrange("p (k c) -> p k c", k=16)
                LD("gpsimd", view, slabs[k_], [f"slab{b_}"])
                issued[0] += 1
            b_ = idx % NSL
            return slab[b_][:, 0:4096].rearrange("p (k c) -> p k c", k=16), f"slab{b_}"

        sidx = 0
        for e in range(n_exp):
            bg = bgu[e % 2]
            bgk = f"bgu{e % 2}"
            LD("sync", bg[:], b_guT[e], [bgk])
            for i in range(NT):
                OP("vector", "tensor_scalar", ["iot", "pos"], [f"Sel{i}"], out=Sel[:, i, :], in0=iot[:], scalar1=pos[:, i, e:e + 1], scalar2=None, op0=ALU.is_equal)
                sw, swk = SelW[i % 2], f"SelW{i % 2}"
                OP("vector", "tensor_scalar", ["iot", "pos", "pm"], [swk], out=sw[:], in0=iot[:], scalar1=pos[:, i, e:e + 1], scalar2=pm[:, i, e:e + 1], op0=ALU.is_equal, op1=ALU.mult)
                for r in range(NR):
                    TR(ptb[:, r * 128:(r + 1) * 128], sw[:, r * 128:(r + 1) * 128], ident_b, [swk, "cmb"], ["ptb"])
                OP("scalar", "activation", ["ptb"], [f"SelP{i}"], out=SelP[:, :, i * 128:(i + 1) * 128], in_=ptb[:, 0:NR * 128].rearrange("p (r q) -> p r q", q=128), func=AF.Identity)
            for c in range(16):
                pb, pkey = pbank[c % 2], f"pb{c % 2}"
                for i in range(NT):
                    MM(pb[:, 0:CAP], h2[:, i, c * 128:(c + 1) * 128], Sel[:, i, :], i == 0, i == NT - 1, [f"h2_{i}", f"Sel{i}"], [pkey])
                OP("scalar", "activation", [pkey], [f"XT{c}"], out=XT[:, c, :], in_=pb[:, 0:CAP], func=AF.Identity)
            xkeys = [f"XT{c}" for c in range(16)]
            for j in range(16):
                view, skey = get_slab(sidx); sidx += 1
                for k in range(16):
                    MM(pbank[2][:, 0:CAP], view[:, k, 0:256:2], XT[:, k, :], k == 0, k == 15, [skey, f"XT{k}"], ["pb2"])
                for k in range(16):
                    MM(pbank[3][:, 0:CAP], view[:, k, 1:256:2], XT[:, k, :], k == 0, k == 15, [skey, f"XT{k}"], ["pb3"])
                OP("vector", "tensor_scalar", ["pb2", bgk], ["gc"], out=gc[:], in0=pbank[2][:, 0:CAP], scalar1=bg[:, j:j + 1], scalar2=7.0, op0=ALU.add, op1=ALU.min)
                OP("scalar", "activation", ["gc"], ["sg"], out=sg[:], in_=gc[:], func=AF.Sigmoid, scale=1.702)
                OP("vector", "tensor_scalar", ["pb3", bgk], ["u1"], out=u1[:], in0=pbank[3][:, 0:CAP], scalar1=bg[:, 16 + j:17 + j], scalar2=7.0, op0=ALU.add, op1=ALU.min)
                OP("vector", "tensor_scalar", ["u1"], ["u1"], out=u1[:], in0=u1[:], scalar1=-7.0, scalar2=1.0, op0=ALU.max, op1=ALU.add)
                OP("vector", "tensor_tensor", ["gc", "sg"], ["sg"], out=sg[:], in0=gc[:], in1=sg[:], op=ALU.mult)
                OP("vector", "tensor_tensor", ["u1", "sg"], [f"act{j}"], out=actT[:, j, :], in0=u1[:], in1=sg[:], op=ALU.mult)
            akeys = [f"act{j}" for j in range(16)]
            for s8 in range(8):
                view, skey = get_slab(sidx); sidx += 1
                yb, ybk = Yb[s8 % 2], f"Yb{s8 % 2}"
                dsl = slice(s8 * 256, (s8 + 1) * 256)
                for r in range(NR):
                    pb, pkey = pbank[r % 2], f"pb{r % 2}"
                    for j in range(16):
                        MM(pb[:, 0:256], actT[:, j, r * 128:(r + 1) * 128], view[:, j, :], j == 0, j == 15, [skey, f"act{j}"], [pkey])
                    OP("scalar", "activation", [pkey], [ybk], out=yb[:, r, :], in_=pb[:, 0:256], func=AF.Identity)
                for i in range(NT):
                    pb, pkey = pbank[4 + (i % 2)], f"pb{4 + (i % 2)}"
                    for r in range(NR):
                        MM(pb[:, 0:256], SelP[:, r, i * 128:(i + 1) * 128], yb[:, r, :], r == 0, r == NR - 1, [f"SelP{i}", ybk], [pkey])
                    OP("vector", "tensor_tensor", [pkey, f"acc{i}"], [f"acc{i}"], out=acc[:, i, dsl], in0=pb[:, 0:256], in1=acc[:, i, dsl], op=ALU.add)
        P.barrier()
        sm2.close()
        for i in range(NT):
            for s8 in range(8):
                dsl = slice(s8 * 256, (s8 + 1) * 256)
                pb, pkey = pbank[4 + (s8 % 2)], f"pb{4 + (s8 % 2)}"
                MM(pb[:, 0:256], pmT[0:n_exp, i * 128:(i + 1) * 128], bdn[0:n_exp, dsl], True, True, ["pmT", "bdn"], [pkey])
                OP("vector", "tensor_tensor", [pkey, f"acc{i}"], [f"acc{i}"], out=acc[:, i, dsl], in0=pb[:, 0:256], in1=acc[:, i, dsl], op=ALU.add)
        g2b = sb("g2b", [128, D]); l2g = sb("l2g", [128, D]); l2b = sb("l2b", [128, D])
        xt = [sb(f"xt{i}", [128, D]) for i in range(2)]
        st6 = sb("st6", [128, 4, 6]); ag = sb("ag", [128, 2])
        LD("sync", g2b[:], modb_d[3], ["g2b"]); LD("sync", l2g[:], lnb[2], ["l2g"]); LD("sync", l2b[:], lnb[3], ["l2b"])
        for i in range(NT):
            x_, xk = xt[i % 2], f"xt{i % 2}"
            ak = f"acc{i}"
            LD("sync", x_[:], x1_d[i * 128:(i + 1) * 128, :], [xk])
            OP("gpsimd", "tensor_tensor", [ak, "g2b"], [ak], out=acc[:, i, :], in0=acc[:, i, :], in1=g2b[:], op=ALU.mult)
            OP("vector", "scalar_tensor_tensor", [xk, ak], [ak], out=acc[:, i, :], in0=x_[:], scalar=ALPHA, in1=acc[:, i, :], op0=ALU.mult, op1=ALU.add)
            for q in range(4):
                OP("vector", "bn_stats", [ak], ["st6"], out=st6[:, q, :], in_=acc[:, i, q * 512:(q + 1) * 512])
            OP("vector", "bn_aggr", ["st6"], ["ag"], out=ag[:], in_=st6[:].rearrange("p a b -> p (a b)"))
            OP("scalar", "activation", ["ag"], ["ag"], out=ag[:, 1:2], in_=ag[:, 1:2], func=AF.Sqrt, bias=epsL[:, 0:1])
            OP("vector", "reciprocal", ["ag"], ["ag"], out=ag[:, 1:2], in_=ag[:, 1:2])
            OP("vector", "tensor_scalar", [ak, "ag"], [ak], out=acc[:, i, :], in0=acc[:, i, :], scalar1=ag[:, 0:1], scalar2=ag[:, 1:2], op0=ALU.subtract, op1=ALU.mult)
            OP("gpsimd", "tensor_tensor", [ak, "l2g"], [ak], out=acc[:, i, :], in0=acc[:, i, :], in1=l2g[:], op=ALU.mult)
            OP("vector", "tensor_tensor", [ak, "l2b"], [ak], out=acc[:, i, :], in0=acc[:, i, :], in1=l2b[:], op=ALU.add)
            LD("sync", out[i * 128:(i + 1) * 128, :], acc[:, i, :], [], [ak])
        P.barrier()


def prep_inputs(inp, stage=2, cores=range(8)):
    f = lambda a: np.ascontiguousarray(a, dtype=np.float32)
    x = inp["x"]; c = inp["c"]
    idx = np.arange(128)
    cmask = np.zeros((128, 6, 128), np.float32)
    cmask[:, 0] = np.eye(128)
    cmask[:, 1] = idx[:, None] < idx[None, :]
    cmask[:, 2] = idx[:, None] > idx[None, :]
    cmask[:, 3] = idx[:, None] <= idx[None, :]
    cmask[:, 4] = (idx[:, None] // 64) == (idx[None, :] // 64)
    cmask[:, 5] = 1.0
    segm = np.ones((128, 512), np.float32); segm[:, ::128] = 0.0
    mu = inp["mu_shift"][0]
    muT = np.zeros((128, 27), np.float32)
    for gi in range(24):
        muT[:, gi] = mu[128 * gi:128 * (gi + 1)]
    muT[:, 24] = mu[3072:3200]; muT[:, 25] = mu[3200:3328]; muT[:32, 26] = mu[3328:3360]
    vecs = [inp["w0"][0], inp["a0"][0], inp["k_k"][0], inp["k_a"][0], inp["r_k"][0].reshape(-1), inp["lnx_g"][0], inp["lnx_b"][0]]
    vecT = np.stack([v.reshape(8, 128).T for v in vecs], axis=1)
    shared = dict(
        cmask=cmask, segm=segm, w_ada=f(inp["w_ada"][0]), b_ada_b=f(np.broadcast_to(inp["b_ada"][0][None, :], (128, 6 * D))),
        w_in=f(inp["w_in"][0]), muT=muT, vecT=f(vecT), w_dup=f(inp["w_decay_up"][0]), w_iup=f(inp["w_iclr_up"][0]),
        w_gup=f(inp["w_glora_up"][0]), w_ro=f(inp["w_rwkv_out"][0]), w_pool=f(inp["w_pool"][0]),
        pscT=f(inp["pool_scale"][0].reshape(16, 128).T), w_out=f(inp["w_out"][0]),
        lnb=f(np.stack([np.broadcast_to(inp[k][0][None, :], (128, D)) for k in ("ln1_g", "ln1_b", "ln2_g", "ln2_b")])),
    )
    if stage >= 2:
        shared.update(
            w_router=f(inp["w_router"][0]), b_router_b=f(np.broadcast_to(inp["b_router"][0][None, :], (128, NEXP))),
            w_gu=f(inp["w_gu"][0]), w_down=f(inp["w_down"][0]), b_down=f(inp["b_down"][0]),
            b_guT=f(np.concatenate([inp["b_gu"][0][:, 0::2].reshape(NEXP, 16, 128), inp["b_gu"][0][:, 1::2].reshape(NEXP, 16, 128)], axis=1).transpose(0, 2, 1)),
            iota_cap=f(np.broadcast_to(np.arange(CAP, dtype=np.float32)[None, :], (128, CAP))),
        )
    maps = []
    for core in cores:
        b, half = core // 2, core % 2
        m = dict(shared)
        m["xo"] = f(x[b, half * T_OWN:(half + 1) * T_OWN])
        m["xp"] = f(x[b, 0:T_OWN])
        m["cT"] = f(c[b].reshape(16, 128).T)
        m["flag"] = np.full((128, 1), float(half), np.float32)
        pc = np.zeros((128, 4, 16), np.float32)
        for g, W in enumerate((2, 4, 8, 16)):
            pc[:, g, :] = 1.0 / W if half == 1 else 1.0 / np.minimum(np.arange(16) + 1, W)
        m["pcorr"] = pc
        maps.append(m)
    return maps


_NC_CACHE = {}


def kernel(**inputs):
    if "full" not in _NC_CACHE:
        _NC_CACHE["full"] = build_program(stage=2, debug=False)
    nc = _NC_CACHE["full"]
    maps = prep_inputs(inputs, stage=2)
    res = run_bass_kernel_spmd(nc, maps, core_ids=list(range(8)))
    out = np.zeros((4, 2048, D), np.float32)
    for core in range(8):
        b, half = core // 2, core % 2
        out[b, half * T_OWN:(half + 1) * T_OWN] = res.results[core]["out"]
    return out
```

```python
from contextlib import ExitStack
import numpy as np
import concourse.bass as bass
import concourse.mybir as mybir
from concourse.bass_utils import run_bass_kernel_spmd

F32 = mybir.dt.float32
BF16 = mybir.dt.bfloat16
AF = mybir.ActivationFunctionType
ALU = mybir.AluOpType
ENGS = ["tensor", "vector", "scalar", "gpsimd", "sync"]
N_DMA_SEMS = 12
EPOCH = 20000

D = 2048
T_OWN = 1024
NEXP = 32
CAP = 512
ALPHA = 2.0 ** 0.25
LN_EPS = 1e-5
GN_EPS = 64e-5
NSHIFT = 3360
NIN = 8480


class Prog:
    def __init__(self, same_engine_sync=True):
        self.ins = []
        self.last_w = {}
        self.readers = {}
        self.same_engine_sync = same_engine_sync
        self.dma_count = [0] * N_DMA_SEMS
        self.n_dma = 0
        self.n_dma_sw = 0

    def _deps(self, reads, writes):
        deps = set()
        for k in reads:
            if k in self.last_w:
                deps.add(self.last_w[k])
        for k in writes:
            if k in self.last_w:
                deps.add(self.last_w[k])
            for r in self.readers.get(k, ()):
                deps.add(r)
        return deps

    def _commit(self, iid, reads, writes):
        for k in reads:
            self.readers.setdefault(k, []).append(iid)
        for k in writes:
            self.last_w[k] = iid
            self.readers[k] = []

    def op(self, eng, fn, reads=(), writes=()):
        deps = self._deps(reads, writes)
        iid = len(self.ins)
        self.ins.append(dict(eng=eng, fn=fn, deps=deps, dma=None, target=False))
        self._commit(iid, reads, writes)
        return iid

    def dma(self, eng, fn, reads=(), writes=()):
        deps = self._deps(reads, writes)
        iid = len(self.ins)
        half = N_DMA_SEMS // 2
        if eng == "gpsimd":
            s = self.n_dma_sw % half
            self.n_dma_sw += 1
        else:
            s = half + self.n_dma % half
            self.n_dma += 1
        self.dma_count[s] += 1
        self.ins.append(dict(eng=eng, fn=fn, deps=deps, dma=(s, 16 * self.dma_count[s]), target=False))
        self._commit(iid, reads, writes)
        return iid

    def barrier(self, engines=ENGS):
        deps = set()
        last = {}
        for i, r in enumerate(self.ins):
            if r["fn"] is None:
                continue
            if r["dma"] is not None:
                deps.add(i)
            else:
                last[r["eng"]] = i
        deps.update(last.values())
        for e in engines:
            self.ins.append(dict(eng=e, fn=None, deps=set(deps), dma=None, target=False))
        self.last_w = {}
        self.readers = {}

    def emit(self, nc, stack):
        ins = self.ins
        ses = self.same_engine_sync
        for r in ins:
            for d in r["deps"]:
                t = ins[d]
                if t["dma"] is None:
                    if t["eng"] == r["eng"] and (t["eng"] == "tensor" or not ses):
                        continue
                    t["target"] = True
        cnt = {e: 0 for e in ENGS}
        for r in ins:
            if r["dma"] is None and r["target"]:
                cnt[r["eng"]] += 1
                r["seq"] = cnt[r["eng"]]
        n_ep = {e: max(1, -(-cnt[e] // EPOCH)) for e in ENGS}
        psem = {e: [stack.enter_context(nc.semaphore(f"p_{e}_{k}")) for k in range(n_ep[e])] for e in ENGS}
        dsem = [stack.enter_context(nc.semaphore(f"dma_{k}")) for k in range(N_DMA_SEMS)]
        per_eng = {e: [] for e in ENGS}
        for i, r in enumerate(ins):
            per_eng[r["eng"]].append(i)
        block = stack.enter_context(nc.Block())

        def run_engine(ename, e):
            waited = {}
            for i in per_eng[ename]:
                r = ins[i]
                need = {}
                for d in r["deps"]:
                    t = ins[d]
                    if t["dma"] is not None:
                        s, v = t["dma"]
                        key = ("d", s)
                    else:
                        if t["eng"] == ename and (ename == "tensor" or not ses):
                            continue
                        ep, v = divmod(t["seq"] - 1, EPOCH)
                        v += 1
                        key = (t["eng"], ep)
                    if v > need.get(key, 0):
                        need[key] = v
                for key, v in need.items():
                    if waited.get(key, 0) >= v:
                        continue
                    waited[key] = v
                    sem = dsem[key[1]] if key[0] == "d" else psem[key[0]][key[1]]
                    e.wait_ge(sem, v)
                if r["fn"] is None:
                    continue
                if r["dma"] is not None and r["dma"][1] > 16:
                    key = ("d", r["dma"][0])
                    if waited.get(key, 0) < r["dma"][1] - 16:
                        waited[key] = r["dma"][1] - 16
                        e.wait_ge(dsem[r["dma"][0]], r["dma"][1] - 16)
                bi = r["fn"](e)
                if r["dma"] is not None:
                    bi.then_inc(dsem[r["dma"][0]], 16)
                elif r["target"]:
                    bi.then_inc(psem[ename][(r["seq"] - 1) // EPOCH], 1)

        block.tensor(lambda e: run_engine("tensor", e))
        block.vector(lambda e: run_engine("vector", e))
        block.scalar(lambda e: run_engine("scalar", e))
        block.gpsimd(lambda e: run_engine("gpsimd", e))
        block.sync(lambda e: run_engine("sync", e))


G_R, G_K, G_V = 0, 1024, 2048
G_WDAD = 3072
G_GD = 3200
G_POOL = 3360
G_GA = 4384
G_GB = 6432


class _Stop(Exception):
    pass


def build_program(stage=2, debug=False, stop=None, n_exp=NEXP):
    def CP(name):
        if stop == name:
            raise _Stop()
    nc = bass.Bass("TRN2", target_bir_lowering=False)
    P = Prog()
    AX = mybir.AxisListType

    def OP(eng, method, reads, writes, **kw):
        P.op(eng, lambda e: getattr(e, method)(**kw), reads=reads, writes=writes)

    def MM(out, lhsT, rhs, start, stop, reads, writes):
        P.op("tensor", lambda e: e.matmul(out, lhsT=lhsT, rhs=rhs, start=start, stop=stop), reads=reads, writes=writes)

    def TR(out, in_, identity, reads, writes):
        P.op("tensor", lambda e: e.transpose(out=out, in_=in_, identity=identity), reads=reads, writes=writes)

    def LD(eng, dst, src, writes, reads=()):
        P.dma(eng, lambda e: e.dma_start(out=dst, in_=src), reads=reads, writes=writes)

    dt_in = lambda n, s, d=F32: nc.dram_tensor(n, s, d, kind="ExternalInput").ap()
    dt_out = lambda n, s, d=F32: nc.dram_tensor(n, s, d, kind="ExternalOutput").ap()
    xo = dt_in("xo", [T_OWN, D]); xp = dt_in("xp", [T_OWN, D])
    cT = dt_in("cT", [128, 16]); flag = dt_in("flag", [128, 1]); pcorr = dt_in("pcorr", [128, 4, 16])
    cmask = dt_in("cmask", [128, 6, 128]); segm = dt_in("segm", [128, 512])
    w_ada = dt_in("w_ada", [D, 6 * D]); b_ada_b = dt_in("b_ada_b", [128, 6 * D])
    w_in = dt_in("w_in", [D, NIN]); muT = dt_in("muT", [128, 27]); vecT = dt_in("vecT", [128, 7, 8])
    w_dup = dt_in("w_dup", [64, 1024]); w_iup = dt_in("w_iup", [64, 1024]); w_gup = dt_in("w_gup", [160, 1024])
    w_ro = dt_in("w_ro", [1024, D]); w_pool = dt_in("w_pool", [4, 256, 512]); pscT = dt_in("pscT", [128, 16])
    w_out = dt_in("w_out", [D, D]); lnb = dt_in("lnb", [4, 128, D])
    if stage >= 2:
        w_router = dt_in("w_router", [D, NEXP]); b_router_b = dt_in("b_router_b", [128, NEXP])
        w_gu = dt_in("w_gu", [NEXP, D, 2 * D]); b_guT = dt_in("b_guT", [NEXP, 128, 32])
        w_down = dt_in("w_down", [NEXP, D, D]); b_down = dt_in("b_down", [NEXP, D])
        iota_cap = dt_in("iota_cap", [128, CAP])
    out = dt_out("out", [T_OWN, D])
    x1_d = nc.dram_tensor("x1_d", [T_OWN, D], F32, kind="Internal").ap()
    h2_d = nc.dram_tensor("h2_d", [T_OWN, D], BF16, kind="Internal").ap()
    modb_d = nc.dram_tensor("modb_d", [4, 128, D], F32, kind="Internal").ap()
    lg_d = nc.dram_tensor("lg_d", [T_OWN, NEXP], F32, kind="ExternalOutput" if debug else "Internal").ap()
    dbg = {}
    if debug:
        dbg["yr"] = dt_out("dbg_yr", [128, 8, T_OWN]); dbg["mixin"] = dt_out("dbg_mixin", [128, 16, T_OWN])
        dbg["modb"] = dt_out("dbg_modb", [128, 2 * D]); dbg["ps"] = dt_out("dbg_ps", [128, 27, T_OWN])
        dbg["ld"] = dt_out("dbg_ld", [128, 8, T_OWN]); dbg["dT"] = dt_out("dbg_dT", [128, 8, T_OWN])
        dbg["y"] = dt_out("dbg_y", [128, 8, T_OWN])
        dbg["x1"] = dt_out("dbg_x1", [T_OWN, D])
        if stage >= 2:
            dbg["pm"] = dt_out("dbg_pm", [T_OWN, NEXP]); dbg["pos"] = dt_out("dbg_pos", [T_OWN, NEXP])

    with ExitStack() as st:
        sb = lambda n, s, d=F32: st.enter_context(nc.sbuf_tensor(n, s, d))
        cm = sb("cm", [128, 6, 128])
        ident, m_su, m_sl, m_ui, m_bd, ones_f = (cm[:, i, :] for i in range(6))
        cmb = sb("cmb", [128, 6, 128], BF16)
        ident_b = cmb[:, 0, :]
        epsG = sb("epsG", [128, 1]); epsL = sb("epsL", [128, 1])
        OP("gpsimd", "memset", [], ["epsG"], ap=epsG[:], constant=GN_EPS)
        OP("gpsimd", "memset", [], ["epsL"], ap=epsL[:], constant=LN_EPS)
        NSL = 4
        slab = [sb(f"slab{i}", [128, 4096], BF16) for i in range(NSL)]
        slab_i = [0]
        pbank = [st.enter_context(nc.psum_tensor(f"pb{i}", [128, 512], F32)) for i in range(7)]
        ptb = st.enter_context(nc.psum_tensor("ptb", [128, 1024], BF16))
        st_p = ExitStack()
        sbp = lambda n, s_, d=F32: st_p.enter_context(nc.sbuf_tensor(n, s_, d))
        seg = sbp("seg", [128, 512]); flg = sbp("flg", [128, 1]); pcr = sbp("pcr", [128, 4, 16])
        mu = sbp("mu", [128, 27]); omu = sbp("omu", [128, 27]); vec = sbp("vec", [128, 7, 8]); psc = sbp("psc", [128, 16])
        condB = sbp("condB", [128, 16, 128], BF16); condf = sbp("condf", [128, 16])
        sc1p = sbp("sc1p", [128, 16]); sh1 = sbp("sh1", [128, 16])
        wlo = sbp("wlo", [128, 3, 1024], BF16)

        pq = []
        LOOK = 2

        def plan(specs):
            pq.extend(dict(tag=t_, src=s_, kc=k_, cols=c_, buf=None) for (t_, s_, k_, c_) in specs)

        def _issue(src_ap, kc, cols):
            i = slab_i[0] % NSL
            slab_i[0] += 1
            view = slab[i][:, 0:kc * cols].rearrange("p (k c) -> p k c", k=kc)
            LD("gpsimd", view, src_ap, [f"slab{i}"])
            return view, f"slab{i}"

        def load_slab(src_ap, kc, cols, tag=None):
            if pq:
                assert tag is not None and pq[0]["tag"] == tag, (tag, pq[0]["tag"])
                for it in pq[:1 + LOOK]:
                    if it["buf"] is None:
                        it["buf"] = _issue(it["src"], it["kc"], it["cols"])
                return pq.pop(0)["buf"]
            return _issue(src_ap, kc, cols)

        def flush_plan():
            assert not pq, [it["tag"] for it in pq]

        for dst, src, key in [(cm[:], cmask, "cm"), (seg[:], segm, "seg"), (flg[:], flag, "flg"), (pcr[:], pcorr, "pcr"), (mu[:], muT, "mu"),
                              (vec[:], vecT, "vec"), (psc[:], pscT, "psc"), (condf[:], cT, "condf")]:
            LD("sync", dst, src, [key])
        LD("gpsimd", wlo[0:64, 0, :], w_dup, ["wlo"]); LD("gpsimd", wlo[64:128, 0, :], w_iup, ["wlo"])
        LD("gpsimd", wlo[:, 1, :], w_gup[0:128, :], ["wlo"]); LD("gpsimd", wlo[0:32, 2, :], w_gup[128:160, :], ["wlo"])
        OP("vector", "tensor_copy", ["cm"], ["cmb"], out=cmb[:], in_=cm[:])
        OP("vector", "tensor_scalar", ["mu"], ["omu"], out=omu[:], in0=mu[:], scalar1=-1.0, scalar2=1.0, op0=ALU.mult, op1=ALU.add)
        OP("scalar", "activation", ["condf"], ["condf"], out=condf[:], in_=condf[:], func=AF.Silu)
        for k in range(16):
            OP("vector", "tensor_copy", ["condf"], ["condB"], out=condB[:, k, :], in_=condf[:, k:k + 1].to_broadcast([128, 128]))

        with ExitStack() as st_a:
            badd = st_a.enter_context(nc.sbuf_tensor("badd", [128, 2, 256], F32))
            mod01 = st_a.enter_context(nc.sbuf_tensor("mod01", [128, 2 * D], F32))
            mtmp = st_a.enter_context(nc.sbuf_tensor("mtmp", [128, 16, 128], F32))

            def ada_slab(si, dst_ap, dst_key):
                c0 = si * 256
                view, key = load_slab(w_ada[:, c0:c0 + 256].rearrange("(k p) c -> p k c", p=128), 16, 256, tag=("ada", si))
                bi = si % 2
                LD("sync", badd[:, bi, :], b_ada_b[:, c0:c0 + 256], [f"badd{bi}"])
                pbk = pbank[si % 2]
                for k in range(16):
                    MM(pbk[:, 0:256], condB[:, k, :], view[:, k, :], k == 0, k == 15, [key, "condB"], [f"pb{si % 2}"])
                OP("vector", "tensor_tensor", [f"pb{si % 2}", f"badd{bi}"], [dst_key], out=dst_ap, in0=pbk[:, 0:256], in1=badd[:, bi, :], op=ALU.add)

            plan([(("ada", si_), w_ada[:, si_ * 256:si_ * 256 + 256].rearrange("(k p) c -> p k c", p=128), 16, 256) for si_ in range(48)])
            for si in range(16):
                ada_slab(si, mod01[:, si * 256:(si + 1) * 256], f"mt{si // 8}")
            for j, (dst, dkey) in enumerate([(sh1, "sh1"), (sc1p, "sc1p")]):
                OP("vector", "tensor_tensor", [f"mt{j}", "cm"], ["mtmp"], out=mtmp[:], in0=mod01[:, j * D:(j + 1) * D].rearrange("p (c q) -> p c q", q=128),
                   in1=cm[:, 0:1, :].to_broadcast([128, 16, 128]), op=ALU.mult)
                OP("vector", "tensor_reduce", ["mtmp"], [dkey], out=dst[:], in_=mtmp[:], axis=AX.X, op=ALU.add)
            OP("vector", "tensor_scalar_add", ["sc1p"], ["sc1p"], out=sc1p[:], in0=sc1p[:], scalar1=1.0)
            if debug:
                LD("sync", dbg["modb"], mod01[:], [], ["mt0", "mt1"])
            order = {2: 0, 3: 1, 4: 2, 5: 3}
            for piece in range(2, 6):
                mt = mod01[:, (piece % 2) * D:(piece % 2 + 1) * D]
                mk = f"mt{piece % 2}"
                for s8 in range(8):
                    ada_slab(piece * 8 + s8, mt[:, s8 * 256:(s8 + 1) * 256], mk)
                if piece == 4:
                    OP("vector", "tensor_scalar_add", [mk], [mk], out=mt, in0=mt, scalar1=1.0)
                LD("sync", modb_d[order[piece]], mt, [], [mk])
            P.barrier()
        if stage == 0:
            st_p.close()
            P.emit(nc, st)
            return nc

        with ExitStack() as st_m:
            sbm = lambda n, s, d=F32: st_m.enter_context(nc.sbuf_tensor(n, s, d))
            hT = sbm("hT", [128, 16, 512], BF16)
            carry = sbm("carry", [128, 27])
            halo = sbm("halo", [128, 8, 16])
            dT = sbm("dT", [128, 8, 512], BF16); yrT = sbm("yrT", [128, 8, 512], BF16); mixT = sbm("mixT", [128, 16, 512], BF16)
            Hst = sbm("Hst", [128, 8, 128])
            import os
            if not os.environ.get("NO_MEMSET"):
                OP("vector", "memset", [], ["H%d" % i for i in range(8)], ap=Hst[:].rearrange("p a b -> p (a b)"), constant=0.0)
                OP("vector", "memset", [], ["carry"], ap=carry[:], constant=0.0)
                OP("vector", "memset", [], ["halo"], ap=halo[:].rearrange("p a b -> p (a b)"), constant=0.0)
            pslot_i = [0]

            def pslot():
                i = pslot_i[0] % 4
                pslot_i[0] += 1
                return pbank[2 + i][:, 0:128], f"pb{2 + i}"

            def in_proj(col0, ncols, ntok0, ntok, pbk, pkey):
                view, key = load_slab(w_in[:, col0:col0 + ncols].rearrange("(k p) c -> p k c", p=128), 16, ncols, tag=("win", col0, ncols))
                for k in range(16):
                    MM(pbk[0:ncols, 0:ntok], view[:, k, :], hT[:, k, ntok0:ntok0 + ntok], k == 0, k == 15, [key, "hT"], [pkey])

            scopes = []
            try:
                for blk in range(4):
                    own = blk >= 2
                    xsrc = xo if own else xp
                    t0 = (blk % 2) * 512
                    tcol = (blk - 2) * 512
                    st1 = ExitStack(); scopes.append(st1)
                    sb1 = lambda n, s_, d=F32: st1.enter_context(nc.sbuf_tensor(f"{n}_b{blk}", s_, d))
                    xs = [sb1(f"xs{i}", [128, D]) for i in range(1)]
                    pr = sb1("pr", [128, 8, 512], BF16); pk = sb1("pk", [128, 8, 512], BF16); pv = sb1("pv", [128, 8, 512], BF16)
                    plo = sb1("plo", [128, 3, 512])
                    upool = sb1("upool", [128, 528])
                    ptmp = [sb1(f"ptmp{i}", [128, 528]) for i in range(2)]
                    tnh = sb1("tnh", [128, 512], BF16); sgd = sb1("sgd", [128, 2, 512], BF16)
                    f_ld = sb1("f_ld", [128, 512]); f_ic = sb1("f_ic", [128, 512]); f_L = sb1("f_L", [128, 512])
                    f_t0 = sb1("f_t0", [128, 512]); f_t1 = sb1("f_t1", [128, 512]); f_t2 = sb1("f_t2", [128, 512])
                    f_kk = sb1("f_kk", [128, 512]); f_km = sb1("f_km", [128, 512]); f_bb = sb1("f_bb", [128, 512])
                    ynT = sb1("ynT", [128, 512])
                    LC = sb1("LC", [128, 4]); WC = sb1("WC", [128, 4])
                    o_r = sb1("o_r", [128, 512], BF16); o_a = sb1("o_a", [128, 512], BF16); o_b = sb1("o_b", [128, 512], BF16)
                    o_k = sb1("o_k", [128, 512], BF16); o_bh = sb1("o_bh", [128, 512], BF16); o_kh = sb1("o_kh", [128, 512], BF16)
                    rF = sb1("rF", [128, 512])
                    tm = sb1("tm", [128, 4, 4, 128], BF16)
                    bonus = sb1("bonus", [128, 512]); gT = sb1("gT", [128, 512])
                    uM = [sb1(f"uM{i}", [128, 2, 128]) for i in range(8)]
                    uN = [sb1(f"uN{i}", [128, 128]) for i in range(8)]
                    uNb = [sb1(f"uNb{i}", [128, 128], BF16) for i in range(4)]
                    uA = [sb1(f"uA{i}", [128, 3, 128], BF16) for i in range(4)]
                    uAt = [sb1(f"uAt{i}", [128, 128], BF16) for i in range(2)]; uX = [sb1(f"uX{i}", [128, 128], BF16) for i in range(2)]
                    uU = [sb1(f"uU{i}", [128, 128], BF16) for i in range(2)]
                    uPT = sb1("uPT", [128, 128]); uQ = sb1("uQ", [128, 128]); uRh = sb1("uRh", [128, 128])
                    uY = sb1("uY", [128, 128]); uZ = sb1("uZ", [128, 128]); ust = sb1("ust", [128, 2, 6]); uag = sb1("uag", [128, 2, 2])
                    ga, gb, t1, t2 = f_ld, f_ic, f_kk, f_km
                    glist = []
                    for j in range(8):
                        glist.append((j, G_R + 128 * j, 128, pr, j, False))
                    for j in range(8):
                        glist.append((8 + j, G_K + 128 * j, 128, pk, j, True))
                    for j in range(8):
                        glist.append((16 + j, G_V + 128 * j, 128, pv, j, True))
                    glist += [(24, G_WDAD, 128, plo, 0, True), (25, G_GD, 128, plo, 1, False), (26, G_GD + 128, 32, plo, 2, False)]

                    CP(f'pre{blk}')
                    wsl = lambda c0_, n_: w_in[:, c0_:c0_ + n_].rearrange("(k p) c -> p k c", p=128)
                    bp = []
                    for (gi_, col0_, ncols_, dst_, di_, fullp_) in glist:
                        if (not own) and (not fullp_) and blk != 1:
                            continue
                        bp.append((("win", col0_, ncols_), wsl(col0_, ncols_), 16, ncols_))
                    if (not own) and blk == 1:
                        bp += [(("win", G_POOL + 128 * j_, 128), wsl(G_POOL + 128 * j_, 128), 16, 128) for j_ in range(8)]
                    if own:
                        bp += [(("win", G_POOL + 128 * j_, 128), wsl(G_POOL + 128 * j_, 128), 16, 128) for j_ in range(8)]
                        for c2_ in range(8):
                            bp.append((("win", G_GA + 256 * c2_, 256), wsl(G_GA + 256 * c2_, 256), 16, 256))
                            bp.append((("win", G_GB + 256 * c2_, 256), wsl(G_GB + 256 * c2_, 256), 16, 256))
                            bp.append((("wro", c2_), w_ro[:, c2_ * 256:(c2_ + 1) * 256].rearrange("(k p) c -> p k c", p=128), 8, 256))
                            for u2_ in range(2):
                                c_ = 2 * c2_ + u2_
                                bp.append((("wpool", c_), w_pool[c_ // 4, :, (c_ % 4) * 128:(c_ % 4 + 1) * 128].rearrange("(k p) c -> p k c", p=128), 2, 128))
                    plan(bp)
                    for tt in range(4):
                        LD("sync", xs[0][:], xsrc[t0 + tt * 128:t0 + (tt + 1) * 128, :], ["xs0"])
                        for c in range(16):
                            TR(pbank[6][:, (c % 4) * 128:(c % 4 + 1) * 128], xs[0][:, c * 128:(c + 1) * 128], ident, ["xs0", "cm"], ["pb6"])
                            if c % 4 == 3:
                                for u_ in range(4):
                                    cc_ = c - 3 + u_
                                    OP("scalar", "activation", ["pb6", "sc1p", "sh1"], ["hT"], out=hT[:, cc_, tt * 128:(tt + 1) * 128], in_=pbank[6][:, u_ * 128:(u_ + 1) * 128],
                                       func=AF.Identity, scale=sc1p[:, cc_:cc_ + 1], bias=sh1[:, cc_:cc_ + 1])
                    CP(f'i{blk}')
                    for (gi, col0, ncols, dst, di, fullp) in glist:
                        pbk, pkey = pbank[gi % 2], f"pb{gi % 2}"
                        if not own and not fullp:
                            if blk == 1:
                                in_proj(col0, ncols, 496, 16, pbk, pkey)
                                OP("vector", "tensor_copy", [pkey], ["carry"], out=carry[0:ncols, gi:gi + 1], in_=pbk[0:ncols, 15:16])
                            continue
                        in_proj(col0, ncols, 0, 512, pbk, pkey)
                        dsl = dst[0:ncols, di, :]
                        OP("scalar", "activation", [pkey, "omu"], ["f_t0"], out=f_t0[0:ncols, :], in_=pbk[0:ncols, :], func=AF.Identity, scale=omu[0:ncols, gi:gi + 1])
                        OP("vector", "scalar_tensor_tensor", [pkey, "mu", "f_t0"], [f"p{gi}"], out=dsl[:, 1:512], in0=pbk[0:ncols, 0:511], scalar=mu[0:ncols, gi:gi + 1],
                           in1=f_t0[0:ncols, 1:512], op0=ALU.mult, op1=ALU.add)
                        OP("vector", "scalar_tensor_tensor", ["carry", "mu", "f_t0"], [f"p{gi}"], out=dsl[:, 0:1], in0=carry[0:ncols, gi:gi + 1], scalar=mu[0:ncols, gi:gi + 1],
                           in1=f_t0[0:ncols, 0:1], op0=ALU.mult, op1=ALU.add)
                        OP("vector", "tensor_copy", [pkey, "carry"], ["carry"], out=carry[0:ncols, gi:gi + 1], in_=pbk[0:ncols, 511:512])
                        if debug and own and dst is plo:
                            LD("sync", dbg["ps"][0:ncols, gi, tcol:tcol + 512], dsl, [], [f"p{gi}"])
                    if blk == 1:
                        OP("vector", "tensor_scalar_mul", ["carry", "flg"], ["carry"], out=carry[:], in0=carry[:], scalar1=flg[:, 0:1])
                    CP(f'ii{blk}')
                    OP("scalar", "activation", ["p24"], ["tnh"], out=tnh[0:64, :], in_=plo[0:64, 0, :], func=AF.Tanh)
                    OP("vector", "tensor_copy", ["p24"], ["tnh"], out=tnh[64:128, :], in_=plo[64:128, 0, :])
                    if own:
                        OP("scalar", "activation", ["p25"], ["sgd"], out=sgd[:, 0, :], in_=plo[:, 1, :], func=AF.Sigmoid)
                        OP("scalar", "activation", ["p26"], ["sgd"], out=sgd[0:32, 1, :], in_=plo[0:32, 2, :], func=AF.Sigmoid)
                    for hp in range(8):
                        cs = slice(hp * 128, (hp + 1) * 128)
                        V = lambda j: vec[:, j, hp:hp + 1]
                        pb6 = pbank[6]
                        kkey, vkey, rkey = f"p{8 + hp}", f"p{16 + hp}", f"p{hp}"
                        MM(pb6[:, :], wlo[0:64, 0, cs], tnh[0:64, :], True, True, ["wlo", "tnh"], ["pb6"])
                        OP("scalar", "activation", ["pb6", "vec"], ["f_ld"], out=f_ld[:], in_=pb6[:, :], func=AF.Sigmoid, bias=V(0))
                        MM(pb6[:, :], wlo[64:128, 0, cs], tnh[64:128, :], True, True, ["wlo", "tnh"], ["pb6"])
                        OP("scalar", "activation", ["pb6", "vec"], ["f_ic"], out=f_ic[:], in_=pb6[:, :], func=AF.Sigmoid, bias=V(1))
                        OP("vector", "tensor_scalar_mul", ["f_ld"], ["f_ld"], out=f_ld[:], in0=f_ld[:], scalar1=-0.6065306597126334)
                        OP("vector", "tensor_tensor_scan", ["seg", "f_ld"], ["f_L"], out=f_L[:], data0=seg[:], data1=f_ld[:], initial=0.0, op0=ALU.mult, op1=ALU.add)
                        OP("vector", "tensor_copy", ["f_L"], ["LC"], out=LC[:], in_=f_L[:].rearrange("p (c t) -> p c t", t=128)[:, :, 127])
                        OP("scalar", "activation", ["LC"], ["WC"], out=WC[:], in_=LC[:], func=AF.Exp)
                        OP("vector", "tensor_scalar_mul", [kkey, "vec"], ["f_kk"], out=f_kk[:], in0=pk[:, hp, :], scalar1=V(2))
                        OP("gpsimd", "tensor_tensor", ["f_kk"], ["f_t0"], out=f_t0[:], in0=f_kk[:], in1=f_kk[:], op=ALU.mult)
                        MM(pb6[:, :], m_bd, f_t0[:], True, True, ["cm", "f_t0"], ["pb6"])
                        OP("scalar", "activation", ["pb6"], ["f_t1"], out=f_t1[:], in_=pb6[:, :], func=AF.Sqrt)
                        OP("vector", "tensor_scalar_max", ["f_t1"], ["f_t1"], out=f_t1[:], in0=f_t1[:], scalar1=1e-12)
                        OP("vector", "reciprocal", ["f_t1"], ["f_t1"], out=f_t1[:], in_=f_t1[:])
                        OP("vector", "tensor_tensor", ["f_kk", "f_t1"], ["f_kk"], out=f_kk[:], in0=f_kk[:], in1=f_t1[:], op=ALU.mult)
                        OP("vector", "tensor_scalar", ["f_ic", "vec"], ["f_t2"], out=f_t2[:], in0=f_ic[:], scalar1=-1.0, scalar2=V(3), op0=ALU.add, op1=ALU.mult)
                        OP("vector", "scalar_tensor_tensor", ["f_t2", kkey], ["f_km"], out=f_km[:], in0=f_t2[:], scalar=1.0, in1=pk[:, hp, :], op0=ALU.add, op1=ALU.mult)
                        OP("gpsimd", "tensor_tensor", ["f_kk", "f_ic"], ["f_bb"], out=f_bb[:], in0=f_kk[:], in1=f_ic[:], op=ALU.mult)
                        OP("scalar", "activation", ["f_L"], ["f_t0"], out=f_t0[:], in_=f_L[:], func=AF.Exp)
                        if own:
                            OP("vector", "tensor_tensor", [rkey, "f_t0"], ["rF"], out=rF[:], in0=pr[:, hp, :], in1=f_t0[:], op=ALU.mult)
                            OP("gpsimd", "tensor_copy", ["rF"], ["o_r"], out=o_r[:], in_=rF[:])
                        OP("vector", "tensor_tensor", ["f_L", "f_ld"], ["f_t1"], out=f_t1[:], in0=f_L[:], in1=f_ld[:], op=ALU.subtract)
                        OP("scalar", "activation", ["f_t1"], ["f_t1"], out=f_t1[:], in_=f_t1[:], func=AF.Exp)
                        OP("vector", "scalar_tensor_tensor", ["f_kk", "f_t1"], ["o_a"], out=o_a[:], in0=f_kk[:], scalar=-1.0, in1=f_t1[:], op0=ALU.mult, op1=ALU.mult)
                        OP("scalar", "activation", ["f_L"], ["f_t2"], out=f_t2[:], in_=f_L[:], func=AF.Exp, scale=-1.0)
                        OP("vector", "tensor_tensor", ["f_bb", "f_t2"], ["o_b"], out=o_b[:], in0=f_bb[:], in1=f_t2[:], op=ALU.mult)
                        OP("gpsimd", "tensor_tensor", ["f_km", "f_t2"], ["o_k"], out=o_k[:], in0=f_km[:], in1=f_t2[:], op=ALU.mult)
                        for c in range(4):
                            OP("scalar", "activation", ["f_L", "LC"], ["f_t0"], out=f_t0[:, c * 128:(c + 1) * 128], in_=f_L[:, c * 128:(c + 1) * 128], func=AF.Exp, scale=-1.0, bias=LC[:, c:c + 1])
                        OP("vector", "tensor_tensor", ["f_bb", "f_t0"], ["o_bh"], out=o_bh[:], in0=f_bb[:], in1=f_t0[:], op=ALU.mult)
                        OP("gpsimd", "tensor_tensor", ["f_km", "f_t0"], ["o_kh"], out=o_kh[:], in0=f_km[:], in1=f_t0[:], op=ALU.mult)
                        CP(f'prep{blk}_{hp}')
                        if debug and own:
                            LD("sync", dbg["ld"][:, hp, tcol:tcol + 512], f_ld[:], [], ["f_ld"])
                        for c in range(4):
                            tc_ = slice(c * 128, (c + 1) * 128)
                            for wi, (src, skey) in enumerate([(o_a[:, tc_], "o_a"), (o_bh[:, tc_], "o_bh"), (o_kh[:, tc_], "o_kh"), (pv[:, hp, tc_], vkey)]):
                                TR(ptb[:, wi * 128:(wi + 1) * 128], src, ident_b, [skey, "cmb"], ["ptb"])
                            if c % 2:
                                OP("vector", "tensor_copy", ["ptb"], [f"tm{c}"], out=tm[:, c, :, :], in_=ptb[:, 0:512].rearrange("p (w q) -> p w q", q=128))
                            else:
                                OP("scalar", "activation", ["ptb"], [f"tm{c}"], out=tm[:, c, :, :], in_=ptb[:, 0:512].rearrange("p (w q) -> p w q", q=128), func=AF.Identity)
                        CP(f'tm{blk}_{hp}')
                        if own:
                            OP("vector", "scalar_tensor_tensor", [rkey, "vec", "f_km"], ["f_t1"], out=f_t1[:], in0=pr[:, hp, :], scalar=V(4), in1=f_km[:], op0=ALU.mult, op1=ALU.mult)
                            MM(pb6[:, :], m_bd, f_t1[:], True, True, ["cm", "f_t1"], ["pb6"])
                            OP("vector", "tensor_tensor", ["pb6", vkey], ["bonus"], out=bonus[:], in0=pb6[:, :], in1=pv[:, hp, :], op=ALU.mult)
                            MM(pb6[:, :], wlo[:, 1, cs], sgd[:, 0, :], True, False, ["wlo", "sgd"], ["pb6"])
                            MM(pb6[:, :], wlo[0:32, 2, cs], sgd[0:32, 1, :], False, True, ["wlo", "sgd"], ["pb6"])
                            OP("scalar", "activation", ["pb6"], ["gT"], out=gT[:], in_=pb6[:, :], func=AF.Identity)

                        def head_chain(h, c, q, cs):
                            tc_ = slice(c * 128, (c + 1) * 128)
                            tmk = f"tm{c}"
                            pbs = slice(h * 64, (h + 1) * 64)
                            bt, at, kt, rt = o_b[pbs, tc_], o_a[pbs, tc_], o_k[pbs, tc_], o_r[pbs, tc_]
                            M0, N0 = uM[2 * q], uN[2 * q]
                            mk0 = f"uM{2 * q}"
                            uAq, uAk = uA[q], f"uA{q}"
                            specs = [(bt, at, m_su, M0[:, 0, :], mk0, ["o_b", "o_a"]), (at, bt, m_sl, M0[:, 1, :], mk0, ["o_b", "o_a"]),
                                     (kt, at, m_su, uAq[:, 2, :], uAk, ["o_k", "o_a"])]
                            if own:
                                specs += [(bt, rt, m_ui, uAq[:, 0, :], uAk, ["o_b", "o_r"]), (kt, rt, m_ui, uAq[:, 1, :], uAk, ["o_k", "o_r"])]
                            for (l_, r_, msk, dst, dkey, rk) in specs:
                                ps_, pk_ = pslot()
                                MM(ps_, l_, r_, True, True, rk, [pk_])
                                OP("vector", "tensor_tensor", [pk_, "cm"], [dkey], out=dst, in0=ps_, in1=msk, op=ALU.mult)
                                yield
                            OP("gpsimd", "tensor_tensor", [mk0, "cm"], [f"uN{2 * q}"], out=N0[:], in0=M0[:, 0, :], in1=ident, op=ALU.add)
                            cur, curk, Nc, Nk = M0, mk0, N0, f"uN{2 * q}"
                            for j in range(1, 7):
                                nxt, nxtk = uM[2 * q + (j % 2)], f"uM{2 * q + (j % 2)}"
                                Nn, Nnk = uN[2 * q + (j % 2)], f"uN{2 * q + (j % 2)}"
                                rk = [curk, curk + "a", curk + "b"]
                                if j < 6:
                                    ps_, pk_ = pslot()
                                    MM(ps_, cur[:, 1, :], cur[:, 0, :], True, True, rk, [pk_])
                                    OP("scalar", "activation", [pk_], [nxtk + "a"], out=nxt[:, 0, :], in_=ps_, func=AF.Identity)
                                ps2, pk2 = pslot()
                                MM(ps2, cur[:, 0, :], cur[:, 1, :], True, True, rk, [pk2])
                                OP("scalar", "activation", [pk2], [nxtk + "b"], out=nxt[:, 1, :], in_=ps2, func=AF.Identity)
                                yield
                                ps3, pk3 = pslot()
                                MM(ps3, nxt[:, 1, :], Nc[:], True, True, [nxtk + "b", Nk], [pk3])
                                OP("vector", "tensor_tensor", [pk3, Nk], [Nnk], out=Nn[:], in0=ps3, in1=Nc[:], op=ALU.add)
                                cur, curk, Nc, Nk = nxt, nxtk, Nn, Nnk
                                yield
                            OP("gpsimd", "tensor_copy", [Nk], [f"uNb{q}"], out=uNb[q][:], in_=Nc[:])
                            ps_, pk_ = pslot()
                            MM(ps_[:, 0:64], uNb[q][:], tm[:, c, 0, pbs], True, True, [f"uNb{q}", tmk], [pk_])
                            OP("vector", "tensor_copy", [pk_], [f"uAt{cs}_{h}"], out=uAt[cs][:, pbs], in_=ps_[:, 0:64])
                            ps_, pk_ = pslot()
                            MM(ps_[:, 0:64], uAq[:, 2, :], tm[:, c, 3, pbs], True, True, [uAk, tmk], [pk_])
                            OP("scalar", "activation", [pk_], [f"uX{cs}_{h}"], out=uX[cs][:, pbs], in_=ps_[:, 0:64], func=AF.Identity)
                            yield
                            ps_, pk_ = pslot()
                            MM(ps_[:, 0:64], uNb[q][:], uX[cs][:, pbs], True, True, [f"uNb{q}", f"uX{cs}_{h}"], [pk_])
                            OP("vector", "tensor_copy", [pk_], [f"uU{cs}_{h}"], out=uU[cs][:, pbs], in_=ps_[:, 0:64])
                            yield

                        def pair_level(c, cs):
                            tc_ = slice(c * 128, (c + 1) * 128)
                            tmk = f"tm{c}"
                            atk = [f"uAt{cs}_0", f"uAt{cs}_1"]
                            uuk = [f"uU{cs}_0", f"uU{cs}_1"]
                            ps_, pk_ = pslot()
                            MM(ps_, uAt[cs][:], tm[:, c, 1, :], True, True, atk + [tmk], [pk_])
                            OP("vector", "tensor_tensor", [pk_, "cm"], ["uPT"], out=uPT[:], in0=ps_, in1=m_bd, op=ALU.mult)
                            OP("vector", "scalar_tensor_tensor", ["uPT", "cm", "WC"], ["uPT"], out=uPT[:], in0=ident, scalar=WC[:, c:c + 1], in1=uPT[:], op0=ALU.mult, op1=ALU.add)
                            ps_, pk_ = pslot()
                            MM(ps_, tm[:, c, 1, :], uU[cs][:], True, False, uuk + [tmk], [pk_])
                            MM(ps_, tm[:, c, 2, :], tm[:, c, 3, :], False, True, [tmk], [pk_])
                            OP("vector", "tensor_tensor", [pk_, "cm"], ["uQ"], out=uQ[:], in0=ps_, in1=m_bd, op=ALU.mult)
                            if own:
                                for h in range(2):
                                    pbs = slice(h * 64, (h + 1) * 64)
                                    q = 2 * cs + h
                                    ps_, pk_ = pslot()
                                    MM(ps_, uAt[cs][:], uA[q][:, 0, :], True, True, atk + [f"uA{q}"], [pk_])
                                    OP("vector", "tensor_tensor", [pk_, "rF"], [f"uRh{h}"], out=uRh[pbs, :], in0=ps_[pbs, :], in1=rF[pbs, tc_], op=ALU.add)
                                psy, pky = pslot()
                                MM(psy, uRh[:], Hst[:, hp, :], True, True, ["uRh0", "uRh1", f"H{hp}"], [pky])
                                ps0, pk0 = pslot()
                                for h in range(2):
                                    pbs = slice(h * 64, (h + 1) * 64)
                                    q = 2 * cs + h
                                    MM(ps0[:, pbs], uA[q][:, 0, :], uU[cs][:, pbs], True, False, [f"uA{q}", f"uU{cs}_{h}"], [pk0])
                                    MM(ps0[:, pbs], uA[q][:, 1, :], tm[:, c, 3, pbs], False, True, [f"uA{q}", tmk], [pk0])
                                OP("scalar", "activation", [pk0], ["uY"], out=uY[:], in_=ps0, func=AF.Identity)
                                OP("vector", "tensor_tensor", [pky, "uY"], ["uY"], out=uY[:], in0=psy, in1=uY[:], op=ALU.add)
                                for h in range(2):
                                    pbs = slice(h * 64, (h + 1) * 64)
                                    OP("vector", "bn_stats", ["uY"], [f"ust{h}"], out=ust[:, h, :], in_=uY[:, pbs])
                                    OP("vector", "bn_aggr", [f"ust{h}"], [f"uag{h}"], out=uag[:, h, :], in_=ust[:, h, :])
                                    OP("scalar", "activation", [f"uag{h}"], [f"uag{h}"], out=uag[:, h, 1:2], in_=uag[:, h, 1:2], func=AF.Sqrt, bias=epsG[:, 0:1])
                                    OP("vector", "reciprocal", [f"uag{h}"], [f"uag{h}"], out=uag[:, h, 1:2], in_=uag[:, h, 1:2])
                                    OP("vector", "tensor_scalar", ["uY", f"uag{h}"], [f"uZ{h}"], out=uZ[:, pbs], in0=uY[:, pbs], scalar1=uag[:, h, 0:1], scalar2=uag[:, h, 1:2],
                                       op0=ALU.subtract, op1=ALU.mult)
                                psz, pkz = pslot()
                                TR(psz, uZ[:], ident, ["uZ0", "uZ1", "cm"], [pkz])
                                OP("scalar", "activation", [pkz, "vec"], ["ynT"], out=ynT[:, tc_], in_=psz, func=AF.Identity, scale=vec[:, 5, hp:hp + 1], bias=vec[:, 6, hp:hp + 1])
                            psh, pkh = pslot()
                            MM(psh, uPT[:], Hst[:, hp, :], True, True, ["uPT", f"H{hp}"], [pkh])
                            OP("vector", "tensor_tensor", [pkh, "uQ"], [f"H{hp}"], out=Hst[:, hp, :], in0=psh, in1=uQ[:], op=ALU.add)
                            if blk == 1 and c == 3:
                                OP("vector", "tensor_scalar_mul", [f"H{hp}", "flg"], [f"H{hp}"], out=Hst[:, hp, :], in0=Hst[:, hp, :], scalar1=flg[:, 0:1])

                        for cp_ in (0, 2):
                            chains = [head_chain(h, cp_ + cs, 2 * cs + h, cs) for cs in range(2) for h in range(2)]
                            live = list(chains)
                            while live:
                                for g_ in list(live):
                                    try:
                                        next(g_)
                                    except StopIteration:
                                        live.remove(g_)
                            pair_level(cp_, 0)
                            pair_level(cp_ + 1, 1)

                        if own:
                            if debug:
                                LD("sync", dbg["y"][:, hp, tcol:tcol + 512], ynT[:], [], ["ynT"])
                            OP("vector", "tensor_tensor", ["ynT", "bonus"], ["ynT"], out=ynT[:], in0=ynT[:], in1=bonus[:], op=ALU.add)
                            OP("vector", "tensor_tensor", ["ynT", "gT"], ["yrT"], out=yrT[:, hp, :], in0=ynT[:], in1=gT[:], op=ALU.mult)
                            if debug:
                                OP("vector", "tensor_tensor", ["ynT", "gT"], ["ynT"], out=ynT[:], in0=ynT[:], in1=gT[:], op=ALU.mult)
                                LD("sync", dbg["yr"][:, hp, tcol:tcol + 512], ynT[:], [], ["ynT"])
                    if not own:
                        if blk == 1:
                            for j in range(8):
                                pbk, pkey = pbank[j % 2], f"pb{j % 2}"
                                in_proj(G_POOL + 128 * j, 128, 496, 16, pbk, pkey)
                                OP("vector", "tensor_scalar_mul", [pkey, "flg"], ["halo"], out=halo[:, j, :], in0=pbk[:, 0:16], scalar1=flg[:, 0:1])
                        P.barrier()
                        st1.close()
                        continue
                    CP(f'rwkv{blk}')
                    for j in range(8):
                        g = j // 2
                        W = (2, 4, 8, 16)[g]
                        pbk, pkey = pbank[j % 2], f"pb{j % 2}"
                        in_proj(G_POOL + 128 * j, 128, 0, 512, pbk, pkey)
                        OP("scalar", "activation", [pkey], ["upool"], out=upool[:, 16:528], in_=pbk[:, :], func=AF.Identity)
                        OP("gpsimd", "tensor_copy", ["halo"], ["upool"], out=upool[:, 0:16], in_=halo[:, j, :])
                        src, skey = upool[:, :], "upool"
                        step, pi = 1, 0
                        while step < W:
                            dstt = ptmp[pi]
                            OP("gpsimd", "tensor_tensor", [skey], [f"ptmp{pi}"], out=dstt[:, step:528], in0=src[:, step:528], in1=src[:, 0:528 - step], op=ALU.add)
                            OP("gpsimd", "tensor_copy", [skey], [f"ptmp{pi}"], out=dstt[:, 0:step], in_=src[:, 0:step])
                            src, skey = dstt[:, :], f"ptmp{pi}"
                            pi ^= 1
                            step *= 2
                        OP("vector", "scalar_tensor_tensor", [skey, "upool"], ["dT"], out=dT[:, j, :], in0=src[:, 16:528], scalar=1.0 / W, in1=upool[:, 16:528], op0=ALU.mult, op1=ALU.subtract)
                        if blk == 2:
                            OP("vector", "tensor_tensor", [skey, "pcr"], ["f_kk"], out=t1[:, 0:16], in0=src[:, 16:32], in1=pcr[:, g, :], op=ALU.mult)
                            OP("vector", "tensor_tensor", ["f_kk", "upool"], ["dT"], out=dT[:, j, 0:16], in0=t1[:, 0:16], in1=upool[:, 16:32], op=ALU.subtract)
                        OP("gpsimd", "tensor_copy", ["upool"], ["halo"], out=halo[:, j, :], in_=upool[:, 512:528])
                    CP(f'pool{blk}')
                    gaT, gaK = [f_ld, f_L], ["f_ld", "f_L"]
                    gbT, gbK = [f_ic, f_t0], ["f_ic", "f_t0"]
                    wv = lambda a_: a_.rearrange("(k p) c -> p k c", p=128)
                    for c2 in range(8):
                        vga, kga = load_slab(wv(w_in[:, G_GA + 256 * c2:G_GA + 256 * c2 + 256]), 16, 256, tag=("win", G_GA + 256 * c2, 256))
                        for u_ in range(2):
                            for k in range(16):
                                MM(pbank[u_][:, :], vga[:, k, u_ * 128:(u_ + 1) * 128], hT[:, k, 0:512], k == 0, k == 15, [kga, "hT"], [f"pb{u_}"])
                            OP("scalar", "activation", [f"pb{u_}"], [gaK[u_]], out=gaT[u_][:], in_=pbank[u_][:, :], func=AF.Sigmoid)
                        vgb, kgb = load_slab(wv(w_in[:, G_GB + 256 * c2:G_GB + 256 * c2 + 256]), 16, 256, tag=("win", G_GB + 256 * c2, 256))
                        for u_ in range(2):
                            for k in range(16):
                                MM(pbank[u_][:, :], vgb[:, k, u_ * 128:(u_ + 1) * 128], hT[:, k, 0:512], k == 0, k == 15, [kgb, "hT"], [f"pb{u_}"])
                            OP("scalar", "activation", [f"pb{u_}"], [gbK[u_]], out=gbT[u_][:], in_=pbank[u_][:, :], func=AF.Sigmoid)
                        vro, kro = load_slab(wv(w_ro[:, c2 * 256:(c2 + 1) * 256]), 8, 256, tag=("wro", c2))
                        for u_ in range(2):
                            c = 2 * c2 + u_
                            for k in range(8):
                                MM(pbank[0][:, :], vro[:, k, u_ * 128:(u_ + 1) * 128], yrT[:, k, :], k == 0, k == 7, [kro, "yrT"], ["pb0"])
                            g = c // 4
                            vpo, kpo = load_slab(w_pool[g, :, (c % 4) * 128:(c % 4 + 1) * 128].rearrange("(k p) c -> p k c", p=128), 2, 128, tag=("wpool", c))
                            for k in range(2):
                                MM(pbank[1][:, :], vpo[:, k, :], dT[:, 2 * g + k, :], k == 0, k == 1, [kpo, "dT"], ["pb1"])
                            OP("vector", "tensor_tensor", ["pb0", gaK[u_]], ["f_kk"], out=t1[:], in0=pbank[0][:, :], in1=gaT[u_][:], op=ALU.mult)
                            OP("vector", "scalar_tensor_tensor", ["pb1", "psc", gbK[u_]], ["f_km"], out=t2[:], in0=pbank[1][:, :], scalar=psc[:, c:c + 1], in1=gbT[u_][:], op0=ALU.mult, op1=ALU.mult)
                            OP("gpsimd", "tensor_tensor", ["f_kk", "f_km"], ["mixT"], out=mixT[:, c, :], in0=t1[:], in1=t2[:], op=ALU.add)
                            if debug:
                                OP("gpsimd", "tensor_tensor", ["f_kk", "f_km"], ["f_t1"], out=f_t1[:], in0=t1[:], in1=t2[:], op=ALU.add)
                                LD("sync", dbg["mixin"][:, c, tcol:tcol + 512], f_t1[:], [], ["f_t1"])
                    CP(f'comb{blk}')
                    P.barrier()
                    st1.close()
                    st2 = ExitStack(); scopes.append(st2)
                    sb2 = lambda n, s_, d=F32: st2.enter_context(nc.sbuf_tensor(f"{n}_c{blk}", s_, d))
                    xs = [sb2(f"xs{i}", [128, D]) for i in range(2)]
                    x1p4 = sb2("x1p4", [128, 4, D]); lnst = sb2("lnst", [128, 4, 6]); lnag = sb2("lnag", [128, 2]); h2t = sb2("h2t", [128, D], BF16)
                    lnw = sb2("lnw", [128, 2, D]); modb = sb2("modb", [128, 3, D])
                    LD("sync", lnw[:, 0, :], lnb[0], ["lnw"]); LD("sync", lnw[:, 1, :], lnb[1], ["lnw"])
                    for k_ in range(3):
                        LD("sync", modb[:, k_, :], modb_d[k_], ["modb"])
                    if stage >= 2:
                        h2T = sb2("h2T", [128, 16, 128]); wr = sb2("wr", [128, 16, NEXP]); brb = sb2("brb", [128, NEXP]); lgt = sb2("lgt", [128, NEXP])
                        LD("sync", wr[:], w_router.rearrange("(k p) e -> p k e", p=128), ["wr"])
                        LD("sync", brb[:], b_router_b, ["brb"])
                    plan([(("wout", 0, s8_), w_out[:, s8_ * 256:(s8_ + 1) * 256].rearrange("(k p) c -> p k c", p=128), 16, 256) for s8_ in range(8)])
                    for s8 in range(8):
                        vwo, kwo = load_slab(w_out[:, s8 * 256:(s8 + 1) * 256].rearrange("(k p) c -> p k c", p=128), 16, 256, tag=("wout", 0, s8))
                        dsl = slice(s8 * 256, (s8 + 1) * 256)
                        for tt in range(4):
                            pbk, pkey = pbank[tt % 2], f"pb{tt % 2}"
                            for k in range(16):
                                MM(pbk[:, 0:256], mixT[:, k, tt * 128:(tt + 1) * 128], vwo[:, k, :], k == 0, k == 15, [kwo, "mixT"], [pkey])
                            OP("vector", "tensor_tensor", [pkey, "modb"], [f"x1p{tt}"], out=x1p4[:, tt, dsl], in0=pbk[:, 0:256], in1=modb[:, 0, dsl], op=ALU.mult)
                    for tt in range(4):
                        x1p = x1p4[:, tt, :]
                        LD("sync", xs[0][:], xo[tcol + tt * 128:tcol + (tt + 1) * 128, :], ["xs0"])
                        OP("gpsimd", "tensor_scalar_mul", ["xs0"], ["xs0"], out=xs[0][:], in0=xs[0][:], scalar1=ALPHA)
                        OP("gpsimd", "tensor_tensor", [f"x1p{tt}", "xs0"], ["x1p"], out=x1p, in0=x1p, in1=xs[0][:], op=ALU.add)
                        for q in range(4):
                            OP("vector", "bn_stats", ["x1p"], ["lnst"], out=lnst[:, q, :], in_=x1p[:, q * 512:(q + 1) * 512])
                        OP("vector", "bn_aggr", ["lnst"], ["lnag"], out=lnag[:], in_=lnst[:].rearrange("p a b -> p (a b)"))
                        OP("scalar", "activation", ["lnag"], ["lnag"], out=lnag[:, 1:2], in_=lnag[:, 1:2], func=AF.Sqrt, bias=epsL[:, 0:1])
                        OP("vector", "reciprocal", ["lnag"], ["lnag"], out=lnag[:, 1:2], in_=lnag[:, 1:2])
                        OP("vector", "tensor_scalar", ["x1p", "lnag"], ["x1p"], out=x1p, in0=x1p, scalar1=lnag[:, 0:1], scalar2=lnag[:, 1:2], op0=ALU.subtract, op1=ALU.mult)
                        OP("gpsimd", "tensor_tensor", ["x1p", "lnw"], ["x1p"], out=x1p, in0=x1p, in1=lnw[:, 0, :], op=ALU.mult)
                        OP("vector", "tensor_tensor", ["x1p", "lnw"], ["x1p"], out=x1p, in0=x1p, in1=lnw[:, 1, :], op=ALU.add)
                        LD("sync", x1_d[tcol + tt * 128:tcol + (tt + 1) * 128, :], x1p, [], ["x1p"])
                        if debug:
                            LD("sync", dbg["x1"][tcol + tt * 128:tcol + (tt + 1) * 128, :], x1p, [], ["x1p"])
                        OP("gpsimd", "tensor_tensor", ["x1p", "modb"], ["xs1"], out=xs[1][:], in0=x1p, in1=modb[:, 2, :], op=ALU.mult)
                        OP("vector", "tensor_tensor", ["xs1", "modb"], ["h2t"], out=h2t[:], in0=xs[1][:], in1=modb[:, 1, :], op=ALU.add)
                        if stage >= 2:
                            OP("gpsimd", "tensor_tensor", ["xs1", "modb"], ["xs1"], out=xs[1][:], in0=xs[1][:], in1=modb[:, 1, :], op=ALU.add)
                            for q4 in range(4):
                                for u_ in range(4):
                                    c_ = q4 * 4 + u_
                                    TR(pbank[6][:, u_ * 128:(u_ + 1) * 128], xs[1][:, c_ * 128:(c_ + 1) * 128], ident, ["xs1", "cm"], ["pb6"])
                                OP("scalar", "activation", ["pb6"], ["h2T"], out=h2T[:, q4 * 4:(q4 + 1) * 4, :], in_=pbank[6][:, :].rearrange("p (u q) -> p u q", q=128), func=AF.Identity)
                            for c_ in range(16):
                                MM(pbank[2][:, 0:NEXP], h2T[:, c_, :], wr[:, c_, :], c_ == 0, c_ == 15, ["h2T", "wr"], ["pb2"])
                            OP("vector", "tensor_tensor", ["pb2", "brb"], ["lgt"], out=lgt[:], in0=pbank[2][:, 0:NEXP], in1=brb[:], op=ALU.add)
                            LD("sync", lg_d[tcol + tt * 128:tcol + (tt + 1) * 128, :], lgt[:], [], ["lgt"])
                        LD("sync", h2_d[tcol + tt * 128:tcol + (tt + 1) * 128, :], h2t[:], [], ["h2t"])
                    P.barrier()
                    st2.close()
            except _Stop:
                P.barrier()
                for sc_ in reversed(scopes):
                    sc_.close()
            P.barrier()
        if stage == 1:
            st_p.close()
            with ExitStack() as st_o:
                ot = st_o.enter_context(nc.sbuf_tensor("ot", [128, D], F32))
                OP("gpsimd", "memset", [], ["ot"], ap=ot[:], constant=0.0)
                for tt in range(8):
                    LD("sync", out[tt * 128:(tt + 1) * 128, :], ot[:], [], ["ot"])
                P.barrier()
            P.emit(nc, st)
            return nc
        st_p.close()
        moe_phase(nc, P, st, dict(OP=OP, MM=MM, TR=TR, LD=LD, slab=slab, NSL=NSL, pbank=pbank, ptb=ptb, cm=cm, cmb=cmb, epsL=epsL,
                                  h2_d=h2_d, x1_d=x1_d, lg_d=lg_d, modb_d=modb_d, lnb=lnb, w_gu=w_gu, b_guT=b_guT, w_down=w_down, b_down=b_down,
                                  iota_cap=iota_cap, out=out, debug=debug, dbg=dbg, n_exp=n_exp))
        P.emit(nc, st)
    return nc


def moe_phase(nc, P, st, env):
    OP, MM, TR, LD = env["OP"], env["MM"], env["TR"], env["LD"]
    slab, NSL, pbank, ptb, cm, cmb, epsL = env["slab"], env["NSL"], env["pbank"], env["ptb"], env["cm"], env["cmb"], env["epsL"]
    h2_d, x1_d, lg_d, modb_d, lnb = env["h2_d"], env["x1_d"], env["lg_d"], env["modb_d"], env["lnb"]
    w_gu, b_guT, w_down, b_down, iota_cap, out = env["w_gu"], env["b_guT"], env["w_down"], env["b_down"], env["iota_cap"], env["out"]
    debug, dbg, n_exp = env["debug"], env["dbg"], env["n_exp"]
    ident, m_su, ones_f = cm[:, 0, :], cm[:, 1, :], cm[:, 5, :]
    ident_b = cmb[:, 0, :]
    NT = T_OWN // 128
    NR = CAP // 128
    with ExitStack() as sm:
        sb = lambda n, s_, d=F32: sm.enter_context(nc.sbuf_tensor(n, s_, d))
        h2 = sb("h2", [128, NT, D], BF16)
        acc = sb("acc", [128, NT, D])
        iot = sb("iot", [128, CAP])
        lg = sb("lg", [128, NT, NEXP]); pm = sb("pm", [128, NT, NEXP]); pos = sb("pos", [128, NT, NEXP]); msk = sb("msk", [128, NT, NEXP])
        mx8 = sb("mx8", [128, 8]); ssum = sb("ssum", [128, 1]); nmx = sb("nmx", [128, 1])
        bgu = [sb(f"bgu{i}", [128, 32]) for i in range(2)]
        bdn = sb("bdn", [32, D], BF16)
        pmT = sb("pmT", [32, T_OWN], BF16)
        sm2 = ExitStack()
        sb2 = lambda n, s_, d=F32: sm2.enter_context(nc.sbuf_tensor(n, s_, d))
        XT = sb2("XT", [128, 16, CAP], BF16)
        actT = sb2("actT", [128, 16, CAP], BF16)
        Sel = sb2("Sel", [128, NT, CAP], BF16)
        SelW = [sb2(f"SelW{i}", [128, CAP], BF16) for i in range(2)]
        SelP = sb2("SelP", [128, NR, T_OWN], BF16)
        Yb = [sb2(f"Yb{i}", [128, NR, 256], BF16) for i in range(2)]
        gc = sb2("gc", [128, CAP]); sg = sb2("sg", [128, CAP]); u1 = sb2("u1", [128, CAP])

        for i in range(NT):
            LD("sync", h2[:, i, :], h2_d[i * 128:(i + 1) * 128, :], [f"h2_{i}"])
        LD("sync", lg[:], lg_d.rearrange("(i p) e -> p i e", p=128), ["lg"])
        LD("sync", iot[:], iota_cap, ["iot"])
        LD("gpsimd", bdn[:], b_down, ["bdn"])
        for i in range(NT):
            OP("gpsimd", "memset", [], [f"acc{i}"], ap=acc[:, i, :], constant=0.0)
        for i in range(NT):
            OP("vector", "max", ["lg"], ["mx8"], out=mx8[:], in_=lg[:, i, :])
            OP("vector", "tensor_scalar", ["lg", "mx8"], ["msk"], out=msk[:, i, :], in0=lg[:, i, :], scalar1=mx8[:, 3:4], scalar2=None, op0=ALU.is_ge)
            OP("vector", "tensor_scalar_mul", ["mx8"], ["nmx"], out=nmx[:], in0=mx8[:, 0:1], scalar1=-1.0)
            OP("scalar", "activation", ["lg", "nmx"], ["pm"], out=pm[:, i, :], in_=lg[:, i, :], func=AF.Exp, bias=nmx[:, 0:1], scale=1.0)
            OP("vector", "tensor_tensor", ["pm", "msk"], ["pm"], out=pm[:, i, :], in0=pm[:, i, :], in1=msk[:, i, :], op=ALU.mult)
            OP("vector", "tensor_reduce", ["pm"], ["ssum"], out=ssum[:], in_=pm[:, i, :], axis=mybir.AxisListType.X, op=ALU.add)
            OP("vector", "reciprocal", ["ssum"], ["ssum"], out=ssum[:], in_=ssum[:])
            OP("vector", "tensor_scalar_mul", ["pm", "ssum"], ["pm"], out=pm[:, i, :], in0=pm[:, i, :], scalar1=ssum[:, 0:1])
        for i in range(NT):
            pb = pbank[2 + (i % 2)]
            pkey = f"pb{2 + (i % 2)}"
            MM(pb[:, 0:NEXP], m_su, msk[:, i, :], True, i == 0, ["cm", "msk"], [pkey])
            for i2 in range(i):
                MM(pb[:, 0:NEXP], ones_f, msk[:, i2, :], False, i2 == i - 1, ["cm", "msk"], [pkey])
            OP("vector", "tensor_tensor", [pkey, "msk"], ["pos"], out=pos[:, i, :], in0=pb[:, 0:NEXP], in1=msk[:, i, :], op=ALU.mult)
            OP("vector", "tensor_tensor", ["pos", "msk"], ["pos"], out=pos[:, i, :], in0=pos[:, i, :], in1=msk[:, i, :], op=ALU.add)
            OP("vector", "tensor_scalar_add", ["pos"], ["pos"], out=pos[:, i, :], in0=pos[:, i, :], scalar1=-1.0)
            OP("vector", "tensor_copy", ["pm"], ["SelW0"], out=SelW[0][:, 0:NEXP], in_=pm[:, i, :])
            TR(ptb[0:NEXP, 0:128], SelW[0][:, 0:NEXP], ident_b, ["SelW0", "cmb"], ["ptb"])
            OP("vector", "tensor_copy", ["ptb"], ["pmT"], out=pmT[:, i * 128:(i + 1) * 128], in_=ptb[0:NEXP, 0:128])
        if debug:
            LD("sync", dbg["pm"].rearrange("(i p) e -> p i e", p=128), pm[:], [], ["pm"])
            LD("sync", dbg["pos"].rearrange("(i p) e -> p i e", p=128), pos[:], [], ["pos"])

        slabs = []
        for e in range(n_exp):
            for j in range(16):
                slabs.append(w_gu[e, :, 256 * j:256 * (j + 1)].rearrange("(k p) c -> p k c", p=128))
            for s8 in range(8):
                slabs.append(w_down[e, :, 256 * s8:256 * (s8 + 1)].rearrange("(k p) c -> p k c", p=128))
        issued = [0]

        def get_slab(idx, look=2):
            while issued[0] <= min(idx + look, len(slabs) - 1):
                k_ = issued[0]
                b_ = k_ % NSL
                view = slab[b_][:, 0:4096].rearrange("p (k c) -> p k c", k=16)
                LD("gpsimd", view, slabs[k_], [f"slab{b_}"])
                issued[0] += 1
            b_ = idx % NSL
            return slab[b_][:, 0:4096].rearrange("p (k c) -> p k c", k=16), f"slab{b_}"

        sidx = 0
        for e in range(n_exp):
            bg = bgu[e % 2]
            bgk = f"bgu{e % 2}"
            LD("sync", bg[:], b_guT[e], [bgk])
            for i in range(NT):
                OP("vector", "tensor_scalar", ["iot", "pos"], [f"Sel{i}"], out=Sel[:, i, :], in0=iot[:], scalar1=pos[:, i, e:e + 1], scalar2=None, op0=ALU.is_equal)
                sw, swk = SelW[i % 2], f"SelW{i % 2}"
                OP("vector", "tensor_scalar", ["iot", "pos", "pm"], [swk], out=sw[:], in0=iot[:], scalar1=pos[:, i, e:e + 1], scalar2=pm[:, i, e:e + 1], op0=ALU.is_equal, op1=ALU.mult)
                for r in range(NR):
                    TR(ptb[:, r * 128:(r + 1) * 128], sw[:, r * 128:(r + 1) * 128], ident_b, [swk, "cmb"], ["ptb"])
                OP("scalar", "activation", ["ptb"], [f"SelP{i}"], out=SelP[:, :, i * 128:(i + 1) * 128], in_=ptb[:, 0:NR * 128].rearrange("p (r q) -> p r q", q=128), func=AF.Identity)
            for c in range(16):
                pb, pkey = pbank[c % 2], f"pb{c % 2}"
                for i in range(NT):
                    MM(pb[:, 0:CAP], h2[:, i, c * 128:(c + 1) * 128], Sel[:, i, :], i == 0, i == NT - 1, [f"h2_{i}", f"Sel{i}"], [pkey])
                OP("scalar", "activation", [pkey], [f"XT{c}"], out=XT[:, c, :], in_=pb[:, 0:CAP], func=AF.Identity)
            xkeys = [f"XT{c}" for c in range(16)]
            for j in range(16):
                view, skey = get_slab(sidx); sidx += 1
                for k in range(16):
                    MM(pbank[2][:, 0:CAP], view[:, k, 0:256:2], XT[:, k, :], k == 0, k == 15, [skey, f"XT{k}"], ["pb2"])
                for k in range(16):
                    MM(pbank[3][:, 0:CAP], view[:, k, 1:256:2], XT[:, k, :], k == 0, k == 15, [skey, f"XT{k}"], ["pb3"])
                OP("vector", "tensor_scalar", ["pb2", bgk], ["gc"], out=gc[:], in0=pbank[2][:, 0:CAP], scalar1=bg[:, j:j + 1], scalar2=7.0, op0=ALU.add, op1=ALU.min)
                OP("scalar", "activation", ["gc"], ["sg"], out=sg[:], in_=gc[:], func=AF.Sigmoid, scale=1.702)
                OP("vector", "tensor_scalar", ["pb3", bgk], ["u1"], out=u1[:], in0=pbank[3][:, 0:CAP], scalar1=bg[:, 16 + j:17 + j], scalar2=7.0, op0=ALU.add, op1=ALU.min)
                OP("vector", "tensor_scalar", ["u1"], ["u1"], out=u1[:], in0=u1[:], scalar1=-7.0, scalar2=1.0, op0=ALU.max, op1=ALU.add)
                OP("vector", "tensor_tensor", ["gc", "sg"], ["sg"], out=sg[:], in0=gc[:], in1=sg[:], op=ALU.mult)
                OP("vector", "tensor_tensor", ["u1", "sg"], [f"act{j}"], out=actT[:, j, :], in0=u1[:], in1=sg[:], op=ALU.mult)
            akeys = [f"act{j}" for j in range(16)]
            for s8 in range(8):
                view, skey = get_slab(sidx); sidx += 1
                yb, ybk = Yb[s8 % 2], f"Yb{s8 % 2}"
                dsl = slice(s8 * 256, (s8 + 1) * 256)
                for r in range(NR):
                    pb, pkey = pbank[r % 2], f"pb{r % 2}"
                    for j in range(16):
                        MM(pb[:, 0:256], actT[:, j, r * 128:(r + 1) * 128], view[:, j, :], j == 0, j == 15, [skey, f"act{j}"], [pkey])
                    OP("scalar", "activation", [pkey], [ybk], out=yb[:, r, :], in_=pb[:, 0:256], func=AF.Identity)
                for i in range(NT):
                    pb, pkey = pbank[4 + (i % 2)], f"pb{4 + (i % 2)}"
                    for r in range(NR):
                        MM(pb[:, 0:256], SelP[:, r, i * 128:(i + 1) * 128], yb[:, r, :], r == 0, r == NR - 1, [f"SelP{i}", ybk], [pkey])
                    OP("vector", "tensor_tensor", [pkey, f"acc{i}"], [f"acc{i}"], out=acc[:, i, dsl], in0=pb[:, 0:256], in1=acc[:, i, dsl], op=ALU.add)
        P.barrier()
        sm2.close()
        for i in range(NT):
            for s8 in range(8):
                dsl = slice(s8 * 256, (s8 + 1) * 256)
                pb, pkey = pbank[4 + (s8 % 2)], f"pb{4 + (s8 % 2)}"
                MM(pb[:, 0:256], pmT[0:n_exp, i * 128:(i + 1) * 128], bdn[0:n_exp, dsl], True, True, ["pmT", "bdn"], [pkey])
                OP("vector", "tensor_tensor", [pkey, f"acc{i}"], [f"acc{i}"], out=acc[:, i, dsl], in0=pb[:, 0:256], in1=acc[:, i, dsl], op=ALU.add)
        g2b = sb("g2b", [128, D]); l2g = sb("l2g", [128, D]); l2b = sb("l2b", [128, D])
        xt = [sb(f"xt{i}", [128, D]) for i in range(2)]
        st6 = sb("st6", [128, 4, 6]); ag = sb("ag", [128, 2])
        LD("sync", g2b[:], modb_d[3], ["g2b"]); LD("sync", l2g[:], lnb[2], ["l2g"]); LD("sync", l2b[:], lnb[3], ["l2b"])
        for i in range(NT):
            x_, xk = xt[i % 2], f"xt{i % 2}"
            ak = f"acc{i}"
            LD("sync", x_[:], x1_d[i * 128:(i + 1) * 128, :], [xk])
            OP("gpsimd", "tensor_tensor", [ak, "g2b"], [ak], out=acc[:, i, :], in0=acc[:, i, :], in1=g2b[:], op=ALU.mult)
            OP("vector", "scalar_tensor_tensor", [xk, ak], [ak], out=acc[:, i, :], in0=x_[:], scalar=ALPHA, in1=acc[:, i, :], op0=ALU.mult, op1=ALU.add)
            for q in range(4):
                OP("vector", "bn_stats", [ak], ["st6"], out=st6[:, q, :], in_=acc[:, i, q * 512:(q + 1) * 512])
            OP("vector", "bn_aggr", ["st6"], ["ag"], out=ag[:], in_=st6[:].rearrange("p a b -> p (a b)"))
            OP("scalar", "activation", ["ag"], ["ag"], out=ag[:, 1:2], in_=ag[:, 1:2], func=AF.Sqrt, bias=epsL[:, 0:1])
            OP("vector", "reciprocal", ["ag"], ["ag"], out=ag[:, 1:2], in_=ag[:, 1:2])
            OP("vector", "tensor_scalar", [ak, "ag"], [ak], out=acc[:, i, :], in0=acc[:, i, :], scalar1=ag[:, 0:1], scalar2=ag[:, 1:2], op0=ALU.subtract, op1=ALU.mult)
            OP("gpsimd", "tensor_tensor", [ak, "l2g"], [ak], out=acc[:, i, :], in0=acc[:, i, :], in1=l2g[:], op=ALU.mult)
            OP("vector", "tensor_tensor", [ak, "l2b"], [ak], out=acc[:, i, :], in0=acc[:, i, :], in1=l2b[:], op=ALU.add)
            LD("sync", out[i * 128:(i + 1) * 128, :], acc[:, i, :], [], [ak])
        P.barrier()


def prep_inputs(inp, stage=2, cores=range(8)):
    f = lambda a: np.ascontiguousarray(a, dtype=np.float32)
    x = inp["x"]; c = inp["c"]
    idx = np.arange(128)
    cmask = np.zeros((128, 6, 128), np.float32)
    cmask[:, 0] = np.eye(128)
    cmask[:, 1] = idx[:, None] < idx[None, :]
    cmask[:, 2] = idx[:, None] > idx[None, :]
    cmask[:, 3] = idx[:, None] <= idx[None, :]
    cmask[:, 4] = (idx[:, None] // 64) == (idx[None, :] // 64)
    cmask[:, 5] = 1.0
    segm = np.ones((128, 512), np.float32); segm[:, ::128] = 0.0
    mu = inp["mu_shift"][0]
    muT = np.zeros((128, 27), np.float32)
    for gi in range(24):
        muT[:, gi] = mu[128 * gi:128 * (gi + 1)]
    muT[:, 24] = mu[3072:3200]; muT[:, 25] = mu[3200:3328]; muT[:32, 26] = mu[3328:3360]
    vecs = [inp["w0"][0], inp["a0"][0], inp["k_k"][0], inp["k_a"][0], inp["r_k"][0].reshape(-1), inp["lnx_g"][0], inp["lnx_b"][0]]
    vecT = np.stack([v.reshape(8, 128).T for v in vecs], axis=1)
    shared = dict(
        cmask=cmask, segm=segm, w_ada=f(inp["w_ada"][0]), b_ada_b=f(np.broadcast_to(inp["b_ada"][0][None, :], (128, 6 * D))),
        w_in=f(inp["w_in"][0]), muT=muT, vecT=f(vecT), w_dup=f(inp["w_decay_up"][0]), w_iup=f(inp["w_iclr_up"][0]),
        w_gup=f(inp["w_glora_up"][0]), w_ro=f(inp["w_rwkv_out"][0]), w_pool=f(inp["w_pool"][0]),
        pscT=f(inp["pool_scale"][0].reshape(16, 128).T), w_out=f(inp["w_out"][0]),
        lnb=f(np.stack([np.broadcast_to(inp[k][0][None, :], (128, D)) for k in ("ln1_g", "ln1_b", "ln2_g", "ln2_b")])),
    )
    if stage >= 2:
        shared.update(
            w_router=f(inp["w_router"][0]), b_router_b=f(np.broadcast_to(inp["b_router"][0][None, :], (128, NEXP))),
            w_gu=f(inp["w_gu"][0]), w_down=f(inp["w_down"][0]), b_down=f(inp["b_down"][0]),
            b_guT=f(np.concatenate([inp["b_gu"][0][:, 0::2].reshape(NEXP, 16, 128), inp["b_gu"][0][:, 1::2].reshape(NEXP, 16, 128)], axis=1).transpose(0, 2, 1)),
            iota_cap=f(np.broadcast_to(np.arange(CAP, dtype=np.float32)[None, :], (128, CAP))),
        )
    maps = []
    for core in cores:
        b, half = core // 2, core % 2
        m = dict(shared)
        m["xo"] = f(x[b, half * T_OWN:(half + 1) * T_OWN])
        m["xp"] = f(x[b, 0:T_OWN])
        m["cT"] = f(c[b].reshape(16, 128).T)
        m["flag"] = np.full((128, 1), float(half), np.float32)
        pc = np.zeros((128, 4, 16), np.float32)
        for g, W in enumerate((2, 4, 8, 16)):
            pc[:, g, :] = 1.0 / W if half == 1 else 1.0 / np.minimum(np.arange(16) + 1, W)
        m["pcorr"] = pc
        maps.append(m)
    return maps


_NC_CACHE = {}


def kernel(**inputs):
    if "full" not in _NC_CACHE:
        _NC_CACHE["full"] = build_program(stage=2, debug=False)
    nc = _NC_CACHE["full"]
    maps = prep_inputs(inputs, stage=2)
    res = run_bass_kernel_spmd(nc, maps, core_ids=list(range(8)))
    out = np.zeros((4, 2048, D), np.float32)
    for core in range(8):
        b, half = core // 2, core % 2
        out[b, half * T_OWN:(half + 1) * T_OWN] = res.results[core]["out"]
    return out
```
